# Optimizing a Trainium2 kernel written in Bass

```python
import math
import jax, jax.numpy as jnp
from jax import lax
import numpy as np

D_MODEL = 1024
BATCH = 8
SEQ = 4096
DEPTH = 4

HEAD_DIM = 64
HEADS_PER_GROUP = 4
GROUP_WIDTH = HEADS_PER_GROUP * HEAD_DIM
N_MIXERS = 4
D_MIX = N_MIXERS * GROUP_WIDTH

MLA_Q_RANK = 192
MLA_KV_RANK = 128
MLA_NOPE_DIM = HEAD_DIM
MLA_ROPE_DIM = 32
MLA_V_DIM = HEAD_DIM
ROPE_THETA = 10000.0

Q_BLOCK = 128

MOBA_BLOCK = 256
MOBA_TOPK = 3
MOBA_Q_CHUNK = 32

CONV_WIDTH = 31

NUM_BUCKETS = 32
MAX_DISTANCE = 1024

NORM_EPS = 1e-6

COLS_MLA = MLA_Q_RANK + MLA_KV_RANK + MLA_ROPE_DIM
COLS_SB = 3 * GROUP_WIDTH
COLS_MOBA = 3 * GROUP_WIDTH
COLS_CONV = 2 * GROUP_WIDTH
COLS_GATE = D_MIX
D_IN_PROJ = COLS_MLA + COLS_SB + COLS_MOBA + COLS_CONV + COLS_GATE
SPLITS = [COLS_MLA, COLS_MLA + COLS_SB, COLS_MLA + COLS_SB + COLS_MOBA,
          COLS_MLA + COLS_SB + COLS_MOBA + COLS_CONV]

kernel_name = "hymba_mla_stickbreak_moba_conformer"


def rms_norm(x, g):
    x32 = x.astype(jnp.float32)
    y = x32 * lax.rsqrt(jnp.mean(x32 * x32, axis=-1, keepdims=True) + NORM_EPS)
    return (y * g.astype(jnp.float32)).astype(x.dtype)


def layer_norm(x, g, b):
    x32 = x.astype(jnp.float32)
    mu = jnp.mean(x32, axis=-1, keepdims=True)
    xc = x32 - mu
    var = jnp.mean(xc * xc, axis=-1, keepdims=True)
    y = xc * lax.rsqrt(var + NORM_EPS)
    return (y * g.astype(jnp.float32) + b.astype(jnp.float32)).astype(x.dtype)


def split_heads(t, n_heads):
    B, S, E = t.shape
    return t.reshape(B, S, n_heads, E // n_heads).transpose(0, 2, 1, 3)


def merge_heads(t):
    B, H, S, d = t.shape
    return t.transpose(0, 2, 1, 3).reshape(B, S, H * d)


def to_blocks(t, blk):
    B, H, S, d = t.shape
    return t.reshape(B, H, S // blk, blk, d).transpose(2, 0, 1, 3, 4)


def from_blocks(t):
    n, B, H, blk, d = t.shape
    return t.transpose(1, 2, 0, 3, 4).reshape(B, H, n * blk, d)


def rope_tables(pos):
    half = MLA_ROPE_DIM // 2
    freqs = ROPE_THETA ** (-jnp.arange(half, dtype=jnp.float32) / half)
    ang = pos.astype(jnp.float32)[:, None] * freqs[None, :]
    return jnp.cos(ang), jnp.sin(ang)


def apply_rope(x, cos, sin):
    half = x.shape[-1] // 2
    c = cos.astype(x.dtype)
    s = sin.astype(x.dtype)
    x1, x2 = x[..., :half], x[..., half:]
    return jnp.concatenate([x1 * c - x2 * s, x1 * s + x2 * c], axis=-1)


def t5_bucket(dist):
    n = jnp.maximum(dist, 0)
    max_exact = NUM_BUCKETS // 2
    n_large = jnp.maximum(n, max_exact).astype(jnp.float32)
    large = max_exact + (jnp.log(n_large / max_exact) / math.log(MAX_DISTANCE / max_exact)
                         * (NUM_BUCKETS - max_exact)).astype(jnp.int32)
    large = jnp.minimum(large, NUM_BUCKETS - 1)
    return jnp.where(n < max_exact, n, large)


def mla_attention(q_nope, q_rope, k_nope, k_rope, v):
    S = q_nope.shape[2]
    scale = (MLA_NOPE_DIM + MLA_ROPE_DIM) ** -0.5
    key_pos = jnp.arange(S, dtype=jnp.int32)

    def one_block(args):
        qn_b, qr_b, i = args
        s = (jnp.einsum('bhqd,bhkd->bhqk', qn_b, k_nope)
             + jnp.einsum('bhqd,bkd->bhqk', qr_b, k_rope)).astype(jnp.float32) * scale
        qpos = i * Q_BLOCK + jnp.arange(Q_BLOCK, dtype=jnp.int32)
        s = jnp.where(key_pos[None, :] <= qpos[:, None], s, -jnp.inf)
        p = jax.nn.softmax(s, axis=-1).astype(v.dtype)
        return jnp.einsum('bhqk,bhkd->bhqd', p, v)

    n = S // Q_BLOCK
    out = lax.map(one_block, (to_blocks(q_nope, Q_BLOCK), to_blocks(q_rope, Q_BLOCK),
                              jnp.arange(n, dtype=jnp.int32)))
    return from_blocks(out)


def stick_breaking_attention(q, k, v):
    S = q.shape[2]
    scale = q.shape[-1] ** -0.5
    key_pos = jnp.arange(S, dtype=jnp.int32)

    def one_block(args):
        q_b, i = args
        z = jnp.einsum('bhqd,bhkd->bhqk', q_b, k).astype(jnp.float32) * scale
        qpos = i * Q_BLOCK + jnp.arange(Q_BLOCK, dtype=jnp.int32)
        past = key_pos[None, :] < qpos[:, None]
        log_beta = jax.nn.log_sigmoid(z)
        log_1m_beta = jnp.where(past, jax.nn.log_sigmoid(-z), 0.0)
        suffix = lax.cumsum(log_1m_beta, axis=3, reverse=True) - log_1m_beta
        a = jnp.where(past, jnp.exp(log_beta + suffix), 0.0).astype(v.dtype)
        return jnp.einsum('bhqk,bhkd->bhqd', a, v)

    n = S // Q_BLOCK
    out = lax.map(one_block, (to_blocks(q, Q_BLOCK), jnp.arange(n, dtype=jnp.int32)))
    return from_blocks(out)


def moba_attention(q, k, v, rel_bias):
    B, H, S, d = q.shape
    nb = -(-S // MOBA_BLOCK)
    pad = nb * MOBA_BLOCK - S
    k_p = jnp.pad(k, ((0, 0), (0, 0), (0, pad), (0, 0)))
    v_p = jnp.pad(v, ((0, 0), (0, 0), (0, pad), (0, 0)))
    k_blocks = k_p.reshape(B, H, nb, MOBA_BLOCK, d)
    v_blocks = v_p.reshape(B, H, nb, MOBA_BLOCK, d)
    k_mean = jnp.mean(k_blocks.astype(jnp.float32), axis=3)
    top_k = min(MOBA_TOPK, nb)
    scale = d ** -0.5
    offs = jnp.arange(MOBA_BLOCK, dtype=jnp.int32)
    blk_ids = jnp.arange(nb, dtype=jnp.int32)
    bi = jnp.arange(B)[:, None, None, None]
    hi = jnp.arange(H)[None, :, None, None]
    bias_t = rel_bias.T

    def one_chunk(args):
        q_c, i = args
        start = i * MOBA_Q_CHUNK
        qpos = start + jnp.arange(MOBA_Q_CHUNK, dtype=jnp.int32)
        own_blk = start // MOBA_BLOCK
        gate = jnp.einsum('bhqd,bhnd->bhqn', q_c.astype(jnp.float32), k_mean)
        gate = jnp.where((blk_ids < own_blk)[None, :], gate, -jnp.inf)
        _, idx = lax.top_k(gate, top_k)
        valid = idx < own_blk
        k_sel = k_blocks[bi, hi, idx]
        v_sel = v_blocks[bi, hi, idx]
        s_sel = jnp.einsum('bhqd,bhqkcd->bhqkc', q_c, k_sel).astype(jnp.float32) * scale
        kpos_sel = idx[..., None] * MOBA_BLOCK + offs
        s_sel = s_sel + bias_t[hi[..., None], t5_bucket(qpos[:, None, None] - kpos_sel)]
        s_sel = jnp.where(valid[..., None], s_sel, -jnp.inf)
        s_sel = s_sel.reshape(B, H, MOBA_Q_CHUNK, top_k * MOBA_BLOCK)
        k_own = lax.dynamic_slice_in_dim(k_p, own_blk * MOBA_BLOCK, MOBA_BLOCK, axis=2)
        v_own = lax.dynamic_slice_in_dim(v_p, own_blk * MOBA_BLOCK, MOBA_BLOCK, axis=2)
        dist_own = qpos[:, None] - (own_blk * MOBA_BLOCK + offs)[None, :]
        s_own = (jnp.einsum('bhqd,bhcd->bhqc', q_c, k_own).astype(jnp.float32) * scale
                 + bias_t[:, t5_bucket(dist_own)])
        s_own = jnp.where(dist_own >= 0, s_own, -jnp.inf)
        p = jax.nn.softmax(jnp.concatenate([s_sel, s_own], axis=-1), axis=-1).astype(v.dtype)
        p_sel = p[..., :top_k * MOBA_BLOCK].reshape(B, H, MOBA_Q_CHUNK, top_k, MOBA_BLOCK)
        p_own = p[..., top_k * MOBA_BLOCK:]
        return (jnp.einsum('bhqkc,bhqkcd->bhqd', p_sel, v_sel)
                + jnp.einsum('bhqc,bhcd->bhqd', p_own, v_own))

    n = S // MOBA_Q_CHUNK
    out = lax.map(one_chunk, (to_blocks(q, MOBA_Q_CHUNK), jnp.arange(n, dtype=jnp.int32)))
    return from_blocks(out)


def conformer_conv(u, dw_w, dw_b, ln_g, ln_b, pw_w, pw_b):
    a, g = jnp.split(u, 2, axis=-1)
    xg = a * jax.nn.sigmoid(g)
    y = lax.conv_general_dilated(
        xg, dw_w[:, None, :].astype(xg.dtype), window_strides=(1,),
        padding=[(CONV_WIDTH - 1, 0)], dimension_numbers=('NWC', 'WIO', 'NWC'),
        feature_group_count=GROUP_WIDTH) + dw_b
    y = jax.nn.silu(layer_norm(y, ln_g, ln_b))
    return jnp.einsum('bsc,ce->bse', y, pw_w) + pw_b


def setup_inputs(seed: int = 0) -> dict:
    key = jax.random.key(seed)
    ks = jax.random.split(key, 16)
    f32 = jnp.float32

    def nrm(k, shape, fan_in):
        return jax.random.normal(k, shape, f32) * (fan_in ** -0.5)

    def gain(k, shape):
        return 1.0 + 0.05 * jax.random.normal(k, shape, f32)

    return {
        "x": jax.random.normal(ks[0], (BATCH, SEQ, D_MODEL), f32),
        "pre_norm_g": gain(ks[1], (DEPTH, D_MODEL)),
        "w_in": nrm(ks[2], (DEPTH, D_MODEL, D_IN_PROJ), D_MODEL),
        "mla_q_norm_g": gain(ks[3], (DEPTH, MLA_Q_RANK)),
        "mla_w_uq": nrm(ks[4], (DEPTH, MLA_Q_RANK, HEADS_PER_GROUP * (MLA_NOPE_DIM + MLA_ROPE_DIM)), MLA_Q_RANK),
        "mla_kv_norm_g": gain(ks[5], (DEPTH, MLA_KV_RANK)),
        "mla_w_ukv": nrm(ks[6], (DEPTH, MLA_KV_RANK, HEADS_PER_GROUP * (MLA_NOPE_DIM + MLA_V_DIM)), MLA_KV_RANK),
        "rel_bias": 0.2 * jax.random.normal(ks[7], (NUM_BUCKETS, HEADS_PER_GROUP), f32),
        "conv_dw_w": nrm(ks[8], (DEPTH, CONV_WIDTH, GROUP_WIDTH), CONV_WIDTH),
        "conv_dw_b": 0.02 * jax.random.normal(ks[9], (DEPTH, GROUP_WIDTH), f32),
        "conv_ln_g": gain(ks[10], (DEPTH, GROUP_WIDTH)),
        "conv_ln_b": 0.02 * jax.random.normal(ks[11], (DEPTH, GROUP_WIDTH), f32),
        "conv_pw_w": nrm(ks[12], (DEPTH, GROUP_WIDTH, GROUP_WIDTH), GROUP_WIDTH),
        "conv_pw_b": 0.02 * jax.random.normal(ks[13], (DEPTH, GROUP_WIDTH), f32),
        "w_out": nrm(ks[14], (DEPTH, D_MIX, D_MODEL), D_MIX),
        "post_norm_g": gain(ks[15], (DEPTH, D_MODEL)),
    }


def reference(x, pre_norm_g, w_in, mla_q_norm_g, mla_w_uq, mla_kv_norm_g, mla_w_ukv,
              rel_bias, conv_dw_w, conv_dw_b, conv_ln_g, conv_ln_b, conv_pw_w, conv_pw_b,
              w_out, post_norm_g):
    S = x.shape[1]
    cos, sin = rope_tables(jnp.arange(S, dtype=jnp.int32))
    H = HEADS_PER_GROUP
    for l in range(DEPTH):
        h = rms_norm(x, pre_norm_g[l])
        u = jnp.einsum('bsd,de->bse', h, w_in[l])
        u_mla, u_sb, u_moba, u_conv, u_gate = jnp.split(u, SPLITS, axis=-1)

        c_q, c_kv, k_rope_raw = jnp.split(u_mla, [MLA_Q_RANK, MLA_Q_RANK + MLA_KV_RANK], axis=-1)
        qa = split_heads(jnp.einsum('bsr,re->bse', rms_norm(c_q, mla_q_norm_g[l]), mla_w_uq[l]), H)
        q_nope = qa[..., :MLA_NOPE_DIM]
        q_rope = apply_rope(qa[..., MLA_NOPE_DIM:], cos, sin)
        kva = split_heads(jnp.einsum('bsr,re->bse', rms_norm(c_kv, mla_kv_norm_g[l]), mla_w_ukv[l]), H)
        k_nope = kva[..., :MLA_NOPE_DIM]
        v_a = kva[..., MLA_NOPE_DIM:]
        k_rope = apply_rope(k_rope_raw, cos, sin)
        o_a = merge_heads(mla_attention(q_nope, q_rope, k_nope, k_rope, v_a))

        qb, kb, vb = jnp.split(u_sb, 3, axis=-1)
        o_b = merge_heads(stick_breaking_attention(split_heads(qb, H), split_heads(kb, H), split_heads(vb, H)))

        qc, kc, vc = jnp.split(u_moba, 3, axis=-1)
        o_c = merge_heads(moba_attention(split_heads(qc, H), split_heads(kc, H), split_heads(vc, H), rel_bias))

        o_d = conformer_conv(u_conv, conv_dw_w[l], conv_dw_b[l], conv_ln_g[l], conv_ln_b[l],
                             conv_pw_w[l], conv_pw_b[l])

        y = jnp.concatenate([o_a, o_b, o_c, o_d], axis=-1) * jax.nn.silu(u_gate)
        y = jnp.einsum('bse,ed->bsd', y, w_out[l])
        x = x + rms_norm(y, post_norm_g[l])
    return x
```

```python
import contextlib
import math
import numpy as np
import concourse.bass as bass
import concourse.mybir as mybir
from concourse.bass_utils import run_bass_kernel_spmd

F32 = mybir.dt.float32
BF16 = mybir.dt.bfloat16
ALU = mybir.AluOpType
AF = mybir.ActivationFunctionType
AX = mybir.AxisListType

S = 4096
D = 1024
NL = 4
NT = 32
NB = 8
EPS = 1e-6
DBG_STOP = 99
DBG_SKIP = ''
SB_WINDOW_TILES = 3

PE, ACT, DVE, POOL, SP = "pe", "act", "dve", "pool", "sp"
COMPUTE = (PE, ACT, DVE, POOL)


class Res:
    __slots__ = ("name", "writer", "readers", "slot", "excl", "persist")

    def __init__(self, name="", excl=False, persist=False):
        self.name = name
        self.excl = excl
        self.persist = persist
        self.writer = None
        self.readers = []
        self.slot = None


class SemSlot:
    __slots__ = ("sem", "count")

    def __init__(self):
        self.sem = None
        self.count = 0


class Op:
    __slots__ = ("eng", "idx", "fn", "waits", "signal", "count", "is_dma", "dma_res",
                 "dma_count", "n_dma")

    def __init__(self, eng, idx, fn):
        self.eng = eng
        self.idx = idx
        self.fn = fn
        self.waits = []
        self.signal = False
        self.count = 0
        self.is_dma = False
        self.dma_res = None
        self.dma_count = 0
        self.n_dma = 0


class Prog:
    def __init__(self, nc):
        self.nc = nc
        self.ops = {e: [] for e in (PE, ACT, DVE, POOL, SP)}
        self.seen = {e: {} for e in self.ops}
        self.slots = []
        self.free_slots = []
        self.phase_res = []

    def release_phase_slots(self):
        for r in self.phase_res:
            if r.slot is not None:
                self.free_slots.append(r.slot)
                r.slot = None
        self.phase_res = []

    def op(self, eng, fn, reads=(), writes=(), dma_slot=None, n_dma=1):
        lst = self.ops[eng]
        o = Op(eng, len(lst), fn)
        ex = [r for r in reads if r.excl and r not in writes]
        if ex:
            writes = list(writes) + ex
        if dma_slot is not None:
            if dma_slot.slot is None:
                if self.free_slots:
                    dma_slot.slot = self.free_slots.pop()
                else:
                    dma_slot.slot = SemSlot()
                    self.slots.append(dma_slot.slot)
                if not dma_slot.persist:
                    self.phase_res.append(dma_slot)
            o.is_dma = True
            o.dma_res = dma_slot.slot
            o.n_dma = n_dma
            dma_slot.slot.count += 16 * n_dma
            o.dma_count = dma_slot.slot.count
        need = {}

        def same(d):
            return (not d.is_dma) and (not o.is_dma) and d.eng == eng

        def add(d):
            if d is None or d is o:
                return
            if d.is_dma:
                key, val = ("dma", id(d.dma_res)), d.dma_count
            else:
                key, val = ("eng", d.eng), d.idx
            if key not in need or val > need[key][0]:
                need[key] = (val, d)

        for r in reads:
            d = r.writer
            if d is not None and not (same(d) and eng == PE):
                add(d)
        for w in writes:
            d = w.writer
            if d is not None and not same(d):
                add(d)
            for rd in w.readers:
                if not same(rd):
                    add(rd)
        seen = self.seen[eng]
        for key, (val, d) in need.items():
            if seen.get(key, -1) >= val:
                continue
            seen[key] = val
            o.waits.append(d)
            if not d.is_dma:
                d.signal = True
        for r in reads:
            r.readers.append(o)
        for w in writes:
            w.writer = o
            w.readers = []
        lst.append(o)
        return o

    def emit(self, final_waits=()):
        nc = self.nc
        final = {}
        for (e, d) in final_waits:
            final.setdefault(e, []).append(d)
            if not d.is_dma:
                d.signal = True
        for e in COMPUTE:
            c = 0
            for o in self.ops[e]:
                if o.signal:
                    c += 1
                o.count = c
        with contextlib.ExitStack() as st:
            esem = {e: st.enter_context(nc.semaphore("prog_" + e)) for e in COMPUTE}
            for i, r in enumerate(self.slots):
                r.sem = st.enter_context(nc.semaphore("dma_%d" % i))
            block = st.enter_context(nc.Block())

            def wait(eng, d):
                if d.is_dma:
                    eng.wait_ge(d.dma_res.sem, d.dma_count)
                else:
                    eng.wait_ge(esem[d.eng], d.count)

            def run(e):
                def body(eng):
                    for o in self.ops[e]:
                        for d in o.waits:
                            wait(eng, d)
                        ins = o.fn(eng)
                        if o.is_dma:
                            if not isinstance(ins, (list, tuple)):
                                ins = [ins]
                            assert len(ins) == o.n_dma, (len(ins), o.n_dma)
                            for i_ in ins:
                                i_.then_inc(o.dma_res.sem, 16)
                        elif o.signal:
                            ins.then_inc(esem[e], 1)
                    for d in final.get(e, []):
                        wait(eng, d)
                return body

            block.tensor(run(PE))
            block.scalar(run(ACT))
            block.vector(run(DVE))
            block.gpsimd(run(POOL))
            block.sync(run(SP))


def _t5_bucket(n):
    n = np.maximum(n, 0)
    max_exact = 16
    n_large = np.maximum(n, max_exact).astype(np.float32)
    large = max_exact + (np.log(n_large / max_exact) / math.log(1024 / max_exact) * (32 - max_exact)).astype(np.int32)
    large = np.minimum(large, 31)
    return np.where(n < max_exact, n, large)


GT_W = 1664


def host_consts():
    c = {}
    c["ident"] = np.eye(128, dtype=np.float32)
    p = np.arange(128)[:, None]
    q = np.arange(128)[None, :]
    c["tri_le"] = (p <= q).astype(np.float32)
    c["tri_lt"] = (p < q).astype(np.float32)
    c["tri_gt"] = (p > q).astype(np.float32)
    c["ones"] = np.ones((128, 128), np.float32)
    half = 16
    freqs = (10000.0 ** (-np.arange(half, dtype=np.float32) / half)).astype(np.float32)
    ang = np.arange(S, dtype=np.float32)[:, None] * freqs[None, :]
    cos = np.cos(ang).astype(np.float32).T
    sin = np.sin(ang).astype(np.float32).T
    sc = np.float32(96 ** -0.5)
    rq_c = np.zeros((96, S), np.float32)
    rq_s = np.zeros((96, S), np.float32)
    rq_c[0:64] = sc
    rq_c[64:80] = cos * sc
    rq_c[80:96] = cos * sc
    rq_s[64:80] = -sin * sc
    rq_s[80:96] = sin * sc
    rk_c = np.zeros((96, S), np.float32)
    rk_s = np.zeros((96, S), np.float32)
    rk_c[64:80] = cos
    rk_c[80:96] = cos
    rk_s[64:80] = -sin
    rk_s[80:96] = sin
    c["rq_c"], c["rq_s"], c["rk_c"], c["rk_s"] = rq_c, rq_s, rk_c, rk_s
    oh = np.zeros((16, S), np.float32)
    for b in range(16):
        oh[b, b * 256:(b + 1) * 256] = 1.0
    c["onehot"] = oh
    am = np.zeros((16, 16), np.float32)
    for ob in range(16):
        am[ob, ob] = 1e30
        am[ob, ob + 1:] = -1e30
    c["addm"] = np.ascontiguousarray(np.broadcast_to(am[None], (128, 16, 16)))
    return c


def host_layout(inp):
    w = {}
    w_in = inp["w_in"]
    w["w_in"] = np.ascontiguousarray(w_in)
    wk = np.zeros((NL, D, 320), np.float32)
    wk[:, :, 0:128] = w_in[:, :, 192:320]
    wk[:, :, 192:224] = w_in[:, :, 320:352]
    wk[:, :, 288:304] = w_in[:, :, 336:352]
    wk[:, :, 304:320] = w_in[:, :, 320:336]
    w["w_mla_k"] = wk
    w["gpre"] = np.ascontiguousarray(inp["pre_norm_g"].reshape(NL, 8, 128).transpose(0, 2, 1))
    gq = np.zeros((NL, 128, 2), np.float32)
    gq[:, :, 0] = inp["mla_q_norm_g"][:, 0:128]
    gq[:, 0:64, 1] = inp["mla_q_norm_g"][:, 128:192]
    w["gq"] = gq
    uq = inp["mla_w_uq"].reshape(NL, 192, 4, 96)
    uq_p = np.zeros((NL, 128, 2, 4, 96), np.float32)
    uq_p[:, :, 0] = uq[:, 0:128]
    uq_p[:, 0:64, 1] = uq[:, 128:192]
    w["w_uq"] = uq_p
    sw = np.zeros((NL, 192, 4, 96), np.float32)
    sw[:, :, :, 64:80] = uq[:, :, :, 80:96]
    sw[:, :, :, 80:96] = uq[:, :, :, 64:80]
    sw_p = np.zeros((NL, 128, 2, 4, 96), np.float32)
    sw_p[:, :, 0] = sw[:, 0:128]
    sw_p[:, 0:64, 1] = sw[:, 128:192]
    w["w_uq_sw"] = sw_p
    w["gkv"] = np.ascontiguousarray(inp["mla_kv_norm_g"].reshape(NL, 128, 1))
    ukv = inp["mla_w_ukv"].reshape(NL, 128, 4, 128)
    w["w_uk"] = np.ascontiguousarray(ukv[:, :, :, 0:64])
    w["w_uv"] = np.ascontiguousarray(ukv[:, :, :, 64:128].reshape(NL, 128, 256))
    rb = inp["rel_bias"]
    pp = np.arange(128)[:, None]
    xx = np.arange(GT_W)[None, :]
    bucket = _t5_bucket(xx - pp)
    w["gt"] = np.ascontiguousarray(rb[bucket].transpose(0, 2, 1))
    w["c31"] = np.ascontiguousarray(np.broadcast_to(rb[31][None, :], (128, 4)))
    w["dw_wT"] = np.ascontiguousarray(inp["conv_dw_w"].transpose(0, 2, 1).reshape(NL, 2, 128, 31).transpose(0, 2, 1, 3))

    def pv(a):
        return np.ascontiguousarray(a.reshape(NL, 2, 128).transpose(0, 2, 1))
    w["dw_b"] = pv(inp["conv_dw_b"])
    w["ln_g"] = pv(inp["conv_ln_g"])
    w["ln_b"] = pv(inp["conv_ln_b"])
    w["pw_b"] = pv(inp["conv_pw_b"])
    w["pw_w"] = np.ascontiguousarray(inp["conv_pw_w"])
    w["w_out"] = np.ascontiguousarray(inp["w_out"])
    w["post_g"] = np.ascontiguousarray(inp["post_norm_g"])
    w.update(host_consts())
    return w


WSHAPES = {
    "w_in": [NL, D, 3424], "w_mla_k": [NL, D, 320], "gpre": [NL, 128, 8], "gq": [NL, 128, 2],
    "w_uq": [NL, 128, 2, 4, 96], "w_uq_sw": [NL, 128, 2, 4, 96], "gkv": [NL, 128, 1],
    "w_uk": [NL, 128, 4, 64], "w_uv": [NL, 128, 256], "gt": [128, 4, GT_W], "c31": [128, 4],
    "dw_wT": [NL, 128, 2, 31], "dw_b": [NL, 128, 2], "ln_g": [NL, 128, 2], "ln_b": [NL, 128, 2],
    "pw_b": [NL, 128, 2], "pw_w": [NL, 256, 256], "w_out": [NL, D, D], "post_g": [NL, D],
    "ident": [128, 128], "tri_le": [128, 128], "tri_lt": [128, 128], "tri_gt": [128, 128],
    "ones": [128, 128], "rq_c": [96, S], "rq_s": [96, S], "rk_c": [96, S], "rk_s": [96, S],
    "onehot": [16, S], "addm": [128, 16, 16],
}


class Bank:
    def __init__(self, t):
        self.t = t
        self.res = Res(excl=True)
        self.f32 = t[:]
        self.bf = t[:].bitcast(BF16)


class RR:
    def __init__(self, items):
        self.items = items
        self.i = 0

    def next(self):
        it = self.items[self.i % len(self.items)]
        self.i += 1
        return it


class Buf:
    def __init__(self, ap, persist=False):
        self.ap = ap if isinstance(ap, bass.AP) else ap[:]
        self.res = Res(persist=persist)


ARENA_BYTES = 122880
MIX_ALL = ("mla", "sb", "moba", "conv")


def build(n_layers=NL, mixers=MIX_ALL, dbg=False):
    nc = bass.Bass("TRN2", target_bir_lowering=False)
    P = Prog(nc)
    dr = {}
    x_in = nc.dram_tensor("x", [S, D], F32, kind="ExternalInput").ap()
    for k, shp in WSHAPES.items():
        dr[k] = nc.dram_tensor(k, shp, F32, kind="ExternalInput").ap()
    out = nc.dram_tensor("out", [S, D], F32, kind="ExternalOutput").ap()
    yT_h = nc.dram_tensor("yT", [D, S], BF16, kind="ExternalOutput" if dbg else "Internal").ap()
    r_yT = [[Res() for _ in range(NB)] for _ in range(8)]
    r_xh = [Res() for _ in range(NT)]

    st = contextlib.ExitStack()
    with st:
        def sb(name, shape, dt):
            return st.enter_context(nc.sbuf_tensor("s_" + name, shape, dt))

        hT = sb("hT", [128, 8, S], BF16)
        r_hT = [Res() for _ in range(NT)]
        wK = Buf(sb("wK", [128, 8, 512], BF16), persist=True)
        wQ = Buf(sb("wQ", [128, 8, 512], BF16), persist=True)
        ident = Buf(sb("ident", [128, 128], BF16), persist=True)
        tri_le = Buf(sb("tri_le", [128, 128], BF16), persist=True)
        tri_lt = Buf(sb("tri_lt", [128, 128], BF16), persist=True)
        tri_gt = Buf(sb("tri_gt", [128, 128], F32), persist=True)
        ones = Buf(sb("ones", [128, 128], F32), persist=True)
        gpre = Buf(sb("gpre", [128, NL, 8], F32), persist=True)
        c31 = Buf(sb("c31", [128, 4], F32), persist=True)
        zeros_bf = Buf(sb("zeros_bf", [128, 512], BF16), persist=True)
        addm = Buf(sb("addm", [128, 16, 16], F32), persist=True)
        arena_t = sb("arena", [128, ARENA_BYTES // 4], F32)
        r_phase = Res("phase")
        dummy = Buf(sb("dummy", [128, 8], F32), persist=True)

        def view(off, shape, dt):
            isz = 2 if dt == BF16 else 4
            n = 1
            for s_ in shape[1:]:
                n *= s_
            nb = n * isz
            assert off % 4 == 0 and nb % 4 == 0 and off + nb <= ARENA_BYTES, (off, nb)
            ap = arena_t[:, off // 4:(off + nb) // 4]
            if dt == BF16:
                ap = ap.bitcast(BF16)
            if len(shape) == 3:
                ap = ap.rearrange("p (a b) -> p a b", a=shape[1])
            elif len(shape) == 4:
                ap = ap.rearrange("p (a b c) -> p a b c", a=shape[1], b=shape[2])
            if shape[0] != 128:
                ap = ap[0:shape[0]]
            return ap

        class Alloc:
            def __init__(self, start=0):
                self.off = start

            def get(self, shape, dt):
                isz = 2 if dt == BF16 else 4
                n = 1
                for s_ in shape[1:]:
                    n *= s_
                nb = (n * isz + 31) // 32 * 32
                v = view(self.off, shape, dt)
                self.off += nb
                return Buf(v)

        banks = [Bank(st.enter_context(nc.psum_tensor("bank%d" % i, [128, 512], F32))) for i in range(8)]
        bS = RR(banks[0:3])
        bO = RR(banks[3:5])
        bW = RR(banks[5:8])

        def dma(q, out_ap, in_ap, reads, writes, slot):
            return P.op(q, lambda e, o=out_ap, i=in_ap: e.dma_start(out=o, in_=i), reads, writes, dma_slot=slot)

        def barrier():
            P.op(DVE, lambda e: e.memset(dummy.ap, 0.0), reads=[], writes=[r_phase, dummy.res])
            P.release_phase_slots()

        def ph(reads):
            return list(reads) + [r_phase]

        def mm(o_ap, lhsT, rhs, start, stop, reads, writes, **kw):
            return P.op(PE, lambda e, o=o_ap, l=lhsT, r=rhs, s0=start, s1=stop, kw=kw:
                        e.matmul(o, l, r, start=s0, stop=s1, **kw), ph(reads), writes)

        def act(o_ap, i_ap, func, reads, writes, scale=1.0, bias=None, accum=None):
            def f(e, o=o_ap, i=i_ap, fu=func, sc=scale, b=bias, a=accum):
                kw = {}
                if b is not None:
                    kw["bias"] = b
                if a is not None:
                    kw["accum_out"] = a
                return e.activation(o, i, fu, scale=sc, **kw)
            return P.op(ACT, f, ph(reads), writes)

        def tt(eng, o_ap, a, b, op, reads, writes):
            return P.op(eng, lambda e, o=o_ap, a=a, b=b, op=op: e.tensor_tensor(o, a, b, op), ph(reads), writes)

        def ts(eng, o_ap, a, s1, s2, op0, op1, reads, writes):
            if op1 is None:
                return P.op(eng, lambda e, o=o_ap, a=a, s1=s1, op0=op0: e.tensor_scalar(o, a, s1, None, op0), ph(reads), writes)
            return P.op(eng, lambda e, o=o_ap, a=a, s1=s1, s2=s2, op0=op0, op1=op1:
                        e.tensor_scalar(o, a, s1, s2, op0, op1), ph(reads), writes)

        def cp(eng, o_ap, i_ap, reads, writes):
            return P.op(eng, lambda e, o=o_ap, i=i_ap: e.tensor_copy(o, i), ph(reads), writes)

        def rsqrt_act(o_ap, i_ap, n_mean, reads, writes):
            act(o_ap, i_ap, AF.Ln, reads, writes, scale=1.0 / n_mean, bias=EPS)
            act(o_ap, o_ap, AF.Exp, writes, writes, scale=-0.5)

        for (b_, nm) in [(ident, "ident"), (tri_le, "tri_le"), (tri_lt, "tri_lt")]:
            dma(POOL, b_.ap[:], dr[nm], [], [b_.res], b_.res)
        dma(SP, tri_gt.ap[:], dr["tri_gt"], [], [tri_gt.res], tri_gt.res)
        dma(SP, ones.ap[:], dr["ones"], [], [ones.res], ones.res)
        dma(SP, gpre.ap[:], dr["gpre"].rearrange("l p c -> p l c"), [], [gpre.res], gpre.res)
        dma(SP, c31.ap[:], dr["c31"], [], [c31.res], c31.res)
        dma(SP, addm.ap[:], dr["addm"], [], [addm.res], addm.res)
        P.op(POOL, lambda e: e.memset(zeros_bf.ap[:], 0.0), [], [zeros_bf.res])

        def load_w(buf, src_ap, ncols, col0=0):
            P.op(POOL, lambda e, o=buf.ap[:, :, col0:col0 + ncols], i=src_ap.rearrange("(c p) n -> p c n", p=128):
                 e.dma_start(out=o, in_=i), [], [buf.res], dma_slot=buf.res)

        def inproj_fm(wbuf, c0, ncols, n, bank, extra_reads=()):
            for kc in range(8):
                mm(bank.f32[0:ncols, :], wbuf.ap[:, kc, c0:c0 + ncols], hT[:, kc, n * 512:(n + 1) * 512],
                   kc == 0, kc == 7, [wbuf.res] + r_hT[4 * n:4 * n + 4] + list(extra_reads), [bank.res])

        def inproj_tm(wbuf, c0, ncols, t, bank):
            for kc in range(8):
                mm(bank.f32[:, 0:ncols], hT[:, kc, t * 128:(t + 1) * 128], wbuf.ap[:, kc, c0:c0 + ncols],
                   kc == 0, kc == 7, [wbuf.res, r_hT[t]], [bank.res])

        def layout_C():
            a = Alloc()
            L = {}
            L["w_out"] = a.get([128, 8, 1024], BF16)
            L["yt"] = [a.get([128, 8, 512], BF16) for _ in range(2)]
            L["x"] = [a.get([128, 1024], F32) for _ in range(2)]
            L["xn"] = [a.get([128, 1024], F32) for _ in range(2)]
            L["xs"] = [a.get([128, 1024], BF16) for _ in range(2)]
            L["junk"] = a.get([128, 1024], BF16)
            L["postg"] = a.get([128, 1024], F32)
            L["st"] = [a.get([128, 8], F32) for _ in range(4)]
            return L

        def norm_transpose(L, l, t, xbuf, i):
            stt = L["st"][i % 4]
            xs = L["xs"][i % 2]
            act(L["junk"].ap, xbuf.ap, AF.Square, [xbuf.res], [L["junk"].res, stt.res], accum=stt.ap[:, 0:1])
            rsqrt_act(stt.ap[:, 1:2], stt.ap[:, 0:1], float(D), [stt.res], [stt.res])
            ts(DVE, xs.ap, xbuf.ap, stt.ap[:, 1:2], None, ALU.mult, None, [xbuf.res, stt.res], [xs.res])
            bk = bW.next()
            for c in range(8):
                P.op(PE, lambda e, o=bk.bf[:, c * 128:(c + 1) * 128], i_=xs.ap[:, c * 128:(c + 1) * 128]:
                     e.transpose(o, i_, ident.ap[:]), ph([xs.res, ident.res]), [bk.res])
            tt(DVE, hT[:, :, t * 128:(t + 1) * 128], bk.bf[:, 0:1024].rearrange("p (c t) -> p c t", c=8),
               gpre.ap[:, l, :].unsqueeze(2).to_broadcast([128, 8, 128]), ALU.mult,
               [bk.res, gpre.res], [r_hT[t]])

        def phase_A0():
            barrier()
            L = layout_C()
            for t in range(NT):
                xb = L["x"][t % 2]
                dma(SP, xb.ap, x_in[t * 128:(t + 1) * 128, :], ph([]), [xb.res], xb.res)
                norm_transpose(L, 0, t, xb, t)

        def phase_C(l, last):
            barrier()
            L = layout_C()
            src = x_in if l == 0 else out
            for half in range(2):
                P.op(POOL, lambda e, o=L["w_out"].ap[:, :, half * 512:(half + 1) * 512],
                     i=dr["w_out"][l][:, half * 512:(half + 1) * 512].rearrange("(c p) n -> p c n", p=128):
                     e.dma_start(out=o, in_=i), ph([]), [L["w_out"].res], dma_slot=L["w_out"].res)
            dma(SP, L["postg"].ap, dr["post_g"][l:l + 1, :].to_broadcast([128, D]), ph([]), [L["postg"].res], L["postg"].res)
            fin = []
            for n in range(NB):
                yt = L["yt"][n % 2]
                P.op(SP, lambda e, o=yt.ap, i=yT_h[:, n * 512:(n + 1) * 512].rearrange("(c p) t -> p c t", p=128):
                     e.dma_start(out=o, in_=i), ph([r_yT[c][n] for c in range(8)]), [yt.res], dma_slot=yt.res)
                for tl in range(4):
                    t = 4 * n + tl
                    i = t
                    xb = L["x"][i % 2]
                    xn = L["xn"][i % 2]
                    stt = L["st"][(i + 2) % 4]
                    dma(SP, xb.ap, src[t * 128:(t + 1) * 128, :], ph([r_xh[t]]), [xb.res], xb.res)
                    bk2 = [bW.next(), bW.next()]
                    for hf in range(2):
                        for kc in range(8):
                            mm(bk2[hf].f32, yt.ap[:, kc, tl * 128:(tl + 1) * 128], L["w_out"].ap[:, kc, hf * 512:(hf + 1) * 512],
                               kc == 0, kc == 7, [yt.res, L["w_out"].res], [bk2[hf].res])
                    for hf in range(2):
                        act(L["junk"].ap[:, hf * 512:(hf + 1) * 512], bk2[hf].f32, AF.Square, [bk2[hf].res],
                            [L["junk"].res, stt.res], accum=stt.ap[:, 2 + hf:3 + hf])
                    tt(DVE, stt.ap[:, 4:5], stt.ap[:, 2:3], stt.ap[:, 3:4], ALU.add, [stt.res], [stt.res])
                    rsqrt_act(stt.ap[:, 5:6], stt.ap[:, 4:5], float(D), [stt.res], [stt.res])
                    for hf in range(2):
                        sl = slice(hf * 512, (hf + 1) * 512)
                        P.op(DVE, lambda e, o=xn.ap[:, sl], a=bk2[hf].f32, s_=stt.ap[:, 5:6], b=L["postg"].ap[:, sl]:
                             e.scalar_tensor_tensor(o, a, s_, b, ALU.mult, ALU.mult),
                             ph([bk2[hf].res, stt.res, L["postg"].res]), [xn.res])
                    tt(POOL, xn.ap, xn.ap, xb.ap, ALU.add, [xn.res, xb.res], [xn.res])
                    o_ = dma(POOL, out[t * 128:(t + 1) * 128, :], xn.ap, ph([xn.res]), [r_xh[t]], xn.res)
                    fin.append(o_)
                    if not last:
                        norm_transpose(L, l + 1, t, xn, i)
            return fin

        def layout_attn():
            a = Alloc()
            L = {}
            L["KT"] = a.get([128, 4, S], BF16)
            L["KT_res"] = [[Res() for _ in range(NB)] for _ in range(4)]
            L["V"] = a.get([128, NT, 4, 65], BF16)
            L["V_res"] = [Res() for _ in range(NT)]
            L["QT"] = [a.get([128, 4, 512], BF16) for _ in range(2)]
            L["QT_res"] = [[Res() for _ in range(4)] for _ in range(2)]
            L["PT"] = [a.get([128, 512], BF16) for _ in range(4)]
            L["YST"] = [a.get([64, 512], BF16) for _ in range(2)]
            sg_ = a.get([64, 4, 512], F32)
            L["SG"] = [sg_, sg_]
            sgr_ = [Res() for _ in range(4)]
            L["SG_res"] = [sgr_, sgr_]
            L["RD"] = [a.get([64, 512], F32) for _ in range(2)]
            L["DEN"] = [a.get([128, 512], F32) for _ in range(2)]
            L["GE"] = [a.get([64, 512], F32) for _ in range(2)]
            L["alloc"] = a
            return L

        def v_lhsT(L, t, h):
            return L["V"].ap[:, t, h, :]

        def init_V_ones(L):
            P.op(POOL, lambda e, o=L["V"].ap[:, :, :, 64:65]: e.memset(o, 1.0), ph([]), list(L["V_res"]))

        def gate_for_head(L, wbuf, c0, n, h, qi):
            bk = bW.next()
            inproj_fm(wbuf, c0, 64, n, bk)
            ge = L["GE"][h % 2]
            sg = L["SG"][qi].ap[:, h, :]
            r_sg = L["SG_res"][qi][h]
            act(ge.ap, bk.f32[0:64, :], AF.Exp, [bk.res], [ge.res], scale=-1.0)
            ts(POOL, ge.ap, ge.ap, 1.0, None, ALU.add, None, [ge.res], [ge.res])
            P.op(DVE, lambda e, o=ge.ap, i=ge.ap: e.reciprocal(o, i), ph([ge.res]), [ge.res])
            tt(DVE, sg, bk.f32[0:64, :], ge.ap, ALU.mult, [bk.res, ge.res], [r_sg])

        def finish_head(L, ob, n, h, qi, row0, cnt):
            rd = L["RD"][cnt % 2]
            ys = L["YST"][cnt % 2]
            den = L["DEN"][cnt % 2]
            P.op(DVE, lambda e, o=den.ap[64:65, :], i=ob.f32[64:65, :]: e.reciprocal(o, i), ph([ob.res]), [den.res])
            bb = bW.next()
            mm(bb.f32[0:64, :], ones.ap[64:65, 0:64], den.ap[64:65, :], True, True, [ones.res, den.res], [bb.res])
            tt(DVE, rd.ap, bb.f32[0:64, :], L["SG"][qi].ap[:, h, :], ALU.mult, [bb.res, L["SG_res"][qi][h]], [rd.res])
            tt(DVE, ys.ap, ob.f32[0:64, :], rd.ap, ALU.mult, [ob.res, rd.res], [ys.res])
            c, r0 = row0 // 128, row0 % 128
            dma(SP, yT_h[row0:row0 + 64, n * 512:(n + 1) * 512], ys.ap, ph([ys.res]), [r_yT[c][n]], ys.res)

        def attn_block(L, n, h, qi, dk, kind, cnt, pend, finq):
            ob = bO.next()
            qt = L["QT"][qi].ap[0:dk, h, :]
            r_q = L["QT_res"][qi][h]
            tiles = [(4 * n + r, r) for r in range(4)] + [(j, -1) for j in range(4 * n)]
            ntl = len(tiles)
            for idx, (j, r) in enumerate(tiles):
                c0 = 128 * r if r > 0 else 0
                sbk = bS.next()
                pt = L["PT"][(cnt[0]) % 4]
                cnt[0] += 1
                mm(sbk.f32[:, c0:512], L["KT"].ap[0:dk, h, j * 128:(j + 1) * 128], qt[:, c0:512], True, True,
                   [L["KT_res"][h][j // 4], r_q], [sbk.res])
                if kind == "moba" and (4 * n - j) <= 9:
                    stg = L["STG"][cnt[0] % 2]
                    dq = 512 * n - 128 * j
                    tt(DVE, stg.ap[:, c0:512], sbk.f32[:, c0:512], L["GT"].ap[:, h, dq + c0:dq + 512], ALU.add,
                       [sbk.res, L["GT"].res], [stg.res])
                    act(pt.ap[:, c0:512], stg.ap[:, c0:512], AF.Exp, [stg.res], [pt.res])
                elif kind == "moba":
                    act(pt.ap[:, c0:512], sbk.f32[:, c0:512], AF.Exp, [sbk.res, c31.res], [pt.res], bias=c31.ap[:, h:h + 1])
                else:
                    act(pt.ap[:, c0:512], sbk.f32[:, c0:512], AF.Exp, [sbk.res], [pt.res])
                if r >= 0:
                    tt(POOL, pt.ap[:, c0:c0 + 128], pt.ap[:, c0:c0 + 128], tri_le.ap[:], ALU.mult, [pt.res, tri_le.res], [pt.res])
                pend.append((ob, v_lhsT(L, j, h), pt, c0, idx == 0, idx == ntl - 1, L["V_res"][j]))
                if len(pend) > 1:
                    flush_pv(pend)
                    if idx == 0:
                        while finq:
                            finq.pop(0)()
            return ob

        def flush_pv(pend):
            (ob, lhsT, pt, c0, s0, s1, r_v) = pend.pop(0)
            mm(ob.f32[0:65, c0:512], lhsT, pt.ap[:, c0:512], s0, s1, [pt.res, r_v], [ob.res], skip_group_check=True)

        def drain(pend, finq):
            while pend:
                flush_pv(pend)
            while finq:
                finq.pop(0)()

        def mixer_mla(l):
            barrier()
            L = layout_attn()
            a = L["alloc"]
            ckv = a.get([128, 512], BF16)
            sq = a.get([128, 2, 512], F32)
            rq = a.get([128, 512], F32)
            rkv = rq
            cq = a.get([128, 2, 512], BF16)
            tq = [a.get([96, 512], F32) for _ in range(2)]
            tk = tq
            csq = [a.get([96, 512], F32) for _ in range(2)]
            t12 = [a.get([96, 512], F32) for _ in range(2)]
            rtm = a.get([128, 8], F32)
            wuq = a.get([128, 2, 4, 96], BF16)
            wsw = a.get([128, 2, 4, 96], BF16)
            wuk = a.get([128, 4, 64], BF16)
            wuv = a.get([128, 256], BF16)
            wtmp = a.get([128, 2, 4, 96], F32)
            wtmp2 = a.get([128, 2, 4, 96], F32)
            wtmp3 = a.get([128, 4, 64], F32)
            wtmp4 = a.get([128, 256], F32)
            gq = a.get([128, 2], F32)
            gkv = a.get([128, 1], F32)
            init_V_ones(L)
            load_w(wK, dr["w_mla_k"][l], 320)
            load_w(wQ, dr["w_in"][l][:, 0:192], 192)
            load_w(wQ, dr["w_in"][l][:, 2400:2656], 256, col0=192)
            dma(SP, gq.ap, dr["gq"][l], ph([]), [gq.res], gq.res)
            dma(SP, gkv.ap, dr["gkv"][l], ph([]), [gkv.res], gkv.res)
            dma(SP, wtmp.ap, dr["w_uq"][l], ph([]), [wtmp.res], wtmp.res)
            dma(SP, wtmp2.ap, dr["w_uq_sw"][l], ph([]), [wtmp2.res], wtmp2.res)
            dma(SP, wtmp3.ap, dr["w_uk"][l], ph([]), [wtmp3.res], wtmp3.res)
            dma(SP, wtmp4.ap, dr["w_uv"][l], ph([]), [wtmp4.res], wtmp4.res)
            for c in range(2):
                ts(DVE, wuq.ap[:, c], wtmp.ap[:, c], gq.ap[:, c:c + 1], None, ALU.mult, None, [wtmp.res, gq.res], [wuq.res])
                ts(DVE, wsw.ap[:, c], wtmp2.ap[:, c], gq.ap[:, c:c + 1], None, ALU.mult, None, [wtmp2.res, gq.res], [wsw.res])
            ts(DVE, wuk.ap, wtmp3.ap, gkv.ap[:, 0:1], None, ALU.mult, None, [wtmp3.res, gkv.res], [wuk.res])
            ts(DVE, wuv.ap, wtmp4.ap, gkv.ap[:, 0:1], None, ALU.mult, None, [wtmp4.res, gkv.res], [wuv.res])
            if DBG_STOP <= 1:
                return
            for n in range(NB):
                if DBG_STOP <= 2 and n >= 1:
                    return
                tkc, tks = tk[0], tk[1]
                blk = slice(n * 512, (n + 1) * 512)
                dma(SP, tkc.ap[64:96, :], dr["rk_c"][64:96, blk], ph([]), [tkc.res], tkc.res)
                dma(SP, tks.ap[64:96, :], dr["rk_s"][64:96, blk], ph([]), [tks.res], tks.res)
                b_ckv, b_kr, b_ks = bW.next(), bW.next(), bW.next()
                inproj_fm(wK, 0, 128, n, b_ckv)
                inproj_fm(wK, 128, 96, n, b_kr)
                inproj_fm(wK, 224, 96, n, b_ks)
                if DBG_STOP <= 2.1:
                    return
                if 'A' not in DBG_SKIP:
                    cp(DVE, ckv.ap, b_ckv.f32, [b_ckv.res], [ckv.res])
                if 'B' not in DBG_SKIP:
                    act(sq.ap[:, 0, :], b_ckv.f32, AF.Square, [b_ckv.res], [sq.res])
                t1, t2 = t12[0], t12[1]
                if 'C' not in DBG_SKIP:
                    tt(DVE, t1.ap[64:96, :], b_kr.f32[64:96, :], tkc.ap[64:96, :], ALU.mult, [b_kr.res, tkc.res], [t1.res])
                if DBG_STOP <= 2.11:
                    return
                tt(DVE, t2.ap[64:96, :], b_ks.f32[64:96, :], tks.ap[64:96, :], ALU.mult, [b_ks.res, tks.res], [t2.res])
                if DBG_STOP <= 2.12:
                    return
                for h in range(4):
                    if DBG_STOP <= 2.13 and h % 2:
                        continue
                    tt(POOL if h % 2 else DVE, L["KT"].ap[64:96, h, blk], t1.ap[64:96, :], t2.ap[64:96, :], ALU.add,
                       [t1.res, t2.res], [L["KT_res"][h][n]])
                if DBG_STOP <= 2.2:
                    return
                b_ss = bW.next()
                mm(b_ss.f32, ones.ap[:], sq.ap[:, 0, :], True, True, [ones.res, sq.res], [b_ss.res])
                rsqrt_act(rkv.ap, b_ss.f32, 128.0, [b_ss.res], [rkv.res])
                if DBG_STOP <= 2.3:
                    return
                for h in range(4):
                    bk = bW.next()
                    mm(bk.f32[0:64, :], wuk.ap[:, h, :], ckv.ap, True, True, [wuk.res, ckv.res], [bk.res])
                    tt(DVE, L["KT"].ap[0:64, h, blk], bk.f32[0:64, :], rkv.ap[0:64, :], ALU.mult, [bk.res, rkv.res],
                       [L["KT_res"][h][n]])
                if DBG_STOP <= 2.4:
                    return
                b_st = bW.next()
                for tl in range(4):
                    mm(b_st.f32[:, tl:tl + 1], sq.ap[:, 0, tl * 128:(tl + 1) * 128], ones.ap[:, 0:1], True, True,
                       [sq.res, ones.res], [b_st.res])
                if DBG_STOP <= 2.5:
                    return
                rsqrt_act(rtm.ap[:, 0:4], b_st.f32[:, 0:4], 128.0, [b_st.res], [rtm.res])
                for tl in range(4):
                    t = 4 * n + tl
                    bk = bW.next()
                    mm(bk.f32[:, 0:256], ckv.ap[:, tl * 128:(tl + 1) * 128], wuv.ap, True, True, [ckv.res, wuv.res], [bk.res])
                    ts(DVE, L["V"].ap[:, t, :, 0:64], bk.f32[:, 0:256].rearrange("p (h d) -> p h d", h=4), rtm.ap[:, tl:tl + 1], None, ALU.mult, None,
                       [bk.res, rtm.res], [L["V_res"][t]])
            if DBG_STOP <= 3:
                return
            pend, finq, cnt, hc = [], [], [0], [0]
            for n in range(NB):
                drain(pend, finq)
                if DBG_STOP <= 4 and n >= 1:
                    break
                qi = n % 2
                blk = slice(n * 512, (n + 1) * 512)
                tqc, tqs = tq[0], tq[1]
                dma(SP, tqc.ap, dr["rq_c"][:, blk], ph([]), [tqc.res], tqc.res)
                dma(SP, tqs.ap, dr["rq_s"][:, blk], ph([]), [tqs.res], tqs.res)
                b0, b1 = bW.next(), bW.next()
                inproj_fm(wQ, 0, 128, n, b0)
                inproj_fm(wQ, 128, 64, n, b1)
                cp(DVE, cq.ap[:, 0, :], b0.f32, [b0.res], [cq.res])
                cp(DVE, cq.ap[0:64, 1, :], b1.f32[0:64, :], [b1.res], [cq.res])
                act(sq.ap[:, 0, :], b0.f32, AF.Square, [b0.res], [sq.res])
                act(sq.ap[0:64, 1, :], b1.f32[0:64, :], AF.Square, [b1.res], [sq.res])
                b_ss = bW.next()
                mm(b_ss.f32, ones.ap[:], sq.ap[:, 0, :], True, False, [ones.res, sq.res], [b_ss.res])
                mm(b_ss.f32, ones.ap[0:64, :], sq.ap[0:64, 1, :], False, True, [ones.res, sq.res], [b_ss.res])
                rsqrt_act(rq.ap, b_ss.f32, 192.0, [b_ss.res], [rq.res])
                tt(DVE, csq[0].ap, tqc.ap, rq.ap[0:96, :], ALU.mult, [tqc.res, rq.res], [csq[0].res])
                tt(DVE, csq[1].ap, tqs.ap, rq.ap[0:96, :], ALU.mult, [tqs.res, rq.res], [csq[1].res])
                for h in range(4):
                    gate_for_head(L, wQ, 192 + 64 * h, n, h, qi)
                for h in range(4):
                    bA, bB = bW.next(), bW.next()
                    mm(bA.f32[0:96, :], wuq.ap[:, 0, h, :], cq.ap[:, 0, :], True, False, [wuq.res, cq.res], [bA.res])
                    mm(bA.f32[0:96, :], wuq.ap[0:64, 1, h, :], cq.ap[0:64, 1, :], False, True, [wuq.res, cq.res], [bA.res])
                    mm(bB.f32[0:96, :], wsw.ap[:, 0, h, :], cq.ap[:, 0, :], True, False, [wsw.res, cq.res], [bB.res])
                    mm(bB.f32[0:96, :], wsw.ap[0:64, 1, h, :], cq.ap[0:64, 1, :], False, True, [wsw.res, cq.res], [bB.res])
                    t1, t2 = t12[0], t12[1]
                    tt(DVE, t1.ap, bA.f32[0:96, :], csq[0].ap, ALU.mult, [bA.res, csq[0].res], [t1.res])
                    tt(DVE, t2.ap, bB.f32[0:96, :], csq[1].ap, ALU.mult, [bB.res, csq[1].res], [t2.res])
                    tt(POOL, L["QT"][qi].ap[0:96, h, :], t1.ap, t2.ap, ALU.add, [t1.res, t2.res], [L["QT_res"][qi][h]])
                if DBG_STOP <= 4:
                    continue
                for h in range(4):
                    ob = attn_block(L, n, h, qi, 96, "mla", cnt, pend, finq)
                    finq.append(lambda ob=ob, n=n, h=h, qi=qi, c=hc[0]: finish_head(L, ob, n, h, qi, 0 + 64 * h, c))
                    hc[0] += 1
            drain(pend, finq)

        def kv_pass(L, l, colK, colV, kmean=None):
            load_w(wK, dr["w_in"][l][:, colK:colK + 256], 256)
            load_w(wK, dr["w_in"][l][:, colV:colV + 256], 256, col0=256)
            for n in range(NB):
                blk = slice(n * 512, (n + 1) * 512)
                for c in range(2):
                    bk = bW.next()
                    inproj_fm(wK, 128 * c, 128, n, bk)
                    cp(DVE, L["KT"].ap[0:64, 2 * c, blk], bk.f32[0:64, :], [bk.res], [L["KT_res"][2 * c][n]])
                    cp(POOL if False else DVE, L["KT"].ap[0:64, 2 * c + 1, blk], bk.f32[64:128, :], [bk.res], [L["KT_res"][2 * c + 1][n]])
                    if kmean is not None:
                        P.op(DVE, lambda e, o=kmean.ap[:, c, 2 * n:2 * n + 2], i=bk.f32.rearrange("p (b t) -> p b t", b=2):
                             e.tensor_reduce(o, i, AX.X, ALU.add), ph([bk.res]), [kmean.res])
                for tl in range(4):
                    t = 4 * n + tl
                    bk = bW.next()
                    inproj_tm(wK, 256, 256, t, bk)
                    cp(DVE, L["V"].ap[:, t, :, 0:64], bk.f32[:, 0:256].rearrange("p (h d) -> p h d", h=4), [bk.res], [L["V_res"][t]])

        def q_pass(L, n, qi, scale, qf=None):
            for c in range(2):
                bk = bW.next()
                inproj_fm(wQ, 128 * c, 128, n, bk)
                for hh in range(2):
                    h = 2 * c + hh
                    ts(DVE, L["QT"][qi].ap[0:64, h, :], bk.f32[64 * hh:64 * hh + 64, :], scale, None, ALU.mult, None,
                       [bk.res], [L["QT_res"][qi][h]])
                    if qf is not None:
                        cp(DVE, qf.ap[:, h, :], bk.f32[64 * hh:64 * hh + 64, :], [bk.res], [qf.res])

        def mixer_sb(l):
            barrier()
            L = layout_attn()
            a = L["alloc"]
            E = [a.get([128, 512], F32) for _ in range(2)]
            LN = [a.get([128, 512], F32) for _ in range(2)]
            LC = [a.get([128, 512], F32) for _ in range(2)]
            TS = [a.get([128, 512], F32) for _ in range(2)]
            c0w = 352
            kv_pass(L, l, c0w + 256, c0w + 512)
            load_w(wQ, dr["w_in"][l][:, c0w:c0w + 256], 256)
            load_w(wQ, dr["w_in"][l][:, 2656:2912], 256, col0=256)
            cnt, hc = [0], [0]
            for n in range(NB):
                qi = n % 2
                q_pass(L, n, qi, 0.125)
                for h in range(4):
                    gate_for_head(L, wQ, 256 + 64 * h, n, h, qi)
                for h in range(4):
                    ob = bO.next()
                    lc = LC[hc[0] % 2]
                    P.op(POOL, lambda e, o=lc.ap: e.memset(o, 0.0), ph([]), [lc.res])
                    tiles = [(4 * n + r, r) for r in (3, 2, 1, 0)] + \
                            [(j, -1) for j in range(4 * n - 1, max(4 * n - 1 - SB_WINDOW_TILES, -1), -1)]
                    qt = L["QT"][qi].ap[0:64, h, :]
                    mm(ob.f32[0:64, :], L["V"].ap[:, 4 * n + 3, h, 0:64], zeros_bf.ap[:], True, False,
                       [zeros_bf.res, L["V_res"][4 * n + 3]], [ob.res], skip_group_check=True)
                    for idx, (j, r) in enumerate(tiles):
                        c0 = 128 * r if r > 0 else 0
                        cs = slice(c0, 512)
                        k = cnt[0]
                        cnt[0] += 1
                        e_, ln_, ts_, pt = E[k % 2], LN[k % 2], TS[k % 2], L["PT"][k % 4]
                        zb = bS.next()
                        mm(zb.f32[:, cs], L["KT"].ap[0:64, h, j * 128:(j + 1) * 128], qt[:, cs], True, True,
                           [L["KT_res"][h][j // 4], L["QT_res"][qi][h]], [zb.res])
                        act(e_.ap[:, cs], zb.f32[:, cs], AF.Exp, [zb.res], [e_.res], scale=-1.0)
                        act(e_.ap[:, cs], e_.ap[:, cs], AF.Ln, [e_.res], [e_.res], bias=1.0)
                        tt(DVE, ln_.ap[:, cs], e_.ap[:, cs], zb.f32[:, cs], ALU.add, [e_.res, zb.res], [ln_.res])
                        if r >= 0:
                            tt(POOL, ln_.ap[:, c0:c0 + 128], ln_.ap[:, c0:c0 + 128], tri_lt.ap[:], ALU.mult,
                               [ln_.res, tri_lt.res], [ln_.res])
                        sb_ = bW.next()
                        mm(sb_.f32[:, cs], tri_gt.ap[:], ln_.ap[:, cs], True, idx == 0, [tri_gt.res, ln_.res], [sb_.res])
                        if idx > 0:
                            mm(sb_.f32[:, cs], ones.ap[:], lc.ap[:, cs], False, True, [ones.res, lc.res], [sb_.res])
                        tt(DVE, ts_.ap[:, cs], e_.ap[:, cs], sb_.f32[:, cs], ALU.add, [e_.res, sb_.res], [ts_.res])
                        act(pt.ap[:, cs], ts_.ap[:, cs], AF.Exp, [ts_.res], [pt.res], scale=-1.0)
                        if r >= 0:
                            tt(POOL, pt.ap[:, c0:c0 + 128], pt.ap[:, c0:c0 + 128], tri_lt.ap[:], ALU.mult,
                               [pt.res, tri_lt.res], [pt.res])
                        if idx < len(tiles) - 1:
                            tt(POOL, lc.ap[:, cs], lc.ap[:, cs], ln_.ap[:, cs], ALU.add, [lc.res, ln_.res], [lc.res])
                        mm(ob.f32[0:64, cs], L["V"].ap[:, j, h, 0:64], pt.ap[:, cs], False, idx == len(tiles) - 1,
                           [pt.res, L["V_res"][j]], [ob.res], skip_group_check=True)
                    ys = L["YST"][hc[0] % 2]
                    tt(DVE, ys.ap, ob.f32[0:64, :], L["SG"][qi].ap[:, h, :], ALU.mult, [ob.res, L["SG_res"][qi][h]], [ys.res])
                    row0 = 256 + 64 * h
                    dma(SP, yT_h[row0:row0 + 64, n * 512:(n + 1) * 512], ys.ap, ph([ys.res]), [r_yT[row0 // 128][n]], ys.res)
                    hc[0] += 1

        def mixer_moba(l):
            barrier()
            L = layout_attn()
            a = L["alloc"]
            L["GT"] = a.get([128, 4, GT_W], BF16)
            L["STG"] = [a.get([128, 512], F32) for _ in range(2)]
            kmean = a.get([128, 2, 16], F32)
            kmh = a.get([64, 4, 16], F32)
            qf = a.get([64, 4, 512], F32)
            g16 = a.get([128, 4, 16], F32)
            top8 = a.get([128, 4, 8], F32)
            mb = a.get([128, 4, 32], BF16)
            init_V_ones(L)
            dma(POOL, L["GT"].ap, dr["gt"], ph([]), [L["GT"].res], L["GT"].res)
            P.op(POOL, lambda e, o=mb.ap: e.memset(o, 0.0), ph([]), [mb.res])
            for h in range(4):
                P.op(POOL, lambda e, o=L["KT"].ap[64:80, h, :], i=dr["onehot"]: e.dma_start(out=o, in_=i),
                     ph([]), [L["KT_res"][h][nn] for nn in range(NB)], dma_slot=L["KT_res"][h][0])
            c0w = 352 + 768
            kv_pass(L, l, c0w + 256, c0w + 512, kmean=kmean)
            for h in range(4):
                cp(DVE, kmh.ap[:, h, :], kmean.ap[64 * (h % 2):64 * (h % 2) + 64, h // 2, :], [kmean.res], [kmh.res])
            load_w(wQ, dr["w_in"][l][:, c0w:c0w + 256], 256)
            load_w(wQ, dr["w_in"][l][:, 2912:3168], 256, col0=256)
            pend, finq, cnt, hc = [], [], [0], [0]
            for n in range(NB):
                drain(pend, finq)
                qi = n % 2
                q_pass(L, n, qi, 0.125, qf=qf)
                for h in range(4):
                    gate_for_head(L, wQ, 256 + 64 * h, n, h, qi)
                for tl in range(4):
                    T = 4 * n + tl
                    ob_ = T // 2
                    bg = bW.next()
                    for h in range(4):
                        mm(bg.f32[:, 16 * h:16 * h + 16], qf.ap[:, h, tl * 128:(tl + 1) * 128], kmh.ap[:, h, :], True, True,
                           [qf.res, kmh.res], [bg.res])
                    tt(DVE, g16.ap, bg.f32[:, 0:64].rearrange("p (h b) -> p h b", h=4),
                       addm.ap[:, ob_, :].unsqueeze(1).to_broadcast([128, 4, 16]), ALU.add, [bg.res, addm.res], [g16.res])
                    for h in range(4):
                        P.op(DVE, lambda e, o=top8.ap[:, h, :], i=g16.ap[:, h, :]: e.max(o, i), ph([g16.res]), [top8.res])
                    for h in range(4):
                        ts(DVE, mb.ap[:, h, 0:16], g16.ap[:, h, :], top8.ap[:, h, 3:4], -30000.0, ALU.is_lt, ALU.mult,
                           [g16.res, top8.res], [mb.res])
                    bt = bW.next()
                    P.op(PE, lambda e, o=bt.bf[:, 0:128], i_=mb.ap.rearrange("p h b -> p (h b)"): e.transpose(o, i_, ident.ap[:]),
                         ph([mb.res, ident.res]), [bt.res])
                    for h in range(4):
                        cp(DVE, L["QT"][qi].ap[64:80, h, tl * 128:(tl + 1) * 128], bt.bf[32 * h:32 * h + 16, 0:128],
                           [bt.res], [L["QT_res"][qi][h]])
                for h in range(4):
                    ob = attn_block(L, n, h, qi, 80, "moba", cnt, pend, finq)
                    finq.append(lambda ob=ob, n=n, h=h, qi=qi, c=hc[0]: finish_head(L, ob, n, h, qi, 512 + 64 * h, c))
                    hc[0] += 1
            drain(pend, finq)

        def mixer_conv(l):
            barrier()
            a = Alloc()
            PADW = 32
            xg = a.get([128, 2, PADW + S], BF16)
            r_xg = [[Res() for _ in range(NB)] for _ in range(2)]
            dg = a.get([128, 2, 31, 128], BF16)
            dwT = a.get([128, 2, 31], F32)
            vb = {k: a.get([128, 2], F32) for k in ("dw_b", "ln_g", "ln_b", "pw_b")}
            pw = a.get([128, 2, 256], BF16)
            sig = [a.get([128, 512], F32) for _ in range(2)]
            yc = a.get([128, 2, 512], F32)
            ysq = a.get([128, 2, 512], F32)
            mean = a.get([128, 512], F32)
            rstd = a.get([128, 512], F32)
            msq = a.get([128, 512], F32)
            sw = a.get([128, 2, 512], BF16)
            ee = a.get([128, 512], F32)
            sg = [a.get([128, 512], F32) for _ in range(2)]
            yo = [a.get([128, 512], BF16) for _ in range(2)]
            identf = a.get([128, 128], F32)
            c0w = 352 + 1536
            load_w(wK, dr["w_in"][l][:, c0w:c0w + 512], 512)
            load_w(wQ, dr["w_in"][l][:, 3168:3424], 256)
            dma(SP, dwT.ap, dr["dw_wT"][l], ph([]), [dwT.res], dwT.res)
            for k_, b_ in vb.items():
                dma(SP, b_.ap, dr[k_][l], ph([]), [b_.res], b_.res)
            dma(SP, identf.ap, dr["ident"], ph([]), [identf.res], identf.res)
            P.op(POOL, lambda e, o=pw.ap, i=dr["pw_w"][l].rearrange("(c p) n -> p c n", p=128): e.dma_start(out=o, in_=i),
                 ph([]), [pw.res], dma_slot=pw.res)
            for c in range(2):
                for j in range(31):
                    ts(POOL if j % 2 else DVE, dg.ap[:, c, j, :], identf.ap, dwT.ap[:, c, j:j + 1], None, ALU.mult, None,
                       [identf.res, dwT.res], [dg.res])
                P.op(POOL, lambda e, o=xg.ap[:, c, 0:PADW]: e.memset(o, 0.0), ph([]), [r_xg[c][0]])
            for n in range(NB):
                for c in range(2):
                    ba, bg_ = bW.next(), bW.next()
                    inproj_fm(wK, 128 * c, 128, n, ba)
                    inproj_fm(wK, 256 + 128 * c, 128, n, bg_)
                    s_ = sig[c]
                    act(s_.ap, bg_.f32, AF.Exp, [bg_.res], [s_.res], scale=-1.0)
                    ts(POOL, s_.ap, s_.ap, 1.0, None, ALU.add, None, [s_.res], [s_.res])
                    P.op(DVE, lambda e, o=s_.ap, i=s_.ap: e.reciprocal(o, i), ph([s_.res]), [s_.res])
                    tt(DVE, xg.ap[:, c, PADW + n * 512:PADW + (n + 1) * 512], ba.f32, s_.ap, ALU.mult, [ba.res, s_.res], [r_xg[c][n]])
            for n in range(NB):
                for c in range(2):
                    bc = bW.next()
                    rd = [dg.res, r_xg[c][n]] + ([r_xg[c][n - 1]] if n > 0 else [])
                    for j in range(31):
                        st0 = PADW + n * 512 - 30 + j
                        mm(bc.f32, dg.ap[:, c, j, :], xg.ap[:, c, st0:st0 + 512], j == 0, j == 30, rd, [bc.res])
                    ts(DVE, yc.ap[:, c, :], bc.f32, vb["dw_b"].ap[:, c:c + 1], None, ALU.add, None, [bc.res, vb["dw_b"].res], [yc.res])
                    act(ysq.ap[:, c, :], yc.ap[:, c, :], AF.Square, [yc.res], [ysq.res])
                bm, bq = bW.next(), bW.next()
                mm(bm.f32, ones.ap[:], yc.ap[:, 0, :], True, False, [ones.res, yc.res], [bm.res])
                mm(bm.f32, ones.ap[:], yc.ap[:, 1, :], False, True, [ones.res, yc.res], [bm.res])
                mm(bq.f32, ones.ap[:], ysq.ap[:, 0, :], True, False, [ones.res, ysq.res], [bq.res])
                mm(bq.f32, ones.ap[:], ysq.ap[:, 1, :], False, True, [ones.res, ysq.res], [bq.res])
                ts(DVE, mean.ap, bm.f32, 1.0 / 256, None, ALU.mult, None, [bm.res], [mean.res])
                tt(DVE, msq.ap, mean.ap, mean.ap, ALU.mult, [mean.res], [msq.res])
                P.op(DVE, lambda e, o=msq.ap, a_=bq.f32, b_=msq.ap: e.scalar_tensor_tensor(o, a_, 1.0 / 256, b_, ALU.mult, ALU.subtract),
                     ph([bq.res, msq.res]), [msq.res])
                act(rstd.ap, msq.ap, AF.Ln, [msq.res], [rstd.res], bias=EPS)
                act(rstd.ap, rstd.ap, AF.Exp, [rstd.res], [rstd.res], scale=-0.5)
                for c in range(2):
                    tt(DVE, yc.ap[:, c, :], yc.ap[:, c, :], mean.ap, ALU.subtract, [yc.res, mean.res], [yc.res])
                    tt(DVE, yc.ap[:, c, :], yc.ap[:, c, :], rstd.ap, ALU.mult, [yc.res, rstd.res], [yc.res])
                    ts(POOL, yc.ap[:, c, :], yc.ap[:, c, :], vb["ln_g"].ap[:, c:c + 1], vb["ln_b"].ap[:, c:c + 1], ALU.mult, ALU.add,
                       [yc.res, vb["ln_g"].res, vb["ln_b"].res], [yc.res])
                    act(ee.ap, yc.ap[:, c, :], AF.Exp, [yc.res], [ee.res], scale=-1.0)
                    ts(POOL, ee.ap, ee.ap, 1.0, None, ALU.add, None, [ee.res], [ee.res])
                    P.op(DVE, lambda e, o=ee.ap, i=ee.ap: e.reciprocal(o, i), ph([ee.res]), [ee.res])
                    tt(DVE, sw.ap[:, c, :], yc.ap[:, c, :], ee.ap, ALU.mult, [yc.res, ee.res], [sw.res])
                for c in range(2):
                    bgt = bW.next()
                    inproj_fm(wQ, 128 * c, 128, n, bgt)
                    g_ = sg[c]
                    act(g_.ap, bgt.f32, AF.Exp, [bgt.res], [g_.res], scale=-1.0)
                    ts(POOL, g_.ap, g_.ap, 1.0, None, ALU.add, None, [g_.res], [g_.res])
                    P.op(DVE, lambda e, o=g_.ap, i=g_.ap: e.reciprocal(o, i), ph([g_.res]), [g_.res])
                    tt(DVE, g_.ap, bgt.f32, g_.ap, ALU.mult, [bgt.res, g_.res], [g_.res])
                    bp = bW.next()
                    mm(bp.f32, pw.ap[:, 0, 128 * c:128 * c + 128], sw.ap[:, 0, :], True, False, [pw.res, sw.res], [bp.res])
                    mm(bp.f32, pw.ap[:, 1, 128 * c:128 * c + 128], sw.ap[:, 1, :], False, True, [pw.res, sw.res], [bp.res])
                    y_ = yo[c]
                    P.op(DVE, lambda e, o=y_.ap, a_=bp.f32, s1=vb["pw_b"].ap[:, c:c + 1], b_=g_.ap:
                         e.scalar_tensor_tensor(o, a_, s1, b_, ALU.add, ALU.mult), ph([bp.res, vb["pw_b"].res, g_.res]), [y_.res])
                    row0 = 768 + 128 * c
                    dma(SP, yT_h[row0:row0 + 128, n * 512:(n + 1) * 512], y_.ap, ph([y_.res]), [r_yT[row0 // 128][n]], y_.res)

        MIX = {"mla": mixer_mla, "sb": mixer_sb, "moba": mixer_moba, "conv": mixer_conv}
        phase_A0()
        fin = []
        for l in range(n_layers):
            for m in MIX_ALL:
                if m in mixers:
                    MIX[m](l)
            fin = phase_C(l, last=(l == n_layers - 1))
        barrier()
        last_bar = P.ops[DVE][-1]
        P.emit(final_waits=[(POOL, d) for d in fin[-2:]] + [(SP, last_bar)])
    return nc


_CACHE = {}


def kernel(**inputs):
    inp = {k: np.asarray(v, dtype=np.float32) for k, v in inputs.items()}
    w = host_layout(inp)
    if "nc" not in _CACHE:
        _CACHE["nc"] = build()
    nc = _CACHE["nc"]
    x = inp["x"]
    in_maps = []
    for c in range(8):
        m = {"x": np.ascontiguousarray(x[c])}
        m.update(w)
        in_maps.append(m)
    res = run_bass_kernel_spmd(nc, in_maps, core_ids=list(range(8)))
    return np.stack([np.asarray(r["out"], dtype=np.float32) for r in res.results], axis=0)
```

```python
import contextlib
import math
import numpy as np
import concourse.bass as bass
import concourse.mybir as mybir
from concourse.bass_utils import run_bass_kernel_spmd

F32 = mybir.dt.float32
BF16 = mybir.dt.bfloat16
ALU = mybir.AluOpType
AF = mybir.ActivationFunctionType
AX = mybir.AxisListType

S = 4096
D = 1024
NL = 4
NT = 32
NB = 8
EPS = 1e-6
DBG_STOP = 99
DBG_SKIP = ''
SB_WINDOW_TILES = 3

PE, ACT, DVE, POOL, SP = "pe", "act", "dve", "pool", "sp"
COMPUTE = (PE, ACT, DVE, POOL)


class Res:
    __slots__ = ("name", "writer", "readers", "slot", "excl", "persist")

    def __init__(self, name="", excl=False, persist=False):
        self.name = name
        self.excl = excl
        self.persist = persist
        self.writer = None
        self.readers = []
        self.slot = None


class SemSlot:
    __slots__ = ("sem", "count")

    def __init__(self):
        self.sem = None
        self.count = 0


class Op:
    __slots__ = ("eng", "idx", "fn", "waits", "signal", "count", "is_dma", "dma_res",
                 "dma_count", "n_dma")

    def __init__(self, eng, idx, fn):
        self.eng = eng
        self.idx = idx
        self.fn = fn
        self.waits = []
        self.signal = False
        self.count = 0
        self.is_dma = False
        self.dma_res = None
        self.dma_count = 0
        self.n_dma = 0


class Prog:
    def __init__(self, nc):
        self.nc = nc
        self.ops = {e: [] for e in (PE, ACT, DVE, POOL, SP)}
        self.seen = {e: {} for e in self.ops}
        self.slots = []
        self.free_slots = []
        self.phase_res = []

    def release_phase_slots(self):
        for r in self.phase_res:
            if r.slot is not None:
                self.free_slots.append(r.slot)
                r.slot = None
        self.phase_res = []

    def op(self, eng, fn, reads=(), writes=(), dma_slot=None, n_dma=1):
        lst = self.ops[eng]
        o = Op(eng, len(lst), fn)
        ex = [r for r in reads if r.excl and r not in writes]
        if ex:
            writes = list(writes) + ex
        if dma_slot is not None:
            if dma_slot.slot is None:
                if self.free_slots:
                    dma_slot.slot = self.free_slots.pop()
                else:
                    dma_slot.slot = SemSlot()
                    self.slots.append(dma_slot.slot)
                if not dma_slot.persist:
                    self.phase_res.append(dma_slot)
            o.is_dma = True
            o.dma_res = dma_slot.slot
            o.n_dma = n_dma
            dma_slot.slot.count += 16 * n_dma
            o.dma_count = dma_slot.slot.count
        need = {}

        def same(d):
            return (not d.is_dma) and (not o.is_dma) and d.eng == eng

        def add(d):
            if d is None or d is o:
                return
            if d.is_dma:
                key, val = ("dma", id(d.dma_res)), d.dma_count
            else:
                key, val = ("eng", d.eng), d.idx
            if key not in need or val > need[key][0]:
                need[key] = (val, d)

        for r in reads:
            d = r.writer
            if d is not None and not (same(d) and eng == PE):
                add(d)
        for w in writes:
            d = w.writer
            if d is not None and not same(d):
                add(d)
            for rd in w.readers:
                if not same(rd):
                    add(rd)
        seen = self.seen[eng]
        for key, (val, d) in need.items():
            if seen.get(key, -1) >= val:
                continue
            seen[key] = val
            o.waits.append(d)
            if not d.is_dma:
                d.signal = True
        for r in reads:
            r.readers.append(o)
        for w in writes:
            w.writer = o
            w.readers = []
        lst.append(o)
        return o

    def emit(self, final_waits=()):
        nc = self.nc
        final = {}
        for (e, d) in final_waits:
            final.setdefault(e, []).append(d)
            if not d.is_dma:
                d.signal = True
        for e in COMPUTE:
            c = 0
            for o in self.ops[e]:
                if o.signal:
                    c += 1
                o.count = c
        with contextlib.ExitStack() as st:
            esem = {e: st.enter_context(nc.semaphore("prog_" + e)) for e in COMPUTE}
            for i, r in enumerate(self.slots):
                r.sem = st.enter_context(nc.semaphore("dma_%d" % i))
            block = st.enter_context(nc.Block())

            def wait(eng, d):
                if d.is_dma:
                    eng.wait_ge(d.dma_res.sem, d.dma_count)
                else:
                    eng.wait_ge(esem[d.eng], d.count)

            def run(e):
                def body(eng):
                    for o in self.ops[e]:
                        for d in o.waits:
                            wait(eng, d)
                        ins = o.fn(eng)
                        if o.is_dma:
                            if not isinstance(ins, (list, tuple)):
                                ins = [ins]
                            assert len(ins) == o.n_dma, (len(ins), o.n_dma)
                            for i_ in ins:
                                i_.then_inc(o.dma_res.sem, 16)
                        elif o.signal:
                            ins.then_inc(esem[e], 1)
                    for d in final.get(e, []):
                        wait(eng, d)
                return body

            block.tensor(run(PE))
            block.scalar(run(ACT))
            block.vector(run(DVE))
            block.gpsimd(run(POOL))
            block.sync(run(SP))


def _t5_bucket(n):
    n = np.maximum(n, 0)
    max_exact = 16
    n_large = np.maximum(n, max_exact).astype(np.float32)
    large = max_exact + (np.log(n_large / max_exact) / math.log(1024 / max_exact) * (32 - max_exact)).astype(np.int32)
    large = np.minimum(large, 31)
    return np.where(n < max_exact, n, large)


GT_W = 1664


def host_consts():
    c = {}
    c["ident"] = np.eye(128, dtype=np.float32)
    p = np.arange(128)[:, None]
    q = np.arange(128)[None, :]
    c["tri_le"] = (p <= q).astype(np.float32)
    c["tri_lt"] = (p < q).astype(np.float32)
    c["tri_gt"] = (p > q).astype(np.float32)
    c["ones"] = np.ones((128, 128), np.float32)
    half = 16
    freqs = (10000.0 ** (-np.arange(half, dtype=np.float32) / half)).astype(np.float32)
    ang = np.arange(S, dtype=np.float32)[:, None] * freqs[None, :]
    cos = np.cos(ang).astype(np.float32).T
    sin = np.sin(ang).astype(np.float32).T
    sc = np.float32(96 ** -0.5)
    rq_c = np.zeros((96, S), np.float32)
    rq_s = np.zeros((96, S), np.float32)
    rq_c[0:64] = sc
    rq_c[64:80] = cos * sc
    rq_c[80:96] = cos * sc
    rq_s[64:80] = -sin * sc
    rq_s[80:96] = sin * sc
    rk_c = np.zeros((96, S), np.float32)
    rk_s = np.zeros((96, S), np.float32)
    rk_c[64:80] = cos
    rk_c[80:96] = cos
    rk_s[64:80] = -sin
    rk_s[80:96] = sin
    c["rq_c"], c["rq_s"], c["rk_c"], c["rk_s"] = rq_c, rq_s, rk_c, rk_s
    oh = np.zeros((16, S), np.float32)
    for b in range(16):
        oh[b, b * 256:(b + 1) * 256] = 1.0
    c["onehot"] = oh
    am = np.zeros((16, 16), np.float32)
    for ob in range(16):
        am[ob, ob] = 1e30
        am[ob, ob + 1:] = -1e30
    c["addm"] = np.ascontiguousarray(np.broadcast_to(am[None], (128, 16, 16)))
    return c


def host_layout(inp):
    w = {}
    w_in = inp["w_in"]
    w["w_in"] = np.ascontiguousarray(w_in)
    wk = np.zeros((NL, D, 320), np.float32)
    wk[:, :, 0:128] = w_in[:, :, 192:320]
    wk[:, :, 192:224] = w_in[:, :, 320:352]
    wk[:, :, 288:304] = w_in[:, :, 336:352]
    wk[:, :, 304:320] = w_in[:, :, 320:336]
    w["w_mla_k"] = wk
    w["gpre"] = np.ascontiguousarray(inp["pre_norm_g"].reshape(NL, 8, 128).transpose(0, 2, 1))
    gq = np.zeros((NL, 128, 2), np.float32)
    gq[:, :, 0] = inp["mla_q_norm_g"][:, 0:128]
    gq[:, 0:64, 1] = inp["mla_q_norm_g"][:, 128:192]
    w["gq"] = gq
    uq = inp["mla_w_uq"].reshape(NL, 192, 4, 96)
    uq_p = np.zeros((NL, 128, 2, 4, 96), np.float32)
    uq_p[:, :, 0] = uq[:, 0:128]
    uq_p[:, 0:64, 1] = uq[:, 128:192]
    w["w_uq"] = uq_p
    sw = np.zeros((NL, 192, 4, 96), np.float32)
    sw[:, :, :, 64:80] = uq[:, :, :, 80:96]
    sw[:, :, :, 80:96] = uq[:, :, :, 64:80]
    sw_p = np.zeros((NL, 128, 2, 4, 96), np.float32)
    sw_p[:, :, 0] = sw[:, 0:128]
    sw_p[:, 0:64, 1] = sw[:, 128:192]
    w["w_uq_sw"] = sw_p
    w["gkv"] = np.ascontiguousarray(inp["mla_kv_norm_g"].reshape(NL, 128, 1))
    ukv = inp["mla_w_ukv"].reshape(NL, 128, 4, 128)
    w["w_uk"] = np.ascontiguousarray(ukv[:, :, :, 0:64])
    w["w_uv"] = np.ascontiguousarray(ukv[:, :, :, 64:128].reshape(NL, 128, 256))
    rb = inp["rel_bias"]
    pp = np.arange(128)[:, None]
    xx = np.arange(GT_W)[None, :]
    bucket = _t5_bucket(xx - pp)
    w["gt"] = np.ascontiguousarray(rb[bucket].transpose(0, 2, 1))
    w["c31"] = np.ascontiguousarray(np.broadcast_to(rb[31][None, :], (128, 4)))
    w["dw_wT"] = np.ascontiguousarray(inp["conv_dw_w"].transpose(0, 2, 1).reshape(NL, 2, 128, 31).transpose(0, 2, 1, 3))

    def pv(a):
        return np.ascontiguousarray(a.reshape(NL, 2, 128).transpose(0, 2, 1))
    w["dw_b"] = pv(inp["conv_dw_b"])
    w["ln_g"] = pv(inp["conv_ln_g"])
    w["ln_b"] = pv(inp["conv_ln_b"])
    w["pw_b"] = pv(inp["conv_pw_b"])
    w["pw_w"] = np.ascontiguousarray(inp["conv_pw_w"])
    w["w_out"] = np.ascontiguousarray(inp["w_out"])
    w["post_g"] = np.ascontiguousarray(inp["post_norm_g"])
    w.update(host_consts())
    return w


WSHAPES = {
    "w_in": [NL, D, 3424], "w_mla_k": [NL, D, 320], "gpre": [NL, 128, 8], "gq": [NL, 128, 2],
    "w_uq": [NL, 128, 2, 4, 96], "w_uq_sw": [NL, 128, 2, 4, 96], "gkv": [NL, 128, 1],
    "w_uk": [NL, 128, 4, 64], "w_uv": [NL, 128, 256], "gt": [128, 4, GT_W], "c31": [128, 4],
    "dw_wT": [NL, 128, 2, 31], "dw_b": [NL, 128, 2], "ln_g": [NL, 128, 2], "ln_b": [NL, 128, 2],
    "pw_b": [NL, 128, 2], "pw_w": [NL, 256, 256], "w_out": [NL, D, D], "post_g": [NL, D],
    "ident": [128, 128], "tri_le": [128, 128], "tri_lt": [128, 128], "tri_gt": [128, 128],
    "ones": [128, 128], "rq_c": [96, S], "rq_s": [96, S], "rk_c": [96, S], "rk_s": [96, S],
    "onehot": [16, S], "addm": [128, 16, 16],
}


class Bank:
    def __init__(self, t):
        self.t = t
        self.res = Res(excl=True)
        self.f32 = t[:]
        self.bf = t[:].bitcast(BF16)


class RR:
    def __init__(self, items):
        self.items = items
        self.i = 0

    def next(self):
        it = self.items[self.i % len(self.items)]
        self.i += 1
        return it


class Buf:
    def __init__(self, ap, persist=False):
        self.ap = ap if isinstance(ap, bass.AP) else ap[:]
        self.res = Res(persist=persist)


ARENA_BYTES = 122880
MIX_ALL = ("mla", "sb", "moba", "conv")


def build(n_layers=NL, mixers=MIX_ALL, dbg=False):
    nc = bass.Bass("TRN2", target_bir_lowering=False)
    P = Prog(nc)
    dr = {}
    x_in = nc.dram_tensor("x", [S, D], F32, kind="ExternalInput").ap()
    for k, shp in WSHAPES.items():
        dr[k] = nc.dram_tensor(k, shp, F32, kind="ExternalInput").ap()
    out = nc.dram_tensor("out", [S, D], F32, kind="ExternalOutput").ap()
    yT_h = nc.dram_tensor("yT", [D, S], BF16, kind="ExternalOutput" if dbg else "Internal").ap()
    r_yT = [[Res() for _ in range(NB)] for _ in range(8)]
    r_xh = [Res() for _ in range(NT)]

    st = contextlib.ExitStack()
    with st:
        def sb(name, shape, dt):
            return st.enter_context(nc.sbuf_tensor("s_" + name, shape, dt))

        hT = sb("hT", [128, 8, S], BF16)
        r_hT = [Res() for _ in range(NT)]
        wK = Buf(sb("wK", [128, 8, 512], BF16), persist=True)
        wQ = Buf(sb("wQ", [128, 8, 512], BF16), persist=True)
        ident = Buf(sb("ident", [128, 128], BF16), persist=True)
        tri_le = Buf(sb("tri_le", [128, 128], BF16), persist=True)
        tri_lt = Buf(sb("tri_lt", [128, 128], BF16), persist=True)
        tri_gt = Buf(sb("tri_gt", [128, 128], F32), persist=True)
        ones = Buf(sb("ones", [128, 128], F32), persist=True)
        gpre = Buf(sb("gpre", [128, NL, 8], F32), persist=True)
        c31 = Buf(sb("c31", [128, 4], F32), persist=True)
        zeros_bf = Buf(sb("zeros_bf", [128, 512], BF16), persist=True)
        addm = Buf(sb("addm", [128, 16, 16], F32), persist=True)
        arena_t = sb("arena", [128, ARENA_BYTES // 4], F32)
        r_phase = Res("phase")
        dummy = Buf(sb("dummy", [128, 8], F32), persist=True)

        def view(off, shape, dt):
            isz = 2 if dt == BF16 else 4
            n = 1
            for s_ in shape[1:]:
                n *= s_
            nb = n * isz
            assert off % 4 == 0 and nb % 4 == 0 and off + nb <= ARENA_BYTES, (off, nb)
            ap = arena_t[:, off // 4:(off + nb) // 4]
            if dt == BF16:
                ap = ap.bitcast(BF16)
            if len(shape) == 3:
                ap = ap.rearrange("p (a b) -> p a b", a=shape[1])
            elif len(shape) == 4:
                ap = ap.rearrange("p (a b c) -> p a b c", a=shape[1], b=shape[2])
            if shape[0] != 128:
                ap = ap[0:shape[0]]
            return ap

        class Alloc:
            def __init__(self, start=0):
                self.off = start

            def get(self, shape, dt):
                isz = 2 if dt == BF16 else 4
                n = 1
                for s_ in shape[1:]:
                    n *= s_
                nb = (n * isz + 31) // 32 * 32
                v = view(self.off, shape, dt)
                self.off += nb
                return Buf(v)

        banks = [Bank(st.enter_context(nc.psum_tensor("bank%d" % i, [128, 512], F32))) for i in range(8)]
        bS = RR(banks[0:3])
        bO = RR(banks[3:5])
        bW = RR(banks[5:8])

        def dma(q, out_ap, in_ap, reads, writes, slot):
            return P.op(q, lambda e, o=out_ap, i=in_ap: e.dma_start(out=o, in_=i), reads, writes, dma_slot=slot)

        def barrier():
            P.op(DVE, lambda e: e.memset(dummy.ap, 0.0), reads=[], writes=[r_phase, dummy.res])
            P.release_phase_slots()

        def ph(reads):
            return list(reads) + [r_phase]

        def mm(o_ap, lhsT, rhs, start, stop, reads, writes, **kw):
            return P.op(PE, lambda e, o=o_ap, l=lhsT, r=rhs, s0=start, s1=stop, kw=kw:
                        e.matmul(o, l, r, start=s0, stop=s1, **kw), ph(reads), writes)

        def act(o_ap, i_ap, func, reads, writes, scale=1.0, bias=None, accum=None):
            def f(e, o=o_ap, i=i_ap, fu=func, sc=scale, b=bias, a=accum):
                kw = {}
                if b is not None:
                    kw["bias"] = b
                if a is not None:
                    kw["accum_out"] = a
                return e.activation(o, i, fu, scale=sc, **kw)
            return P.op(ACT, f, ph(reads), writes)

        def tt(eng, o_ap, a, b, op, reads, writes):
            return P.op(eng, lambda e, o=o_ap, a=a, b=b, op=op: e.tensor_tensor(o, a, b, op), ph(reads), writes)

        def ts(eng, o_ap, a, s1, s2, op0, op1, reads, writes):
            if op1 is None:
                return P.op(eng, lambda e, o=o_ap, a=a, s1=s1, op0=op0: e.tensor_scalar(o, a, s1, None, op0), ph(reads), writes)
            return P.op(eng, lambda e, o=o_ap, a=a, s1=s1, s2=s2, op0=op0, op1=op1:
                        e.tensor_scalar(o, a, s1, s2, op0, op1), ph(reads), writes)

        def cp(eng, o_ap, i_ap, reads, writes):
            return P.op(eng, lambda e, o=o_ap, i=i_ap: e.tensor_copy(o, i), ph(reads), writes)

        def rsqrt_act(o_ap, i_ap, n_mean, reads, writes):
            act(o_ap, i_ap, AF.Ln, reads, writes, scale=1.0 / n_mean, bias=EPS)
            act(o_ap, o_ap, AF.Exp, writes, writes, scale=-0.5)

        for (b_, nm) in [(ident, "ident"), (tri_le, "tri_le"), (tri_lt, "tri_lt")]:
            dma(POOL, b_.ap[:], dr[nm], [], [b_.res], b_.res)
        dma(SP, tri_gt.ap[:], dr["tri_gt"], [], [tri_gt.res], tri_gt.res)
        dma(SP, ones.ap[:], dr["ones"], [], [ones.res], ones.res)
        dma(SP, gpre.ap[:], dr["gpre"].rearrange("l p c -> p l c"), [], [gpre.res], gpre.res)
        dma(SP, c31.ap[:], dr["c31"], [], [c31.res], c31.res)
        dma(SP, addm.ap[:], dr["addm"], [], [addm.res], addm.res)
        P.op(POOL, lambda e: e.memset(zeros_bf.ap[:], 0.0), [], [zeros_bf.res])

        def load_w(buf, src_ap, ncols, col0=0):
            P.op(POOL, lambda e, o=buf.ap[:, :, col0:col0 + ncols], i=src_ap.rearrange("(c p) n -> p c n", p=128):
                 e.dma_start(out=o, in_=i), [], [buf.res], dma_slot=buf.res)

        def inproj_fm(wbuf, c0, ncols, n, bank, extra_reads=()):
            for kc in range(8):
                mm(bank.f32[0:ncols, :], wbuf.ap[:, kc, c0:c0 + ncols], hT[:, kc, n * 512:(n + 1) * 512],
                   kc == 0, kc == 7, [wbuf.res] + r_hT[4 * n:4 * n + 4] + list(extra_reads), [bank.res])

        def inproj_tm(wbuf, c0, ncols, t, bank):
            for kc in range(8):
                mm(bank.f32[:, 0:ncols], hT[:, kc, t * 128:(t + 1) * 128], wbuf.ap[:, kc, c0:c0 + ncols],
                   kc == 0, kc == 7, [wbuf.res, r_hT[t]], [bank.res])

        def layout_C():
            a = Alloc()
            L = {}
            L["w_out"] = a.get([128, 8, 1024], BF16)
            L["yt"] = [a.get([128, 8, 512], BF16) for _ in range(2)]
            L["x"] = [a.get([128, 1024], F32) for _ in range(2)]
            L["xn"] = [a.get([128, 1024], F32) for _ in range(2)]
            L["xs"] = [a.get([128, 1024], BF16) for _ in range(2)]
            L["junk"] = a.get([128, 1024], BF16)
            L["postg"] = a.get([128, 1024], F32)
            L["st"] = [a.get([128, 8], F32) for _ in range(4)]
            return L

        def norm_transpose(L, l, t, xbuf, i):
            stt = L["st"][i % 4]
            xs = L["xs"][i % 2]
            act(L["junk"].ap, xbuf.ap, AF.Square, [xbuf.res], [L["junk"].res, stt.res], accum=stt.ap[:, 0:1])
            rsqrt_act(stt.ap[:, 1:2], stt.ap[:, 0:1], float(D), [stt.res], [stt.res])
            ts(DVE, xs.ap, xbuf.ap, stt.ap[:, 1:2], None, ALU.mult, None, [xbuf.res, stt.res], [xs.res])
            bk = bW.next()
            for c in range(8):
                P.op(PE, lambda e, o=bk.bf[:, c * 128:(c + 1) * 128], i_=xs.ap[:, c * 128:(c + 1) * 128]:
                     e.transpose(o, i_, ident.ap[:]), ph([xs.res, ident.res]), [bk.res])
            tt(DVE, hT[:, :, t * 128:(t + 1) * 128], bk.bf[:, 0:1024].rearrange("p (c t) -> p c t", c=8),
               gpre.ap[:, l, :].unsqueeze(2).to_broadcast([128, 8, 128]), ALU.mult,
               [bk.res, gpre.res], [r_hT[t]])

        def phase_A0():
            barrier()
            L = layout_C()
            for t in range(NT):
                xb = L["x"][t % 2]
                dma(SP, xb.ap, x_in[t * 128:(t + 1) * 128, :], ph([]), [xb.res], xb.res)
                norm_transpose(L, 0, t, xb, t)

        def phase_C(l, last):
            barrier()
            L = layout_C()
            src = x_in if l == 0 else out
            for half in range(2):
                P.op(POOL, lambda e, o=L["w_out"].ap[:, :, half * 512:(half + 1) * 512],
                     i=dr["w_out"][l][:, half * 512:(half + 1) * 512].rearrange("(c p) n -> p c n", p=128):
                     e.dma_start(out=o, in_=i), ph([]), [L["w_out"].res], dma_slot=L["w_out"].res)
            dma(SP, L["postg"].ap, dr["post_g"][l:l + 1, :].to_broadcast([128, D]), ph([]), [L["postg"].res], L["postg"].res)
            fin = []
            for n in range(NB):
                yt = L["yt"][n % 2]
                P.op(SP, lambda e, o=yt.ap, i=yT_h[:, n * 512:(n + 1) * 512].rearrange("(c p) t -> p c t", p=128):
                     e.dma_start(out=o, in_=i), ph([r_yT[c][n] for c in range(8)]), [yt.res], dma_slot=yt.res)
                for tl in range(4):
                    t = 4 * n + tl
                    i = t
                    xb = L["x"][i % 2]
                    xn = L["xn"][i % 2]
                    stt = L["st"][(i + 2) % 4]
                    dma(SP, xb.ap, src[t * 128:(t + 1) * 128, :], ph([r_xh[t]]), [xb.res], xb.res)
                    bk2 = [bW.next(), bW.next()]
                    for hf in range(2):
                        for kc in range(8):
                            mm(bk2[hf].f32, yt.ap[:, kc, tl * 128:(tl + 1) * 128], L["w_out"].ap[:, kc, hf * 512:(hf + 1) * 512],
                               kc == 0, kc == 7, [yt.res, L["w_out"].res], [bk2[hf].res])
                    for hf in range(2):
                        act(L["junk"].ap[:, hf * 512:(hf + 1) * 512], bk2[hf].f32, AF.Square, [bk2[hf].res],
                            [L["junk"].res, stt.res], accum=stt.ap[:, 2 + hf:3 + hf])
                    tt(DVE, stt.ap[:, 4:5], stt.ap[:, 2:3], stt.ap[:, 3:4], ALU.add, [stt.res], [stt.res])
                    rsqrt_act(stt.ap[:, 5:6], stt.ap[:, 4:5], float(D), [stt.res], [stt.res])
                    for hf in range(2):
                        sl = slice(hf * 512, (hf + 1) * 512)
                        P.op(DVE, lambda e, o=xn.ap[:, sl], a=bk2[hf].f32, s_=stt.ap[:, 5:6], b=L["postg"].ap[:, sl]:
                             e.scalar_tensor_tensor(o, a, s_, b, ALU.mult, ALU.mult),
                             ph([bk2[hf].res, stt.res, L["postg"].res]), [xn.res])
                    tt(POOL, xn.ap, xn.ap, xb.ap, ALU.add, [xn.res, xb.res], [xn.res])
                    o_ = dma(POOL, out[t * 128:(t + 1) * 128, :], xn.ap, ph([xn.res]), [r_xh[t]], xn.res)
                    fin.append(o_)
                    if not last:
                        norm_transpose(L, l + 1, t, xn, i)
            return fin

        def layout_attn():
            a = Alloc()
            L = {}
            L["KT"] = a.get([128, 4, S], BF16)
            L["KT_res"] = [[Res() for _ in range(NB)] for _ in range(4)]
            L["V"] = a.get([128, NT, 4, 65], BF16)
            L["V_res"] = [Res() for _ in range(NT)]
            L["QT"] = [a.get([128, 4, 512], BF16) for _ in range(2)]
            L["QT_res"] = [[Res() for _ in range(4)] for _ in range(2)]
            L["PT"] = [a.get([128, 512], BF16) for _ in range(4)]
            L["YST"] = [a.get([64, 512], BF16) for _ in range(2)]
            sg_ = a.get([64, 4, 512], F32)
            L["SG"] = [sg_, sg_]
            sgr_ = [Res() for _ in range(4)]
            L["SG_res"] = [sgr_, sgr_]
            L["RD"] = [a.get([64, 512], F32) for _ in range(2)]
            L["DEN"] = [a.get([128, 512], F32) for _ in range(2)]
            L["GE"] = [a.get([64, 512], F32) for _ in range(2)]
            L["alloc"] = a
            return L

        def v_lhsT(L, t, h):
            return L["V"].ap[:, t, h, :]

        def init_V_ones(L):
            P.op(POOL, lambda e, o=L["V"].ap[:, :, :, 64:65]: e.memset(o, 2.0), ph([]), list(L["V_res"]))

        def gates_for_block(L, wbuf, c0, n, qi):
            for c in range(2):
                bk = bW.next()
                inproj_fm(wbuf, c0 + 128 * c, 128, n, bk)
                for hh in range(2):
                    h = 2 * c + hh
                    ge = L["GE"][h % 2]
                    sg = L["SG"][qi].ap[:, h, :]
                    r_sg = L["SG_res"][qi][h]
                    src = bk.f32[64 * hh:64 * hh + 64, :]
                    act(ge.ap, src, AF.Tanh, [bk.res], [ge.res], scale=0.5)
                    P.op(DVE, lambda e, o=sg, a_=ge.ap, b_=src: e.scalar_tensor_tensor(o, a_, 1.0, b_, ALU.add, ALU.mult),
                         ph([ge.res, bk.res]), [r_sg])

        def finish_head(L, ob, n, h, qi, row0, cnt):
            rd = L["RD"][cnt % 2]
            ys = L["YST"][cnt % 2]
            den = L["DEN"][cnt % 2]
            act(den.ap[64:65, :], ob.f32[64:65, :], AF.Ln, [ob.res], [den.res])
            act(den.ap[64:65, :], den.ap[64:65, :], AF.Exp, [den.res], [den.res], scale=-1.0)
            bb = bW.next()
            mm(bb.f32[0:64, :], ones.ap[64:65, 0:64], den.ap[64:65, :], True, True, [ones.res, den.res], [bb.res])
            tt(DVE, rd.ap, bb.f32[0:64, :], L["SG"][qi].ap[:, h, :], ALU.mult, [bb.res, L["SG_res"][qi][h]], [rd.res])
            tt(DVE, ys.ap, ob.f32[0:64, :], rd.ap, ALU.mult, [ob.res, rd.res], [ys.res])
            c, r0 = row0 // 128, row0 % 128
            dma(SP, yT_h[row0:row0 + 64, n * 512:(n + 1) * 512], ys.ap, ph([ys.res]), [r_yT[c][n]], ys.res)

        def attn_block(L, n, h, qi, dk, kind, cnt, pend, finq):
            ob = bO.next()
            qt = L["QT"][qi].ap[0:dk, h, :]
            r_q = L["QT_res"][qi][h]
            tiles = [(4 * n + r, r) for r in range(4)] + [(j, -1) for j in range(4 * n)]
            ntl = len(tiles)
            for idx, (j, r) in enumerate(tiles):
                c0 = 128 * r if r > 0 else 0
                sbk = bS.next()
                pt = L["PT"][(cnt[0]) % 4]
                cnt[0] += 1
                mm(sbk.f32[:, c0:512], L["KT"].ap[0:dk, h, j * 128:(j + 1) * 128], qt[:, c0:512], True, True,
                   [L["KT_res"][h][j // 4], r_q], [sbk.res])
                if kind == "moba" and (4 * n - j) <= 9:
                    stg = L["STG"][cnt[0] % 2]
                    dq = 512 * n - 128 * j
                    tt(DVE, stg.ap[:, c0:512], sbk.f32[:, c0:512], L["GT"].ap[:, h, dq + c0:dq + 512], ALU.add,
                       [sbk.res, L["GT"].res], [stg.res])
                    act(pt.ap[:, c0:512], stg.ap[:, c0:512], AF.Exp, [stg.res], [pt.res])
                elif kind == "moba":
                    act(pt.ap[:, c0:512], sbk.f32[:, c0:512], AF.Exp, [sbk.res, c31.res], [pt.res], bias=c31.ap[:, h:h + 1])
                else:
                    act(pt.ap[:, c0:512], sbk.f32[:, c0:512], AF.Exp, [sbk.res], [pt.res])
                if r >= 0:
                    tt(POOL, pt.ap[:, c0:c0 + 128], pt.ap[:, c0:c0 + 128], tri_le.ap[:], ALU.mult, [pt.res, tri_le.res], [pt.res])
                pend.append((ob, v_lhsT(L, j, h), pt, c0, idx == 0, idx == ntl - 1, L["V_res"][j]))
                if len(pend) > 1:
                    flush_pv(pend)
                    if idx == 0:
                        while finq:
                            finq.pop(0)()
            return ob

        def flush_pv(pend):
            (ob, lhsT, pt, c0, s0, s1, r_v) = pend.pop(0)
            mm(ob.f32[0:65, c0:512], lhsT, pt.ap[:, c0:512], s0, s1, [pt.res, r_v], [ob.res], skip_group_check=True)

        def drain(pend, finq):
            while pend:
                flush_pv(pend)
            while finq:
                finq.pop(0)()

        def mixer_mla(l):
            barrier()
            L = layout_attn()
            a = L["alloc"]
            ckv = a.get([128, 512], BF16)
            sq = a.get([128, 2, 512], F32)
            rq = a.get([128, 512], F32)
            rkv = rq
            cq = a.get([128, 2, 512], BF16)
            tq = [a.get([96, 512], F32) for _ in range(2)]
            tk = tq
            csq = [a.get([96, 512], F32) for _ in range(2)]
            t12 = [a.get([96, 512], F32) for _ in range(2)]
            rtm = a.get([128, 8], F32)
            wuq = a.get([128, 2, 4, 96], BF16)
            wsw = a.get([128, 2, 4, 96], BF16)
            wuk = a.get([128, 4, 64], BF16)
            wuv = a.get([128, 256], BF16)
            wtmp = a.get([128, 2, 4, 96], F32)
            wtmp2 = a.get([128, 2, 4, 96], F32)
            wtmp3 = a.get([128, 4, 64], F32)
            wtmp4 = a.get([128, 256], F32)
            gq = a.get([128, 2], F32)
            gkv = a.get([128, 1], F32)
            init_V_ones(L)
            load_w(wK, dr["w_mla_k"][l], 320)
            load_w(wQ, dr["w_in"][l][:, 0:192], 192)
            load_w(wQ, dr["w_in"][l][:, 2400:2656], 256, col0=192)
            dma(SP, gq.ap, dr["gq"][l], ph([]), [gq.res], gq.res)
            dma(SP, gkv.ap, dr["gkv"][l], ph([]), [gkv.res], gkv.res)
            dma(SP, wtmp.ap, dr["w_uq"][l], ph([]), [wtmp.res], wtmp.res)
            dma(SP, wtmp2.ap, dr["w_uq_sw"][l], ph([]), [wtmp2.res], wtmp2.res)
            dma(SP, wtmp3.ap, dr["w_uk"][l], ph([]), [wtmp3.res], wtmp3.res)
            dma(SP, wtmp4.ap, dr["w_uv"][l], ph([]), [wtmp4.res], wtmp4.res)
            for c in range(2):
                ts(DVE, wuq.ap[:, c], wtmp.ap[:, c], gq.ap[:, c:c + 1], None, ALU.mult, None, [wtmp.res, gq.res], [wuq.res])
                ts(DVE, wsw.ap[:, c], wtmp2.ap[:, c], gq.ap[:, c:c + 1], None, ALU.mult, None, [wtmp2.res, gq.res], [wsw.res])
            ts(DVE, wuk.ap, wtmp3.ap, gkv.ap[:, 0:1], None, ALU.mult, None, [wtmp3.res, gkv.res], [wuk.res])
            ts(DVE, wuv.ap, wtmp4.ap, gkv.ap[:, 0:1], None, ALU.mult, None, [wtmp4.res, gkv.res], [wuv.res])
            if DBG_STOP <= 1:
                return
            for n in range(NB):
                if DBG_STOP <= 2 and n >= 1:
                    return
                tkc, tks = tk[0], tk[1]
                blk = slice(n * 512, (n + 1) * 512)
                dma(SP, tkc.ap[64:96, :], dr["rk_c"][64:96, blk], ph([]), [tkc.res], tkc.res)
                dma(SP, tks.ap[64:96, :], dr["rk_s"][64:96, blk], ph([]), [tks.res], tks.res)
                b_ckv, b_kr, b_ks = bW.next(), bW.next(), bW.next()
                inproj_fm(wK, 0, 128, n, b_ckv)
                inproj_fm(wK, 128, 96, n, b_kr)
                inproj_fm(wK, 224, 96, n, b_ks)
                if DBG_STOP <= 2.1:
                    return
                if 'A' not in DBG_SKIP:
                    cp(DVE, ckv.ap, b_ckv.f32, [b_ckv.res], [ckv.res])
                if 'B' not in DBG_SKIP:
                    act(sq.ap[:, 0, :], b_ckv.f32, AF.Square, [b_ckv.res], [sq.res])
                t1, t2 = t12[0], t12[1]
                if 'C' not in DBG_SKIP:
                    tt(DVE, t1.ap[64:96, :], b_kr.f32[64:96, :], tkc.ap[64:96, :], ALU.mult, [b_kr.res, tkc.res], [t1.res])
                if DBG_STOP <= 2.11:
                    return
                tt(DVE, t2.ap[64:96, :], b_ks.f32[64:96, :], tks.ap[64:96, :], ALU.mult, [b_ks.res, tks.res], [t2.res])
                if DBG_STOP <= 2.12:
                    return
                for h in range(4):
                    if DBG_STOP <= 2.13 and h % 2:
                        continue
                    tt(POOL if h % 2 else DVE, L["KT"].ap[64:96, h, blk], t1.ap[64:96, :], t2.ap[64:96, :], ALU.add,
                       [t1.res, t2.res], [L["KT_res"][h][n]])
                if DBG_STOP <= 2.2:
                    return
                b_ss = bW.next()
                mm(b_ss.f32, ones.ap[:], sq.ap[:, 0, :], True, True, [ones.res, sq.res], [b_ss.res])
                rsqrt_act(rkv.ap, b_ss.f32, 128.0, [b_ss.res], [rkv.res])
                if DBG_STOP <= 2.3:
                    return
                for h in range(4):
                    bk = bW.next()
                    mm(bk.f32[0:64, :], wuk.ap[:, h, :], ckv.ap, True, True, [wuk.res, ckv.res], [bk.res])
                    tt(DVE, L["KT"].ap[0:64, h, blk], bk.f32[0:64, :], rkv.ap[0:64, :], ALU.mult, [bk.res, rkv.res],
                       [L["KT_res"][h][n]])
                if DBG_STOP <= 2.4:
                    return
                b_st = bW.next()
                for tl in range(4):
                    mm(b_st.f32[:, tl:tl + 1], sq.ap[:, 0, tl * 128:(tl + 1) * 128], ones.ap[:, 0:1], True, True,
                       [sq.res, ones.res], [b_st.res])
                if DBG_STOP <= 2.5:
                    return
                rsqrt_act(rtm.ap[:, 0:4], b_st.f32[:, 0:4], 128.0, [b_st.res], [rtm.res])
                for tl in range(4):
                    t = 4 * n + tl
                    bk = bW.next()
                    mm(bk.f32[:, 0:256], ckv.ap[:, tl * 128:(tl + 1) * 128], wuv.ap, True, True, [ckv.res, wuv.res], [bk.res])
                    ts(DVE, L["V"].ap[:, t, :, 0:64], bk.f32[:, 0:256].rearrange("p (h d) -> p h d", h=4), rtm.ap[:, tl:tl + 1], None, ALU.mult, None,
                       [bk.res, rtm.res], [L["V_res"][t]])
            if DBG_STOP <= 3:
                return
            pend, finq, cnt, hc = [], [], [0], [0]
            for n in range(NB):
                drain(pend, finq)
                if DBG_STOP <= 4 and n >= 1:
                    break
                qi = n % 2
                blk = slice(n * 512, (n + 1) * 512)
                tqc, tqs = tq[0], tq[1]
                dma(SP, tqc.ap, dr["rq_c"][:, blk], ph([]), [tqc.res], tqc.res)
                dma(SP, tqs.ap, dr["rq_s"][:, blk], ph([]), [tqs.res], tqs.res)
                b0, b1 = bW.next(), bW.next()
                inproj_fm(wQ, 0, 128, n, b0)
                inproj_fm(wQ, 128, 64, n, b1)
                cp(DVE, cq.ap[:, 0, :], b0.f32, [b0.res], [cq.res])
                cp(DVE, cq.ap[0:64, 1, :], b1.f32[0:64, :], [b1.res], [cq.res])
                act(sq.ap[:, 0, :], b0.f32, AF.Square, [b0.res], [sq.res])
                act(sq.ap[0:64, 1, :], b1.f32[0:64, :], AF.Square, [b1.res], [sq.res])
                b_ss = bW.next()
                mm(b_ss.f32, ones.ap[:], sq.ap[:, 0, :], True, False, [ones.res, sq.res], [b_ss.res])
                mm(b_ss.f32, ones.ap[0:64, :], sq.ap[0:64, 1, :], False, True, [ones.res, sq.res], [b_ss.res])
                rsqrt_act(rq.ap, b_ss.f32, 192.0, [b_ss.res], [rq.res])
                tt(DVE, csq[0].ap, tqc.ap, rq.ap[0:96, :], ALU.mult, [tqc.res, rq.res], [csq[0].res])
                tt(DVE, csq[1].ap, tqs.ap, rq.ap[0:96, :], ALU.mult, [tqs.res, rq.res], [csq[1].res])
                gates_for_block(L, wQ, 192, n, qi)
                for h in range(4):
                    bA, bB = bW.next(), bW.next()
                    mm(bA.f32[0:96, :], wuq.ap[:, 0, h, :], cq.ap[:, 0, :], True, False, [wuq.res, cq.res], [bA.res])
                    mm(bA.f32[0:96, :], wuq.ap[0:64, 1, h, :], cq.ap[0:64, 1, :], False, True, [wuq.res, cq.res], [bA.res])
                    mm(bB.f32[0:96, :], wsw.ap[:, 0, h, :], cq.ap[:, 0, :], True, False, [wsw.res, cq.res], [bB.res])
                    mm(bB.f32[0:96, :], wsw.ap[0:64, 1, h, :], cq.ap[0:64, 1, :], False, True, [wsw.res, cq.res], [bB.res])
                    t1, t2 = t12[0], t12[1]
                    tt(DVE, t1.ap, bA.f32[0:96, :], csq[0].ap, ALU.mult, [bA.res, csq[0].res], [t1.res])
                    tt(DVE, t2.ap, bB.f32[0:96, :], csq[1].ap, ALU.mult, [bB.res, csq[1].res], [t2.res])
                    tt(POOL, L["QT"][qi].ap[0:96, h, :], t1.ap, t2.ap, ALU.add, [t1.res, t2.res], [L["QT_res"][qi][h]])
                if DBG_STOP <= 4:
                    continue
                for h in range(4):
                    ob = attn_block(L, n, h, qi, 96, "mla", cnt, pend, finq)
                    finq.append(lambda ob=ob, n=n, h=h, qi=qi, c=hc[0]: finish_head(L, ob, n, h, qi, 0 + 64 * h, c))
                    hc[0] += 1
            drain(pend, finq)

        def kv_pass(L, l, colK, colV, kmean=None):
            load_w(wK, dr["w_in"][l][:, colK:colK + 256], 256)
            load_w(wK, dr["w_in"][l][:, colV:colV + 256], 256, col0=256)
            for n in range(NB):
                blk = slice(n * 512, (n + 1) * 512)
                for c in range(2):
                    bk = bW.next()
                    inproj_fm(wK, 128 * c, 128, n, bk)
                    cp(DVE, L["KT"].ap[0:64, 2 * c, blk], bk.f32[0:64, :], [bk.res], [L["KT_res"][2 * c][n]])
                    cp(POOL if False else DVE, L["KT"].ap[0:64, 2 * c + 1, blk], bk.f32[64:128, :], [bk.res], [L["KT_res"][2 * c + 1][n]])
                    if kmean is not None:
                        P.op(DVE, lambda e, o=kmean.ap[:, c, 2 * n:2 * n + 2], i=bk.f32.rearrange("p (b t) -> p b t", b=2):
                             e.tensor_reduce(o, i, AX.X, ALU.add), ph([bk.res]), [kmean.res])
                for tl in range(4):
                    t = 4 * n + tl
                    bk = bW.next()
                    inproj_tm(wK, 256, 256, t, bk)
                    cp(DVE, L["V"].ap[:, t, :, 0:64], bk.f32[:, 0:256].rearrange("p (h d) -> p h d", h=4), [bk.res], [L["V_res"][t]])

        def q_pass(L, n, qi, scale, qf=None):
            for c in range(2):
                bk = bW.next()
                inproj_fm(wQ, 128 * c, 128, n, bk)
                for hh in range(2):
                    h = 2 * c + hh
                    ts(DVE, L["QT"][qi].ap[0:64, h, :], bk.f32[64 * hh:64 * hh + 64, :], scale, None, ALU.mult, None,
                       [bk.res], [L["QT_res"][qi][h]])
                    if qf is not None:
                        cp(DVE, qf.ap[:, h, :], bk.f32[64 * hh:64 * hh + 64, :], [bk.res], [qf.res])

        def mixer_sb(l):
            barrier()
            L = layout_attn()
            a = L["alloc"]
            E = [a.get([128, 512], F32) for _ in range(2)]
            LN = [a.get([128, 512], F32) for _ in range(2)]
            LC = [a.get([128, 512], F32) for _ in range(2)]
            TS = [a.get([128, 512], F32) for _ in range(2)]
            c0w = 352
            kv_pass(L, l, c0w + 256, c0w + 512)
            load_w(wQ, dr["w_in"][l][:, c0w:c0w + 256], 256)
            load_w(wQ, dr["w_in"][l][:, 2656:2912], 256, col0=256)
            cnt, hc = [0], [0]
            for n in range(NB):
                qi = n % 2
                q_pass(L, n, qi, 0.125)
                gates_for_block(L, wQ, 256, n, qi)
                for h in range(4):
                    ob = bO.next()
                    lc = LC[hc[0] % 2]
                    P.op(POOL, lambda e, o=lc.ap: e.memset(o, 0.0), ph([]), [lc.res])
                    tiles = [(4 * n + r, r) for r in (3, 2, 1, 0)] + \
                            [(j, -1) for j in range(4 * n - 1, max(4 * n - 1 - SB_WINDOW_TILES, -1), -1)]
                    qt = L["QT"][qi].ap[0:64, h, :]
                    mm(ob.f32[0:64, :], L["V"].ap[:, 4 * n + 3, h, 0:64], zeros_bf.ap[:], True, False,
                       [zeros_bf.res, L["V_res"][4 * n + 3]], [ob.res], skip_group_check=True)
                    for idx, (j, r) in enumerate(tiles):
                        c0 = 128 * r if r > 0 else 0
                        cs = slice(c0, 512)
                        k = cnt[0]
                        cnt[0] += 1
                        e_, ln_, ts_, pt = E[k % 2], LN[k % 2], TS[k % 2], L["PT"][k % 4]
                        zb = bS.next()
                        mm(zb.f32[:, cs], L["KT"].ap[0:64, h, j * 128:(j + 1) * 128], qt[:, cs], True, True,
                           [L["KT_res"][h][j // 4], L["QT_res"][qi][h]], [zb.res])
                        act(e_.ap[:, cs], zb.f32[:, cs], AF.Exp, [zb.res], [e_.res], scale=-1.0)
                        act(e_.ap[:, cs], e_.ap[:, cs], AF.Ln, [e_.res], [e_.res], bias=1.0)
                        tt(DVE, ln_.ap[:, cs], e_.ap[:, cs], zb.f32[:, cs], ALU.add, [e_.res, zb.res], [ln_.res])
                        if r >= 0:
                            tt(POOL, ln_.ap[:, c0:c0 + 128], ln_.ap[:, c0:c0 + 128], tri_lt.ap[:], ALU.mult,
                               [ln_.res, tri_lt.res], [ln_.res])
                        sb_ = bW.next()
                        mm(sb_.f32[:, cs], tri_gt.ap[:], ln_.ap[:, cs], True, idx == 0, [tri_gt.res, ln_.res], [sb_.res])
                        if idx > 0:
                            mm(sb_.f32[:, cs], ones.ap[:], lc.ap[:, cs], False, True, [ones.res, lc.res], [sb_.res])
                        tt(DVE, ts_.ap[:, cs], e_.ap[:, cs], sb_.f32[:, cs], ALU.add, [e_.res, sb_.res], [ts_.res])
                        act(pt.ap[:, cs], ts_.ap[:, cs], AF.Exp, [ts_.res], [pt.res], scale=-1.0)
                        if r >= 0:
                            tt(POOL, pt.ap[:, c0:c0 + 128], pt.ap[:, c0:c0 + 128], tri_lt.ap[:], ALU.mult,
                               [pt.res, tri_lt.res], [pt.res])
                        if idx < len(tiles) - 1:
                            tt(POOL, lc.ap[:, cs], lc.ap[:, cs], ln_.ap[:, cs], ALU.add, [lc.res, ln_.res], [lc.res])
                        mm(ob.f32[0:64, cs], L["V"].ap[:, j, h, 0:64], pt.ap[:, cs], False, idx == len(tiles) - 1,
                           [pt.res, L["V_res"][j]], [ob.res], skip_group_check=True)
                    ys = L["YST"][hc[0] % 2]
                    P.op(DVE, lambda e, o=ys.ap, a_=ob.f32[0:64, :], b_=L["SG"][qi].ap[:, h, :]:
                         e.scalar_tensor_tensor(o, a_, 0.5, b_, ALU.mult, ALU.mult), ph([ob.res, L["SG_res"][qi][h]]), [ys.res])
                    row0 = 256 + 64 * h
                    dma(SP, yT_h[row0:row0 + 64, n * 512:(n + 1) * 512], ys.ap, ph([ys.res]), [r_yT[row0 // 128][n]], ys.res)
                    hc[0] += 1

        def mixer_moba(l):
            barrier()
            L = layout_attn()
            a = L["alloc"]
            L["GT"] = a.get([128, 4, GT_W], BF16)
            L["STG"] = [a.get([128, 512], F32) for _ in range(2)]
            kmean = a.get([128, 2, 16], F32)
            kmh = a.get([64, 4, 16], F32)
            qf = a.get([64, 4, 512], F32)
            g16 = a.get([128, 4, 16], F32)
            top8 = a.get([128, 4, 8], F32)
            mb = a.get([128, 4, 32], BF16)
            init_V_ones(L)
            dma(POOL, L["GT"].ap, dr["gt"], ph([]), [L["GT"].res], L["GT"].res)
            P.op(POOL, lambda e, o=mb.ap: e.memset(o, 0.0), ph([]), [mb.res])
            for h in range(4):
                P.op(POOL, lambda e, o=L["KT"].ap[64:80, h, :], i=dr["onehot"]: e.dma_start(out=o, in_=i),
                     ph([]), [L["KT_res"][h][nn] for nn in range(NB)], dma_slot=L["KT_res"][h][0])
            c0w = 352 + 768
            kv_pass(L, l, c0w + 256, c0w + 512, kmean=kmean)
            for h in range(4):
                cp(DVE, kmh.ap[:, h, :], kmean.ap[64 * (h % 2):64 * (h % 2) + 64, h // 2, :], [kmean.res], [kmh.res])
            load_w(wQ, dr["w_in"][l][:, c0w:c0w + 256], 256)
            load_w(wQ, dr["w_in"][l][:, 2912:3168], 256, col0=256)
            pend, finq, cnt, hc = [], [], [0], [0]
            for n in range(NB):
                drain(pend, finq)
                qi = n % 2
                q_pass(L, n, qi, 0.125, qf=qf)
                gates_for_block(L, wQ, 256, n, qi)
                for tl in range(4):
                    T = 4 * n + tl
                    ob_ = T // 2
                    bg = bW.next()
                    for h in range(4):
                        mm(bg.f32[:, 16 * h:16 * h + 16], qf.ap[:, h, tl * 128:(tl + 1) * 128], kmh.ap[:, h, :], True, True,
                           [qf.res, kmh.res], [bg.res])
                    tt(DVE, g16.ap, bg.f32[:, 0:64].rearrange("p (h b) -> p h b", h=4),
                       addm.ap[:, ob_, :].unsqueeze(1).to_broadcast([128, 4, 16]), ALU.add, [bg.res, addm.res], [g16.res])
                    for h in range(4):
                        P.op(DVE, lambda e, o=top8.ap[:, h, :], i=g16.ap[:, h, :]: e.max(o, i), ph([g16.res]), [top8.res])
                    for h in range(4):
                        ts(DVE, mb.ap[:, h, 0:16], g16.ap[:, h, :], top8.ap[:, h, 3:4], -30000.0, ALU.is_lt, ALU.mult,
                           [g16.res, top8.res], [mb.res])
                    bt = bW.next()
                    P.op(PE, lambda e, o=bt.bf[:, 0:128], i_=mb.ap.rearrange("p h b -> p (h b)"): e.transpose(o, i_, ident.ap[:]),
                         ph([mb.res, ident.res]), [bt.res])
                    for h in range(4):
                        cp(DVE, L["QT"][qi].ap[64:80, h, tl * 128:(tl + 1) * 128], bt.bf[32 * h:32 * h + 16, 0:128],
                           [bt.res], [L["QT_res"][qi][h]])
                for h in range(4):
                    ob = attn_block(L, n, h, qi, 80, "moba", cnt, pend, finq)
                    finq.append(lambda ob=ob, n=n, h=h, qi=qi, c=hc[0]: finish_head(L, ob, n, h, qi, 512 + 64 * h, c))
                    hc[0] += 1
            drain(pend, finq)

        def mixer_conv(l):
            barrier()
            a = Alloc()
            PADW = 32
            xg = a.get([128, 2, PADW + S], BF16)
            r_xg = [[Res() for _ in range(NB)] for _ in range(2)]
            dg = a.get([128, 2, 31, 128], BF16)
            dwT = a.get([128, 2, 31], F32)
            vb = {k: a.get([128, 2], F32) for k in ("dw_b", "ln_g", "ln_b", "pw_b")}
            pw = a.get([128, 2, 256], BF16)
            sig = [a.get([128, 512], F32) for _ in range(2)]
            yc = a.get([128, 2, 512], F32)
            ysq = a.get([128, 2, 512], F32)
            mean = a.get([128, 512], F32)
            rstd = a.get([128, 512], F32)
            msq = a.get([128, 512], F32)
            sw = a.get([128, 2, 512], BF16)
            ee = a.get([128, 512], F32)
            sg = [a.get([128, 512], F32) for _ in range(2)]
            yo = [a.get([128, 512], BF16) for _ in range(2)]
            identf = a.get([128, 128], F32)
            c0w = 352 + 1536
            load_w(wK, dr["w_in"][l][:, c0w:c0w + 512], 512)
            load_w(wQ, dr["w_in"][l][:, 3168:3424], 256)
            dma(SP, dwT.ap, dr["dw_wT"][l], ph([]), [dwT.res], dwT.res)
            for k_, b_ in vb.items():
                dma(SP, b_.ap, dr[k_][l], ph([]), [b_.res], b_.res)
            dma(SP, identf.ap, dr["ident"], ph([]), [identf.res], identf.res)
            P.op(POOL, lambda e, o=pw.ap, i=dr["pw_w"][l].rearrange("(c p) n -> p c n", p=128): e.dma_start(out=o, in_=i),
                 ph([]), [pw.res], dma_slot=pw.res)
            ts(DVE, pw.ap, pw.ap, 0.25, None, ALU.mult, None, [pw.res], [pw.res])
            ts(DVE, vb["pw_b"].ap, vb["pw_b"].ap, 0.5, None, ALU.mult, None, [vb["pw_b"].res], [vb["pw_b"].res])
            for c in range(2):
                for j in range(31):
                    ts(POOL if j % 2 else DVE, dg.ap[:, c, j, :], identf.ap, dwT.ap[:, c, j:j + 1], 0.5, ALU.mult, ALU.mult,
                       [identf.res, dwT.res], [dg.res])
                P.op(POOL, lambda e, o=xg.ap[:, c, 0:PADW]: e.memset(o, 0.0), ph([]), [r_xg[c][0]])
            for n in range(NB):
                for c in range(2):
                    ba, bg_ = bW.next(), bW.next()
                    inproj_fm(wK, 128 * c, 128, n, ba)
                    inproj_fm(wK, 256 + 128 * c, 128, n, bg_)
                    s_ = sig[c]
                    act(s_.ap, bg_.f32, AF.Tanh, [bg_.res], [s_.res], scale=0.5)
                    P.op(DVE, lambda e, o=xg.ap[:, c, PADW + n * 512:PADW + (n + 1) * 512], a_=s_.ap, b_=ba.f32:
                         e.scalar_tensor_tensor(o, a_, 1.0, b_, ALU.add, ALU.mult), ph([s_.res, ba.res]), [r_xg[c][n]])
            for n in range(NB):
                for c in range(2):
                    bc = bW.next()
                    rd = [dg.res, r_xg[c][n]] + ([r_xg[c][n - 1]] if n > 0 else [])
                    for j in range(31):
                        st0 = PADW + n * 512 - 30 + j
                        mm(bc.f32, dg.ap[:, c, j, :], xg.ap[:, c, st0:st0 + 512], j == 0, j == 30, rd, [bc.res])
                    ts(DVE, yc.ap[:, c, :], bc.f32, vb["dw_b"].ap[:, c:c + 1], None, ALU.add, None, [bc.res, vb["dw_b"].res], [yc.res])
                    act(ysq.ap[:, c, :], yc.ap[:, c, :], AF.Square, [yc.res], [ysq.res])
                bm, bq = bW.next(), bW.next()
                mm(bm.f32, ones.ap[:], yc.ap[:, 0, :], True, False, [ones.res, yc.res], [bm.res])
                mm(bm.f32, ones.ap[:], yc.ap[:, 1, :], False, True, [ones.res, yc.res], [bm.res])
                mm(bq.f32, ones.ap[:], ysq.ap[:, 0, :], True, False, [ones.res, ysq.res], [bq.res])
                mm(bq.f32, ones.ap[:], ysq.ap[:, 1, :], False, True, [ones.res, ysq.res], [bq.res])
                ts(DVE, mean.ap, bm.f32, 1.0 / 256, None, ALU.mult, None, [bm.res], [mean.res])
                tt(DVE, msq.ap, mean.ap, mean.ap, ALU.mult, [mean.res], [msq.res])
                P.op(DVE, lambda e, o=msq.ap, a_=bq.f32, b_=msq.ap: e.scalar_tensor_tensor(o, a_, 1.0 / 256, b_, ALU.mult, ALU.subtract),
                     ph([bq.res, msq.res]), [msq.res])
                act(rstd.ap, msq.ap, AF.Ln, [msq.res], [rstd.res], bias=EPS)
                act(rstd.ap, rstd.ap, AF.Exp, [rstd.res], [rstd.res], scale=-0.5)
                for c in range(2):
                    tt(DVE, yc.ap[:, c, :], yc.ap[:, c, :], mean.ap, ALU.subtract, [yc.res, mean.res], [yc.res])
                    tt(DVE, yc.ap[:, c, :], yc.ap[:, c, :], rstd.ap, ALU.mult, [yc.res, rstd.res], [yc.res])
                    ts(DVE, yc.ap[:, c, :], yc.ap[:, c, :], vb["ln_g"].ap[:, c:c + 1], vb["ln_b"].ap[:, c:c + 1], ALU.mult, ALU.add,
                       [yc.res, vb["ln_g"].res, vb["ln_b"].res], [yc.res])
                    act(ee.ap, yc.ap[:, c, :], AF.Tanh, [yc.res], [ee.res], scale=0.5)
                    P.op(DVE, lambda e, o=sw.ap[:, c, :], a_=ee.ap, b_=yc.ap[:, c, :]:
                         e.scalar_tensor_tensor(o, a_, 1.0, b_, ALU.add, ALU.mult), ph([ee.res, yc.res]), [sw.res])
                for c in range(2):
                    bgt = bW.next()
                    inproj_fm(wQ, 128 * c, 128, n, bgt)
                    g_ = sg[c]
                    act(g_.ap, bgt.f32, AF.Tanh, [bgt.res], [g_.res], scale=0.5)
                    P.op(DVE, lambda e, o=g_.ap, a_=g_.ap, b_=bgt.f32: e.scalar_tensor_tensor(o, a_, 1.0, b_, ALU.add, ALU.mult),
                         ph([g_.res, bgt.res]), [g_.res])
                    bp = bW.next()
                    mm(bp.f32, pw.ap[:, 0, 128 * c:128 * c + 128], sw.ap[:, 0, :], True, False, [pw.res, sw.res], [bp.res])
                    mm(bp.f32, pw.ap[:, 1, 128 * c:128 * c + 128], sw.ap[:, 1, :], False, True, [pw.res, sw.res], [bp.res])
                    y_ = yo[c]
                    P.op(DVE, lambda e, o=y_.ap, a_=bp.f32, s1=vb["pw_b"].ap[:, c:c + 1], b_=g_.ap:
                         e.scalar_tensor_tensor(o, a_, s1, b_, ALU.add, ALU.mult), ph([bp.res, vb["pw_b"].res, g_.res]), [y_.res])
                    row0 = 768 + 128 * c
                    dma(SP, yT_h[row0:row0 + 128, n * 512:(n + 1) * 512], y_.ap, ph([y_.res]), [r_yT[row0 // 128][n]], y_.res)

        MIX = {"mla": mixer_mla, "sb": mixer_sb, "moba": mixer_moba, "conv": mixer_conv}
        phase_A0()
        fin = []
        for l in range(n_layers):
            for m in MIX_ALL:
                if m in mixers:
                    MIX[m](l)
            fin = phase_C(l, last=(l == n_layers - 1))
        barrier()
        last_bar = P.ops[DVE][-1]
        P.emit(final_waits=[(POOL, d) for d in fin[-2:]] + [(SP, last_bar)])
    return nc


_CACHE = {}


def kernel(**inputs):
    inp = {k: np.asarray(v, dtype=np.float32) for k, v in inputs.items()}
    w = host_layout(inp)
    if "nc" not in _CACHE:
        _CACHE["nc"] = build()
    nc = _CACHE["nc"]
    x = inp["x"]
    in_maps = []
    for c in range(8):
        m = {"x": np.ascontiguousarray(x[c])}
        m.update(w)
        in_maps.append(m)
    res = run_bass_kernel_spmd(nc, in_maps, core_ids=list(range(8)))
    return np.stack([np.asarray(r["out"], dtype=np.float32) for r in res.results], axis=0)
```

```python
import contextlib
import math
import numpy as np
import concourse.bass as bass
import concourse.mybir as mybir
from concourse.bass_utils import run_bass_kernel_spmd

F32 = mybir.dt.float32
BF16 = mybir.dt.bfloat16
F32R = mybir.dt.float32r
ALU = mybir.AluOpType
AF = mybir.ActivationFunctionType
AX = mybir.AxisListType

S = 4096
D = 1024
NL = 4
NT = 32
NB = 8
EPS = 1e-6
DBG_STOP = 99
DBG_SKIP = ''
SB_WINDOW_TILES = 2

PE, ACT, DVE, POOL, SP = "pe", "act", "dve", "pool", "sp"
COMPUTE = (PE, ACT, DVE, POOL)


class Res:
    __slots__ = ("name", "writer", "readers", "slot", "excl", "persist")

    def __init__(self, name="", excl=False, persist=False):
        self.name = name
        self.excl = excl
        self.persist = persist
        self.writer = None
        self.readers = []
        self.slot = None


class SemSlot:
    __slots__ = ("sem", "count")

    def __init__(self):
        self.sem = None
        self.count = 0


class Op:
    __slots__ = ("eng", "idx", "fn", "waits", "signal", "count", "is_dma", "dma_res",
                 "dma_count", "n_dma")

    def __init__(self, eng, idx, fn):
        self.eng = eng
        self.idx = idx
        self.fn = fn
        self.waits = []
        self.signal = False
        self.count = 0
        self.is_dma = False
        self.dma_res = None
        self.dma_count = 0
        self.n_dma = 0


class Prog:
    def __init__(self, nc):
        self.nc = nc
        self.ops = {e: [] for e in (PE, ACT, DVE, POOL, SP)}
        self.seen = {e: {} for e in self.ops}
        self.slots = []
        self.free_slots = []
        self.phase_res = []

    def release_phase_slots(self):
        for r in self.phase_res:
            if r.slot is not None:
                self.free_slots.append(r.slot)
                r.slot = None
        self.phase_res = []

    def op(self, eng, fn, reads=(), writes=(), dma_slot=None, n_dma=1):
        lst = self.ops[eng]
        o = Op(eng, len(lst), fn)
        ex = [r for r in reads if r.excl and r not in writes]
        if ex:
            writes = list(writes) + ex
        if dma_slot is not None:
            if dma_slot.slot is None:
                if self.free_slots:
                    dma_slot.slot = self.free_slots.pop()
                else:
                    dma_slot.slot = SemSlot()
                    self.slots.append(dma_slot.slot)
                if not dma_slot.persist:
                    self.phase_res.append(dma_slot)
            o.is_dma = True
            o.dma_res = dma_slot.slot
            o.n_dma = n_dma
            dma_slot.slot.count += 16 * n_dma
            o.dma_count = dma_slot.slot.count
        need = {}

        def same(d):
            return (not d.is_dma) and (not o.is_dma) and d.eng == eng

        def add(d):
            if d is None or d is o:
                return
            if d.is_dma:
                key, val = ("dma", id(d.dma_res)), d.dma_count
            else:
                key, val = ("eng", d.eng), d.idx
            if key not in need or val > need[key][0]:
                need[key] = (val, d)

        for r in reads:
            d = r.writer
            if d is not None and not (same(d) and eng == PE):
                add(d)
        for w in writes:
            d = w.writer
            if d is not None and not same(d):
                add(d)
            for rd in w.readers:
                if not same(rd):
                    add(rd)
        seen = self.seen[eng]
        for key, (val, d) in need.items():
            if seen.get(key, -1) >= val:
                continue
            seen[key] = val
            o.waits.append(d)
            if not d.is_dma:
                d.signal = True
        for r in reads:
            r.readers.append(o)
        for w in writes:
            w.writer = o
            w.readers = []
        lst.append(o)
        return o

    def emit(self, final_waits=()):
        nc = self.nc
        final = {}
        for (e, d) in final_waits:
            final.setdefault(e, []).append(d)
            if not d.is_dma:
                d.signal = True
        for e in COMPUTE:
            c = 0
            for o in self.ops[e]:
                if o.signal:
                    c += 1
                o.count = c
        with contextlib.ExitStack() as st:
            esem = {e: st.enter_context(nc.semaphore("prog_" + e)) for e in COMPUTE}
            for i, r in enumerate(self.slots):
                r.sem = st.enter_context(nc.semaphore("dma_%d" % i))
            block = st.enter_context(nc.Block())

            def wait(eng, d):
                if d.is_dma:
                    eng.wait_ge(d.dma_res.sem, d.dma_count)
                else:
                    eng.wait_ge(esem[d.eng], d.count)

            def run(e):
                def body(eng):
                    for o in self.ops[e]:
                        for d in o.waits:
                            wait(eng, d)
                        ins = o.fn(eng)
                        if o.is_dma:
                            if not isinstance(ins, (list, tuple)):
                                ins = [ins]
                            assert len(ins) == o.n_dma, (len(ins), o.n_dma)
                            for i_ in ins:
                                i_.then_inc(o.dma_res.sem, 16)
                        elif o.signal:
                            ins.then_inc(esem[e], 1)
                    for d in final.get(e, []):
                        wait(eng, d)
                return body

            block.tensor(run(PE))
            block.scalar(run(ACT))
            block.vector(run(DVE))
            block.gpsimd(run(POOL))
            block.sync(run(SP))


def _t5_bucket(n):
    n = np.maximum(n, 0)
    max_exact = 16
    n_large = np.maximum(n, max_exact).astype(np.float32)
    large = max_exact + (np.log(n_large / max_exact) / math.log(1024 / max_exact) * (32 - max_exact)).astype(np.int32)
    large = np.minimum(large, 31)
    return np.where(n < max_exact, n, large)


GT_W = 1664


def host_consts():
    c = {}
    c["ident"] = np.eye(128, dtype=np.float32)
    p = np.arange(128)[:, None]
    q = np.arange(128)[None, :]
    c["tri_le"] = (p <= q).astype(np.float32)
    c["tri_lt"] = (p < q).astype(np.float32)
    c["tri_gt"] = (p > q).astype(np.float32)
    c["ones"] = np.ones((128, 128), np.float32)
    half = 16
    freqs = (10000.0 ** (-np.arange(half, dtype=np.float32) / half)).astype(np.float32)
    ang = np.arange(S, dtype=np.float32)[:, None] * freqs[None, :]
    cos = np.cos(ang).astype(np.float32).T
    sin = np.sin(ang).astype(np.float32).T
    sc = np.float32(96 ** -0.5)
    rq_c = np.zeros((96, S), np.float32)
    rq_s = np.zeros((96, S), np.float32)
    rq_c[0:64] = sc
    rq_c[64:80] = cos * sc
    rq_c[80:96] = cos * sc
    rq_s[64:80] = -sin * sc
    rq_s[80:96] = sin * sc
    rk_c = np.zeros((96, S), np.float32)
    rk_s = np.zeros((96, S), np.float32)
    rk_c[64:80] = cos
    rk_c[80:96] = cos
    rk_s[64:80] = -sin
    rk_s[80:96] = sin
    c["rq_c"], c["rq_s"], c["rk_c"], c["rk_s"] = rq_c, rq_s, rk_c, rk_s
    oh = np.zeros((16, S), np.float32)
    for b in range(16):
        oh[b, b * 256:(b + 1) * 256] = 1.0
    c["onehot"] = oh
    am = np.zeros((16, 16), np.float32)
    for ob in range(16):
        am[ob, ob] = 1e30
        am[ob, ob + 1:] = -1e30
    c["addm"] = np.ascontiguousarray(np.broadcast_to(am[None], (128, 16, 16)))
    return c


def host_layout(inp):
    w = {}
    w_in = inp["w_in"]
    w["w_in"] = np.ascontiguousarray(w_in)
    wk = np.zeros((NL, D, 320), np.float32)
    wk[:, :, 0:128] = w_in[:, :, 192:320]
    wk[:, :, 192:224] = w_in[:, :, 320:352]
    wk[:, :, 288:304] = w_in[:, :, 336:352]
    wk[:, :, 304:320] = w_in[:, :, 320:336]
    w["w_mla_k"] = wk
    w["gpre"] = np.ascontiguousarray(inp["pre_norm_g"].reshape(NL, 8, 128).transpose(0, 2, 1))
    gq = np.zeros((NL, 128, 2), np.float32)
    gq[:, :, 0] = inp["mla_q_norm_g"][:, 0:128]
    gq[:, 0:64, 1] = inp["mla_q_norm_g"][:, 128:192]
    w["gq"] = gq
    uq = inp["mla_w_uq"].reshape(NL, 192, 4, 96)
    uq_p = np.zeros((NL, 128, 2, 4, 96), np.float32)
    uq_p[:, :, 0] = uq[:, 0:128]
    uq_p[:, 0:64, 1] = uq[:, 128:192]
    w["w_uq"] = uq_p
    sw = np.zeros((NL, 192, 4, 96), np.float32)
    sw[:, :, :, 64:80] = uq[:, :, :, 80:96]
    sw[:, :, :, 80:96] = uq[:, :, :, 64:80]
    sw_p = np.zeros((NL, 128, 2, 4, 96), np.float32)
    sw_p[:, :, 0] = sw[:, 0:128]
    sw_p[:, 0:64, 1] = sw[:, 128:192]
    w["w_uq_sw"] = sw_p
    w["gkv"] = np.ascontiguousarray(inp["mla_kv_norm_g"].reshape(NL, 128, 1))
    ukv = inp["mla_w_ukv"].reshape(NL, 128, 4, 128)
    w["w_uk"] = np.ascontiguousarray(ukv[:, :, :, 0:64])
    w["w_uv"] = np.ascontiguousarray(ukv[:, :, :, 64:128].reshape(NL, 128, 256))
    rb = inp["rel_bias"]
    pp = np.arange(128)[:, None]
    xx = np.arange(GT_W)[None, :]
    bucket = _t5_bucket(xx - pp)
    w["gt"] = np.ascontiguousarray(rb[bucket].transpose(0, 2, 1))
    w["c31"] = np.ascontiguousarray(np.broadcast_to(rb[31][None, :], (128, 4)))
    w["dw_wT"] = np.ascontiguousarray(inp["conv_dw_w"].transpose(0, 2, 1).reshape(NL, 2, 128, 31).transpose(0, 2, 1, 3))

    def pv(a):
        return np.ascontiguousarray(a.reshape(NL, 2, 128).transpose(0, 2, 1))
    w["dw_b"] = pv(inp["conv_dw_b"])
    w["ln_g"] = pv(inp["conv_ln_g"])
    w["ln_b"] = pv(inp["conv_ln_b"])
    w["pw_b"] = pv(inp["conv_pw_b"])
    w["pw_w"] = np.ascontiguousarray(inp["conv_pw_w"])
    w["w_out"] = np.ascontiguousarray(inp["w_out"])
    w["post_g"] = np.ascontiguousarray(inp["post_norm_g"])
    w.update(host_consts())
    return w


WSHAPES = {
    "w_in": [NL, D, 3424], "w_mla_k": [NL, D, 320], "gpre": [NL, 128, 8], "gq": [NL, 128, 2],
    "w_uq": [NL, 128, 2, 4, 96], "w_uq_sw": [NL, 128, 2, 4, 96], "gkv": [NL, 128, 1],
    "w_uk": [NL, 128, 4, 64], "w_uv": [NL, 128, 256], "gt": [128, 4, GT_W], "c31": [128, 4],
    "dw_wT": [NL, 128, 2, 31], "dw_b": [NL, 128, 2], "ln_g": [NL, 128, 2], "ln_b": [NL, 128, 2],
    "pw_b": [NL, 128, 2], "pw_w": [NL, 256, 256], "w_out": [NL, D, D], "post_g": [NL, D],
    "ident": [128, 128], "tri_le": [128, 128], "tri_lt": [128, 128], "tri_gt": [128, 128],
    "ones": [128, 128], "rq_c": [96, S], "rq_s": [96, S], "rk_c": [96, S], "rk_s": [96, S],
    "onehot": [16, S], "addm": [128, 16, 16],
}


class Bank:
    def __init__(self, t):
        self.t = t
        self.res = Res(excl=True)
        self.f32 = t[:]
        self.bf = t[:].bitcast(BF16)


class RR:
    def __init__(self, items):
        self.items = items
        self.i = 0

    def next(self):
        it = self.items[self.i % len(self.items)]
        self.i += 1
        return it


class Buf:
    def __init__(self, ap, persist=False):
        self.ap = ap if isinstance(ap, bass.AP) else ap[:]
        self.res = Res(persist=persist)

    @property
    def r(self):
        return self.ap.bitcast(F32R)


ARENA_BYTES = 111104
MIX_ALL = ("mla", "sb", "moba", "conv")


def build(n_layers=NL, mixers=MIX_ALL, dbg=False):
    nc = bass.Bass("TRN2", target_bir_lowering=False)
    P = Prog(nc)
    dr = {}
    x_in = nc.dram_tensor("x", [S, D], F32, kind="ExternalInput").ap()
    for k, shp in WSHAPES.items():
        dr[k] = nc.dram_tensor(k, shp, F32, kind="ExternalInput").ap()
    out = nc.dram_tensor("out", [S, D], F32, kind="ExternalOutput").ap()
    yT_h = nc.dram_tensor("yT", [D, S], BF16, kind="ExternalOutput" if dbg else "Internal").ap()
    r_yT = [[Res() for _ in range(NB)] for _ in range(8)]
    r_xh = [Res() for _ in range(NT)]

    st = contextlib.ExitStack()
    with st:
        def sb(name, shape, dt):
            return st.enter_context(nc.sbuf_tensor("s_" + name, shape, dt))

        hT = sb("hT", [128, 8, S], BF16)
        r_hT = [Res() for _ in range(NT)]
        wK = Buf(sb("wK", [128, 8, 512], BF16), persist=True)
        wQ = Buf(sb("wQ", [128, 8, 512], BF16), persist=True)
        ident = Buf(sb("ident", [128, 128], BF16), persist=True)
        tri_le = Buf(sb("tri_le", [128, 128], BF16), persist=True)
        tri_lt = Buf(sb("tri_lt", [128, 128], BF16), persist=True)
        tri_gt = Buf(sb("tri_gt", [128, 128], F32), persist=True)
        tri_lef = Buf(sb("tri_lef", [128, 128], F32), persist=True)
        ones = Buf(sb("ones", [128, 128], F32), persist=True)
        gpre = Buf(sb("gpre", [128, NL, 8], F32), persist=True)
        c31 = Buf(sb("c31", [128, 4], F32), persist=True)
        zeros_bf = Buf(sb("zeros_bf", [128, 512], BF16), persist=True)
        addm = Buf(sb("addm", [128, 16, 16], F32), persist=True)
        LNR = [Buf(sb("lnr%d" % i, [128, 512], F32), persist=True) for i in range(4)]
        arena_t = sb("arena", [128, ARENA_BYTES // 4], F32)
        r_phase = Res("phase")
        dummy = Buf(sb("dummy", [128, 8], F32), persist=True)

        def view(off, shape, dt):
            isz = 2 if dt == BF16 else 4
            n = 1
            for s_ in shape[1:]:
                n *= s_
            nb = n * isz
            assert off % 4 == 0 and nb % 4 == 0 and off + nb <= ARENA_BYTES, (off, nb)
            ap = arena_t[:, off // 4:(off + nb) // 4]
            if dt == BF16:
                ap = ap.bitcast(BF16)
            if len(shape) == 3:
                ap = ap.rearrange("p (a b) -> p a b", a=shape[1])
            elif len(shape) == 4:
                ap = ap.rearrange("p (a b c) -> p a b c", a=shape[1], b=shape[2])
            if shape[0] != 128:
                ap = ap[0:shape[0]]
            return ap

        class Alloc:
            def __init__(self, start=0):
                self.off = start

            def get(self, shape, dt):
                isz = 2 if dt == BF16 else 4
                n = 1
                for s_ in shape[1:]:
                    n *= s_
                nb = (n * isz + 31) // 32 * 32
                v = view(self.off, shape, dt)
                b_ = Buf(v)
                b_.off = self.off
                self.off += nb
                return b_

        banks = [Bank(st.enter_context(nc.psum_tensor("bank%d" % i, [128, 512], F32))) for i in range(8)]
        bS = RR(banks[0:3])
        bO = RR(banks[3:5])
        bW = RR(banks[5:8])

        def dma(q, out_ap, in_ap, reads, writes, slot):
            return P.op(q, lambda e, o=out_ap, i=in_ap: e.dma_start(out=o, in_=i), reads, writes, dma_slot=slot)

        def barrier():
            P.op(DVE, lambda e: e.memset(dummy.ap, 0.0), reads=[], writes=[r_phase, dummy.res])
            P.release_phase_slots()

        def ph(reads):
            return list(reads) + [r_phase]

        def mm(o_ap, lhsT, rhs, start, stop, reads, writes, **kw):
            return P.op(PE, lambda e, o=o_ap, l=lhsT, r=rhs, s0=start, s1=stop, kw=kw:
                        e.matmul(o, l, r, start=s0, stop=s1, **kw), ph(reads), writes)

        def act(o_ap, i_ap, func, reads, writes, scale=1.0, bias=None, accum=None):
            def f(e, o=o_ap, i=i_ap, fu=func, sc=scale, b=bias, a=accum):
                kw = {}
                if b is not None:
                    kw["bias"] = b
                if a is not None:
                    kw["accum_out"] = a
                return e.activation(o, i, fu, scale=sc, **kw)
            return P.op(ACT, f, ph(reads), writes)

        def tt(eng, o_ap, a, b, op, reads, writes):
            return P.op(eng, lambda e, o=o_ap, a=a, b=b, op=op: e.tensor_tensor(o, a, b, op), ph(reads), writes)

        def ts(eng, o_ap, a, s1, s2, op0, op1, reads, writes):
            if op1 is None:
                return P.op(eng, lambda e, o=o_ap, a=a, s1=s1, op0=op0: e.tensor_scalar(o, a, s1, None, op0), ph(reads), writes)
            return P.op(eng, lambda e, o=o_ap, a=a, s1=s1, s2=s2, op0=op0, op1=op1:
                        e.tensor_scalar(o, a, s1, s2, op0, op1), ph(reads), writes)

        def cp(eng, o_ap, i_ap, reads, writes):
            return P.op(eng, lambda e, o=o_ap, i=i_ap: e.tensor_copy(o, i), ph(reads), writes)

        def rsqrt_act(o_ap, i_ap, n_mean, reads, writes):
            act(o_ap, i_ap, AF.Ln, reads, writes, scale=1.0 / n_mean, bias=EPS)
            act(o_ap, o_ap, AF.Exp, writes, writes, scale=-0.5)

        for (b_, nm) in [(ident, "ident"), (tri_le, "tri_le"), (tri_lt, "tri_lt")]:
            dma(POOL, b_.ap[:], dr[nm], [], [b_.res], b_.res)
        dma(SP, tri_gt.ap[:], dr["tri_gt"], [], [tri_gt.res], tri_gt.res)
        dma(SP, tri_lef.ap[:], dr["tri_le"], [], [tri_lef.res], tri_lef.res)
        dma(SP, ones.ap[:], dr["ones"], [], [ones.res], ones.res)
        dma(SP, gpre.ap[:], dr["gpre"].rearrange("l p c -> p l c"), [], [gpre.res], gpre.res)
        dma(SP, c31.ap[:], dr["c31"], [], [c31.res], c31.res)
        ones_r = Buf(sb("ones_r", [128, 128], F32), persist=True)
        tri_gt_r = Buf(sb("tri_gt_r", [128, 128], F32), persist=True)
        tri_le_r = Buf(sb("tri_le_r", [128, 128], F32), persist=True)
        for (dst_, src_) in [(ones_r, ones), (tri_gt_r, tri_gt), (tri_le_r, tri_lef)]:
            P.op(DVE, lambda e, o=dst_.r, i=src_.ap: e.tensor_copy(o, i), [src_.res], [dst_.res])
        dma(SP, addm.ap[:], dr["addm"], [], [addm.res], addm.res)
        P.op(POOL, lambda e: e.memset(zeros_bf.ap[:], 0.0), [], [zeros_bf.res])

        def load_w(buf, src_ap, ncols, col0=0):
            P.op(POOL, lambda e, o=buf.ap[:, :, col0:col0 + ncols], i=src_ap.rearrange("(c p) n -> p c n", p=128):
                 e.dma_start(out=o, in_=i), [], [buf.res], dma_slot=buf.res)

        def inproj_fm(wbuf, c0, ncols, n, bank, extra_reads=()):
            for kc in range(8):
                mm(bank.f32[0:ncols, :], wbuf.ap[:, kc, c0:c0 + ncols], hT[:, kc, n * 512:(n + 1) * 512],
                   kc == 0, kc == 7, [wbuf.res] + r_hT[4 * n:4 * n + 4] + list(extra_reads), [bank.res])

        def inproj_tm(wbuf, c0, ncols, t, bank):
            for kc in range(8):
                mm(bank.f32[:, 0:ncols], hT[:, kc, t * 128:(t + 1) * 128], wbuf.ap[:, kc, c0:c0 + ncols],
                   kc == 0, kc == 7, [wbuf.res, r_hT[t]], [bank.res])

        def layout_C():
            a = Alloc()
            L = {}
            L["w_out"] = a.get([128, 8, 1024], BF16)
            L["yt"] = [a.get([128, 8, 512], BF16) for _ in range(2)]
            L["x"] = [a.get([128, 1024], F32) for _ in range(2)]
            L["xn"] = [a.get([128, 1024], F32) for _ in range(2)]
            L["xs"] = [a.get([128, 1024], BF16) for _ in range(2)]
            L["junk"] = a.get([128, 1024], BF16)
            L["postg"] = a.get([128, 1024], F32)
            L["st"] = [a.get([128, 8], F32) for _ in range(4)]
            return L

        def norm_transpose(L, l, t, xbuf, i):
            stt = L["st"][i % 4]
            xs = L["xs"][i % 2]
            act(L["junk"].ap, xbuf.ap, AF.Square, [xbuf.res], [L["junk"].res, stt.res], accum=stt.ap[:, 0:1])
            rsqrt_act(stt.ap[:, 1:2], stt.ap[:, 0:1], float(D), [stt.res], [stt.res])
            ts(DVE, xs.ap, xbuf.ap, stt.ap[:, 1:2], None, ALU.mult, None, [xbuf.res, stt.res], [xs.res])
            bk = bW.next()
            for c in range(8):
                P.op(PE, lambda e, o=bk.bf[:, c * 128:(c + 1) * 128], i_=xs.ap[:, c * 128:(c + 1) * 128]:
                     e.transpose(o, i_, ident.ap[:]), ph([xs.res, ident.res]), [bk.res])
            tt(DVE, hT[:, :, t * 128:(t + 1) * 128], bk.bf[:, 0:1024].rearrange("p (c t) -> p c t", c=8),
               gpre.ap[:, l, :].unsqueeze(2).to_broadcast([128, 8, 128]), ALU.mult,
               [bk.res, gpre.res], [r_hT[t]])

        def phase_A0():
            barrier()
            L = layout_C()
            for t in range(NT):
                xb = L["x"][t % 2]
                dma(SP, xb.ap, x_in[t * 128:(t + 1) * 128, :], ph([]), [xb.res], xb.res)
                norm_transpose(L, 0, t, xb, t)

        def phase_C(l, last):
            barrier()
            L = layout_C()
            src = x_in if l == 0 else out
            for half in range(2):
                P.op(POOL, lambda e, o=L["w_out"].ap[:, :, half * 512:(half + 1) * 512],
                     i=dr["w_out"][l][:, half * 512:(half + 1) * 512].rearrange("(c p) n -> p c n", p=128):
                     e.dma_start(out=o, in_=i), ph([]), [L["w_out"].res], dma_slot=L["w_out"].res)
            dma(SP, L["postg"].ap, dr["post_g"][l:l + 1, :].to_broadcast([128, D]), ph([]), [L["postg"].res], L["postg"].res)
            fin = []
            for n in range(NB):
                yt = L["yt"][n % 2]
                P.op(SP, lambda e, o=yt.ap, i=yT_h[:, n * 512:(n + 1) * 512].rearrange("(c p) t -> p c t", p=128):
                     e.dma_start(out=o, in_=i), ph([r_yT[c][n] for c in range(8)]), [yt.res], dma_slot=yt.res)
                for tl in range(4):
                    t = 4 * n + tl
                    i = t
                    xb = L["x"][i % 2]
                    xn = L["xn"][i % 2]
                    stt = L["st"][(i + 2) % 4]
                    dma(SP, xb.ap, src[t * 128:(t + 1) * 128, :], ph([r_xh[t]]), [xb.res], xb.res)
                    bk2 = [bW.next(), bW.next()]
                    for hf in range(2):
                        for kc in range(8):
                            mm(bk2[hf].f32, yt.ap[:, kc, tl * 128:(tl + 1) * 128], L["w_out"].ap[:, kc, hf * 512:(hf + 1) * 512],
                               kc == 0, kc == 7, [yt.res, L["w_out"].res], [bk2[hf].res])
                    for hf in range(2):
                        act(L["junk"].ap[:, hf * 512:(hf + 1) * 512], bk2[hf].f32, AF.Square, [bk2[hf].res],
                            [L["junk"].res, stt.res], accum=stt.ap[:, 2 + hf:3 + hf])
                    tt(DVE, stt.ap[:, 4:5], stt.ap[:, 2:3], stt.ap[:, 3:4], ALU.add, [stt.res], [stt.res])
                    rsqrt_act(stt.ap[:, 5:6], stt.ap[:, 4:5], float(D), [stt.res], [stt.res])
                    for hf in range(2):
                        sl = slice(hf * 512, (hf + 1) * 512)
                        P.op(DVE, lambda e, o=xn.ap[:, sl], a=bk2[hf].f32, s_=stt.ap[:, 5:6], b=L["postg"].ap[:, sl]:
                             e.scalar_tensor_tensor(o, a, s_, b, ALU.mult, ALU.mult),
                             ph([bk2[hf].res, stt.res, L["postg"].res]), [xn.res])
                    tt(POOL, xn.ap, xn.ap, xb.ap, ALU.add, [xn.res, xb.res], [xn.res])
                    o_ = dma(POOL, out[t * 128:(t + 1) * 128, :], xn.ap, ph([xn.res]), [r_xh[t]], xn.res)
                    fin.append(o_)
                    if not last:
                        norm_transpose(L, l + 1, t, xn, i)
            return fin

        def layout_attn():
            a = Alloc()
            L = {}
            L["KT"] = a.get([128, 4, S], BF16)
            L["KT_res"] = [[Res() for _ in range(NB)] for _ in range(4)]
            L["V"] = a.get([128, NT, 4, 65], BF16)
            L["V_res"] = [Res() for _ in range(NT)]
            L["QT"] = [a.get([128, 4, 512], BF16) for _ in range(2)]
            L["QT_res"] = [[Res() for _ in range(4)] for _ in range(2)]
            L["PT"] = [a.get([128, 512], BF16) for _ in range(4)]
            L["YST"] = [a.get([64, 512], BF16) for _ in range(2)]
            sg_ = a.get([64, 4, 512], F32)
            L["SG"] = [sg_, sg_]
            sgr_ = [Res() for _ in range(4)]
            L["SG_res"] = [sgr_, sgr_]
            L["RD"] = [a.get([64, 512], F32) for _ in range(2)]
            L["DEN"] = [LNR[0], LNR[1]]
            L["GE"] = [a.get([64, 512], F32) for _ in range(2)]
            L["alloc"] = a
            return L

        def v_lhsT(L, t, h):
            return L["V"].ap[:, t, h, :]

        def init_V_ones(L):
            P.op(POOL, lambda e, o=L["V"].ap[:, :, :, 64:65]: e.memset(o, 2.0), ph([]), list(L["V_res"]))

        def gates_for_block(L, wbuf, c0, n, qi):
            for c in range(2):
                bk = bW.next()
                inproj_fm(wbuf, c0 + 128 * c, 128, n, bk)
                for hh in range(2):
                    h = 2 * c + hh
                    ge = L["GE"][h % 2]
                    sg = L["SG"][qi].ap[:, h, :]
                    r_sg = L["SG_res"][qi][h]
                    src = bk.f32[64 * hh:64 * hh + 64, :]
                    act(ge.ap, src, AF.Tanh, [bk.res], [ge.res], scale=0.5)
                    P.op(DVE, lambda e, o=sg, a_=ge.ap, b_=src: e.scalar_tensor_tensor(o, a_, 1.0, b_, ALU.add, ALU.mult),
                         ph([ge.res, bk.res]), [r_sg])

        def finish_head(L, ob, n, h, qi, row0, cnt):
            rd = L["RD"][cnt % 2]
            ys = L["YST"][cnt % 2]
            den = L["DEN"][cnt % 2]
            act(rd.ap[0:1, :], ob.f32[64:65, :], AF.Ln, [ob.res], [rd.res])
            act(den.r[64:65, :], rd.ap[0:1, :], AF.Exp, [rd.res], [den.res], scale=-1.0)
            bb = bW.next()
            mm(bb.f32[0:64, :], ones_r.r[64:65, 0:64], den.r[64:65, :], True, True, [ones_r.res, den.res], [bb.res])
            tt(DVE, rd.ap, bb.f32[0:64, :], L["SG"][qi].ap[:, h, :], ALU.mult, [bb.res, L["SG_res"][qi][h]], [rd.res])
            tt(DVE, ys.ap, ob.f32[0:64, :], rd.ap, ALU.mult, [ob.res, rd.res], [ys.res])
            c, r0 = row0 // 128, row0 % 128
            dma(SP, yT_h[row0:row0 + 64, n * 512:(n + 1) * 512], ys.ap, ph([ys.res]), [r_yT[c][n]], ys.res)

        def attn_block(L, n, h, qi, dk, kind, cnt, pend, finq):
            ob = bO.next()
            qt = L["QT"][qi].ap[0:dk, h, :]
            r_q = L["QT_res"][qi][h]
            tiles = [(4 * n + r, r) for r in range(4)] + [(j, -1) for j in range(4 * n)]
            ntl = len(tiles)
            for idx, (j, r) in enumerate(tiles):
                c0 = 128 * r if r > 0 else 0
                sbk = bS.next()
                pt = L["PT"][(cnt[0]) % 4]
                cnt[0] += 1
                mm(sbk.f32[:, c0:512], L["KT"].ap[0:dk, h, j * 128:(j + 1) * 128], qt[:, c0:512], True, True,
                   [L["KT_res"][h][j // 4], r_q], [sbk.res])
                if kind == "moba" and (4 * n - j) <= 9:
                    stg = L["STG"][cnt[0] % 2]
                    dq = 512 * n - 128 * j
                    tt(DVE, stg.ap[:, c0:512], sbk.f32[:, c0:512], L["GT"].ap[:, h, dq + c0:dq + 512], ALU.add,
                       [sbk.res, L["GT"].res], [stg.res])
                    act(pt.ap[:, c0:512], stg.ap[:, c0:512], AF.Exp, [stg.res], [pt.res])
                elif kind == "moba":
                    act(pt.ap[:, c0:512], sbk.f32[:, c0:512], AF.Exp, [sbk.res, c31.res], [pt.res], bias=c31.ap[:, h:h + 1])
                else:
                    act(pt.ap[:, c0:512], sbk.f32[:, c0:512], AF.Exp, [sbk.res], [pt.res])
                if r >= 0:
                    tt(POOL, pt.ap[:, c0:c0 + 128], pt.ap[:, c0:c0 + 128], tri_le.ap[:], ALU.mult, [pt.res, tri_le.res], [pt.res])
                pend.append((ob, v_lhsT(L, j, h), pt, c0, idx == 0, idx == ntl - 1, L["V_res"][j]))
                if len(pend) > 1:
                    flush_pv(pend)
                    if idx == 0:
                        while finq:
                            finq.pop(0)()
            return ob

        def flush_pv(pend):
            (ob, lhsT, pt, c0, s0, s1, r_v) = pend.pop(0)
            mm(ob.f32[0:65, c0:512], lhsT, pt.ap[:, c0:512], s0, s1, [pt.res, r_v], [ob.res], skip_group_check=True)

        def drain(pend, finq):
            while pend:
                flush_pv(pend)
            while finq:
                finq.pop(0)()

        def mixer_mla(l):
            barrier()
            L = layout_attn()
            a = L["alloc"]
            ckv = a.get([128, 512], BF16)
            sq = a.get([128, 2, 512], F32)
            rq = a.get([128, 512], F32)
            rkv = rq
            cq = a.get([128, 2, 512], BF16)
            tq = [a.get([96, 512], F32) for _ in range(2)]
            tk = tq
            csq = [a.get([96, 512], F32) for _ in range(2)]
            t12 = [a.get([96, 512], F32) for _ in range(2)]
            rtm = a.get([128, 8], F32)
            wuq = a.get([128, 2, 4, 96], BF16)
            wsw = a.get([128, 2, 4, 96], BF16)
            wuk = a.get([128, 4, 64], BF16)
            wuv = a.get([128, 256], BF16)
            wtmp = Buf(view(sq.off, [128, 2, 4, 96], F32)); wtmp.res = sq.res
            wtmp2 = Buf(view(tq[0].off, [128, 2, 4, 96], F32)); wtmp2.res = tq[0].res; wtmp2.res2 = tq[1].res
            wtmp3 = Buf(view(csq[0].off, [128, 4, 64], F32)); wtmp3.res = csq[0].res
            wtmp4 = Buf(view(csq[1].off, [128, 256], F32)); wtmp4.res = csq[1].res
            gq = a.get([128, 2], F32)
            gkv = a.get([128, 1], F32)
            init_V_ones(L)
            load_w(wK, dr["w_mla_k"][l], 320)
            load_w(wQ, dr["w_in"][l][:, 0:192], 192)
            load_w(wQ, dr["w_in"][l][:, 2400:2656], 256, col0=192)
            dma(SP, gq.ap, dr["gq"][l], ph([]), [gq.res], gq.res)
            dma(SP, gkv.ap, dr["gkv"][l], ph([]), [gkv.res], gkv.res)
            dma(SP, wtmp.ap, dr["w_uq"][l], ph([]), [wtmp.res], wtmp.res)
            dma(SP, wtmp2.ap, dr["w_uq_sw"][l], ph([]), [wtmp2.res, wtmp2.res2], wtmp2.res)
            dma(SP, wtmp3.ap, dr["w_uk"][l], ph([]), [wtmp3.res], wtmp3.res)
            dma(SP, wtmp4.ap, dr["w_uv"][l], ph([]), [wtmp4.res], wtmp4.res)
            for c in range(2):
                ts(DVE, wuq.ap[:, c], wtmp.ap[:, c], gq.ap[:, c:c + 1], None, ALU.mult, None, [wtmp.res, gq.res], [wuq.res])
                ts(DVE, wsw.ap[:, c], wtmp2.ap[:, c], gq.ap[:, c:c + 1], None, ALU.mult, None, [wtmp2.res, wtmp2.res2, gq.res], [wsw.res])
            ts(DVE, wuk.ap, wtmp3.ap, gkv.ap[:, 0:1], None, ALU.mult, None, [wtmp3.res, gkv.res], [wuk.res])
            ts(DVE, wuv.ap, wtmp4.ap, gkv.ap[:, 0:1], None, ALU.mult, None, [wtmp4.res, gkv.res], [wuv.res])
            if DBG_STOP <= 1:
                return
            for n in range(NB):
                if DBG_STOP <= 2 and n >= 1:
                    return
                tkc, tks = tk[0], tk[1]
                blk = slice(n * 512, (n + 1) * 512)
                dma(SP, tkc.ap[64:96, :], dr["rk_c"][64:96, blk], ph([]), [tkc.res], tkc.res)
                dma(SP, tks.ap[64:96, :], dr["rk_s"][64:96, blk], ph([]), [tks.res], tks.res)
                b_ckv, b_kr, b_ks = bW.next(), bW.next(), bW.next()
                inproj_fm(wK, 0, 128, n, b_ckv)
                inproj_fm(wK, 128, 96, n, b_kr)
                inproj_fm(wK, 224, 96, n, b_ks)
                if DBG_STOP <= 2.1:
                    return
                if 'A' not in DBG_SKIP:
                    cp(DVE, ckv.ap, b_ckv.f32, [b_ckv.res], [ckv.res])
                if 'B' not in DBG_SKIP:
                    act(sq.ap[:, 0, :], b_ckv.f32, AF.Square, [b_ckv.res], [sq.res])
                t1, t2 = t12[0], t12[1]
                if 'C' not in DBG_SKIP:
                    tt(DVE, t1.ap[64:96, :], b_kr.f32[64:96, :], tkc.ap[64:96, :], ALU.mult, [b_kr.res, tkc.res], [t1.res])
                if DBG_STOP <= 2.11:
                    return
                tt(DVE, t2.ap[64:96, :], b_ks.f32[64:96, :], tks.ap[64:96, :], ALU.mult, [b_ks.res, tks.res], [t2.res])
                if DBG_STOP <= 2.12:
                    return
                for h in range(4):
                    if DBG_STOP <= 2.13 and h % 2:
                        continue
                    tt(POOL if h % 2 else DVE, L["KT"].ap[64:96, h, blk], t1.ap[64:96, :], t2.ap[64:96, :], ALU.add,
                       [t1.res, t2.res], [L["KT_res"][h][n]])
                if DBG_STOP <= 2.2:
                    return
                b_ss = bW.next()
                mm(b_ss.f32, ones.ap[:], sq.ap[:, 0, :], True, True, [ones.res, sq.res], [b_ss.res])
                rsqrt_act(rkv.ap, b_ss.f32, 128.0, [b_ss.res], [rkv.res])
                if DBG_STOP <= 2.3:
                    return
                for h in range(4):
                    bk = bW.next()
                    mm(bk.f32[0:64, :], wuk.ap[:, h, :], ckv.ap, True, True, [wuk.res, ckv.res], [bk.res])
                    tt(DVE, L["KT"].ap[0:64, h, blk], bk.f32[0:64, :], rkv.ap[0:64, :], ALU.mult, [bk.res, rkv.res],
                       [L["KT_res"][h][n]])
                if DBG_STOP <= 2.4:
                    return
                b_st = bW.next()
                for tl in range(4):
                    mm(b_st.f32[:, tl:tl + 1], sq.ap[:, 0, tl * 128:(tl + 1) * 128], ones.ap[:, 0:1], True, True,
                       [sq.res, ones.res], [b_st.res])
                if DBG_STOP <= 2.5:
                    return
                rsqrt_act(rtm.ap[:, 0:4], b_st.f32[:, 0:4], 128.0, [b_st.res], [rtm.res])
                for tl in range(4):
                    t = 4 * n + tl
                    bk = bW.next()
                    mm(bk.f32[:, 0:256], ckv.ap[:, tl * 128:(tl + 1) * 128], wuv.ap, True, True, [ckv.res, wuv.res], [bk.res])
                    ts(DVE, L["V"].ap[:, t, :, 0:64], bk.f32[:, 0:256].rearrange("p (h d) -> p h d", h=4), rtm.ap[:, tl:tl + 1], None, ALU.mult, None,
                       [bk.res, rtm.res], [L["V_res"][t]])
            if DBG_STOP <= 3:
                return
            pend, finq, cnt, hc = [], [], [0], [0]
            for n in range(NB):
                drain(pend, finq)
                if DBG_STOP <= 4 and n >= 1:
                    break
                qi = n % 2
                blk = slice(n * 512, (n + 1) * 512)
                tqc, tqs = tq[0], tq[1]
                dma(SP, tqc.ap, dr["rq_c"][:, blk], ph([]), [tqc.res], tqc.res)
                dma(SP, tqs.ap, dr["rq_s"][:, blk], ph([]), [tqs.res], tqs.res)
                b0, b1 = bW.next(), bW.next()
                inproj_fm(wQ, 0, 128, n, b0)
                inproj_fm(wQ, 128, 64, n, b1)
                cp(DVE, cq.ap[:, 0, :], b0.f32, [b0.res], [cq.res])
                cp(DVE, cq.ap[0:64, 1, :], b1.f32[0:64, :], [b1.res], [cq.res])
                act(sq.ap[:, 0, :], b0.f32, AF.Square, [b0.res], [sq.res])
                act(sq.ap[0:64, 1, :], b1.f32[0:64, :], AF.Square, [b1.res], [sq.res])
                b_ss = bW.next()
                mm(b_ss.f32, ones.ap[:], sq.ap[:, 0, :], True, False, [ones.res, sq.res], [b_ss.res])
                mm(b_ss.f32, ones.ap[0:64, :], sq.ap[0:64, 1, :], False, True, [ones.res, sq.res], [b_ss.res])
                rsqrt_act(rq.ap, b_ss.f32, 192.0, [b_ss.res], [rq.res])
                tt(DVE, csq[0].ap, tqc.ap, rq.ap[0:96, :], ALU.mult, [tqc.res, rq.res], [csq[0].res])
                tt(DVE, csq[1].ap, tqs.ap, rq.ap[0:96, :], ALU.mult, [tqs.res, rq.res], [csq[1].res])
                gates_for_block(L, wQ, 192, n, qi)
                for h in range(4):
                    bA, bB = bW.next(), bW.next()
                    mm(bA.f32[0:96, :], wuq.ap[:, 0, h, :], cq.ap[:, 0, :], True, False, [wuq.res, cq.res], [bA.res])
                    mm(bA.f32[0:96, :], wuq.ap[0:64, 1, h, :], cq.ap[0:64, 1, :], False, True, [wuq.res, cq.res], [bA.res])
                    mm(bB.f32[0:96, :], wsw.ap[:, 0, h, :], cq.ap[:, 0, :], True, False, [wsw.res, cq.res], [bB.res])
                    mm(bB.f32[0:96, :], wsw.ap[0:64, 1, h, :], cq.ap[0:64, 1, :], False, True, [wsw.res, cq.res], [bB.res])
                    t1, t2 = t12[0], t12[1]
                    tt(DVE, t1.ap, bA.f32[0:96, :], csq[0].ap, ALU.mult, [bA.res, csq[0].res], [t1.res])
                    tt(DVE, t2.ap, bB.f32[0:96, :], csq[1].ap, ALU.mult, [bB.res, csq[1].res], [t2.res])
                    tt(POOL, L["QT"][qi].ap[0:96, h, :], t1.ap, t2.ap, ALU.add, [t1.res, t2.res], [L["QT_res"][qi][h]])
                if DBG_STOP <= 4:
                    continue
                for h in range(4):
                    ob = attn_block(L, n, h, qi, 96, "mla", cnt, pend, finq)
                    finq.append(lambda ob=ob, n=n, h=h, qi=qi, c=hc[0]: finish_head(L, ob, n, h, qi, 0 + 64 * h, c))
                    hc[0] += 1
            drain(pend, finq)

        def kv_pass(L, l, colK, colV, kmean=None):
            load_w(wK, dr["w_in"][l][:, colK:colK + 256], 256)
            load_w(wK, dr["w_in"][l][:, colV:colV + 256], 256, col0=256)
            for n in range(NB):
                blk = slice(n * 512, (n + 1) * 512)
                for c in range(2):
                    bk = bW.next()
                    inproj_fm(wK, 128 * c, 128, n, bk)
                    cp(DVE, L["KT"].ap[0:64, 2 * c, blk], bk.f32[0:64, :], [bk.res], [L["KT_res"][2 * c][n]])
                    cp(POOL if False else DVE, L["KT"].ap[0:64, 2 * c + 1, blk], bk.f32[64:128, :], [bk.res], [L["KT_res"][2 * c + 1][n]])
                    if kmean is not None:
                        P.op(DVE, lambda e, o=kmean.ap[:, c, 2 * n:2 * n + 2], i=bk.f32.rearrange("p (b t) -> p b t", b=2):
                             e.tensor_reduce(o, i, AX.X, ALU.add), ph([bk.res]), [kmean.res])
                for tl in range(4):
                    t = 4 * n + tl
                    bk = bW.next()
                    inproj_tm(wK, 256, 256, t, bk)
                    cp(DVE, L["V"].ap[:, t, :, 0:64], bk.f32[:, 0:256].rearrange("p (h d) -> p h d", h=4), [bk.res], [L["V_res"][t]])

        def q_pass(L, n, qi, scale, qf=None):
            for c in range(2):
                bk = bW.next()
                inproj_fm(wQ, 128 * c, 128, n, bk)
                for hh in range(2):
                    h = 2 * c + hh
                    ts(DVE, L["QT"][qi].ap[0:64, h, :], bk.f32[64 * hh:64 * hh + 64, :], scale, None, ALU.mult, None,
                       [bk.res], [L["QT_res"][qi][h]])
                    if qf is not None:
                        cp(DVE, qf.ap[:, h, :], bk.f32[64 * hh:64 * hh + 64, :], [bk.res], [qf.res])

        def mixer_sb(l):
            barrier()
            L = layout_attn()
            a = L["alloc"]
            E = [[a.get([128, 512], F32) for _ in range(2)] for _ in range(2)]
            LN = [[LNR[0], LNR[1]], [LNR[2], LNR[3]]]
            TS = [a.get([128, 512], F32) for _ in range(2)]
            c0w = 352
            kv_pass(L, l, c0w + 256, c0w + 512)
            load_w(wQ, dr["w_in"][l][:, c0w:c0w + 256], 256)
            load_w(wQ, dr["w_in"][l][:, 2656:2912], 256, col0=256)
            cnt, hc = [0], [0]
            for n in range(NB):
                qi = n % 2
                q_pass(L, n, qi, 0.125)
                gates_for_block(L, wQ, 256, n, qi)
                tiles = [(4 * n + r, r) for r in (3, 2, 1, 0)] + \
                        [(j, -1) for j in range(4 * n - 1, max(4 * n - 1 - SB_WINDOW_TILES, -1), -1)]
                ntl = len(tiles)
                for hp in range(2):
                    heads = (2 * hp, 2 * hp + 1)
                    obs = [bO.next(), bO.next()]
                    Bs = [bW.next(), bW.next()]
                    for hh, h in enumerate(heads):
                        mm(obs[hh].f32[0:64, :], L["V"].ap[:, 4 * n + 3, h, 0:64], zeros_bf.ap[:], True, False,
                           [zeros_bf.res, L["V_res"][4 * n + 3]], [obs[hh].res], skip_group_check=True)
                        mm(Bs[hh].f32, ident.ap[:], zeros_bf.ap[:], True, False, [ident.res, zeros_bf.res], [Bs[hh].res], skip_group_check=True)

                    def stage_a(idx):
                        j, r = tiles[idx]
                        c0 = 128 * r if r > 0 else 0
                        cs = slice(c0, 512)
                        for hh, h in enumerate(heads):
                            e_, ln_ = E[hh][idx % 2], LN[hh][idx % 2]
                            zb = bS.next()
                            mm(zb.f32[:, cs], L["KT"].ap[0:64, h, j * 128:(j + 1) * 128], L["QT"][qi].ap[0:64, h, cs], True, True,
                               [L["KT_res"][h][j // 4], L["QT_res"][qi][h]], [zb.res])
                            act(e_.ap[:, cs], zb.f32[:, cs], AF.Exp, [zb.res], [e_.res], scale=-1.0)
                            act(e_.ap[:, cs], e_.ap[:, cs], AF.Ln, [e_.res], [e_.res], bias=1.0)
                            tt(DVE, ln_.r[:, cs], e_.ap[:, cs], zb.f32[:, cs], ALU.add, [e_.res, zb.res], [ln_.res])
                            if r >= 0:
                                tt(POOL, ln_.r[:, c0:c0 + 128], ln_.r[:, c0:c0 + 128], tri_lt.ap[:], ALU.mult,
                                   [ln_.res, tri_lt.res], [ln_.res])

                    def stage_b(idx):
                        j, r = tiles[idx]
                        c0 = 128 * r if r > 0 else 0
                        cs = slice(c0, 512)
                        for hh, h in enumerate(heads):
                            e_, ln_, ts_ = E[hh][idx % 2], LN[hh][idx % 2], TS[hh]
                            B, ob = Bs[hh], obs[hh]
                            pt = L["PT"][cnt[0] % 4]
                            cnt[0] += 1
                            mm(B.f32[:, cs], tri_gt_r.r, ln_.r[:, cs], False, False, [tri_gt_r.res, ln_.res], [B.res], skip_group_check=True)
                            tt(DVE, ts_.ap[:, cs], e_.ap[:, cs], B.f32[:, cs], ALU.add, [e_.res, B.res], [ts_.res])
                            act(pt.ap[:, cs], ts_.ap[:, cs], AF.Exp, [ts_.res], [pt.res], scale=-1.0)
                            if r >= 0:
                                tt(POOL, pt.ap[:, c0:c0 + 128], pt.ap[:, c0:c0 + 128], tri_lt.ap[:], ALU.mult,
                                   [pt.res, tri_lt.res], [pt.res])
                            if idx < ntl - 1:
                                mm(B.f32[:, cs], tri_le_r.r, ln_.r[:, cs], False, False, [tri_le_r.res, ln_.res], [B.res], skip_group_check=True)
                            mm(ob.f32[0:64, cs], L["V"].ap[:, j, h, 0:64], pt.ap[:, cs], False, idx == ntl - 1,
                               [pt.res, L["V_res"][j]], [ob.res], skip_group_check=True)

                    stage_a(0)
                    for idx in range(ntl):
                        if idx + 1 < ntl:
                            stage_a(idx + 1)
                        stage_b(idx)
                    for hh, h in enumerate(heads):
                        ys = L["YST"][hc[0] % 2]
                        P.op(DVE, lambda e, o=ys.ap, a_=obs[hh].f32[0:64, :], b_=L["SG"][qi].ap[:, h, :]:
                             e.scalar_tensor_tensor(o, a_, 0.5, b_, ALU.mult, ALU.mult), ph([obs[hh].res, L["SG_res"][qi][h]]), [ys.res])
                        row0 = 256 + 64 * h
                        dma(SP, yT_h[row0:row0 + 64, n * 512:(n + 1) * 512], ys.ap, ph([ys.res]), [r_yT[row0 // 128][n]], ys.res)
                        hc[0] += 1

        def mixer_moba(l):
            barrier()
            L = layout_attn()
            a = L["alloc"]
            L["GT"] = a.get([128, 4, GT_W], BF16)
            L["STG"] = [a.get([128, 512], F32) for _ in range(2)]
            kmean = a.get([128, 2, 16], F32)
            kmh = a.get([64, 4, 16], F32)
            qf = a.get([64, 4, 512], F32)
            g16 = a.get([128, 4, 16], F32)
            top8 = a.get([128, 4, 8], F32)
            mb = a.get([128, 4, 32], BF16)
            init_V_ones(L)
            dma(POOL, L["GT"].ap, dr["gt"], ph([]), [L["GT"].res], L["GT"].res)
            P.op(POOL, lambda e, o=mb.ap: e.memset(o, 0.0), ph([]), [mb.res])
            for h in range(4):
                P.op(POOL, lambda e, o=L["KT"].ap[64:80, h, :], i=dr["onehot"]: e.dma_start(out=o, in_=i),
                     ph([]), [L["KT_res"][h][nn] for nn in range(NB)], dma_slot=L["KT_res"][h][0])
            c0w = 352 + 768
            kv_pass(L, l, c0w + 256, c0w + 512, kmean=kmean)
            for h in range(4):
                cp(DVE, kmh.ap[:, h, :], kmean.ap[64 * (h % 2):64 * (h % 2) + 64, h // 2, :], [kmean.res], [kmh.res])
            load_w(wQ, dr["w_in"][l][:, c0w:c0w + 256], 256)
            load_w(wQ, dr["w_in"][l][:, 2912:3168], 256, col0=256)
            pend, finq, cnt, hc = [], [], [0], [0]
            for n in range(NB):
                drain(pend, finq)
                qi = n % 2
                q_pass(L, n, qi, 0.125, qf=qf)
                gates_for_block(L, wQ, 256, n, qi)
                for tl in range(4):
                    T = 4 * n + tl
                    ob_ = T // 2
                    bg = bW.next()
                    for h in range(4):
                        mm(bg.f32[:, 16 * h:16 * h + 16], qf.ap[:, h, tl * 128:(tl + 1) * 128], kmh.ap[:, h, :], True, True,
                           [qf.res, kmh.res], [bg.res])
                    tt(DVE, g16.ap, bg.f32[:, 0:64].rearrange("p (h b) -> p h b", h=4),
                       addm.ap[:, ob_, :].unsqueeze(1).to_broadcast([128, 4, 16]), ALU.add, [bg.res, addm.res], [g16.res])
                    for h in range(4):
                        P.op(DVE, lambda e, o=top8.ap[:, h, :], i=g16.ap[:, h, :]: e.max(o, i), ph([g16.res]), [top8.res])
                    for h in range(4):
                        ts(DVE, mb.ap[:, h, 0:16], g16.ap[:, h, :], top8.ap[:, h, 3:4], -30000.0, ALU.is_lt, ALU.mult,
                           [g16.res, top8.res], [mb.res])
                    bt = bW.next()
                    P.op(PE, lambda e, o=bt.bf[:, 0:128], i_=mb.ap.rearrange("p h b -> p (h b)"): e.transpose(o, i_, ident.ap[:]),
                         ph([mb.res, ident.res]), [bt.res])
                    for h in range(4):
                        cp(DVE, L["QT"][qi].ap[64:80, h, tl * 128:(tl + 1) * 128], bt.bf[32 * h:32 * h + 16, 0:128],
                           [bt.res], [L["QT_res"][qi][h]])
                for h in range(4):
                    ob = attn_block(L, n, h, qi, 80, "moba", cnt, pend, finq)
                    finq.append(lambda ob=ob, n=n, h=h, qi=qi, c=hc[0]: finish_head(L, ob, n, h, qi, 512 + 64 * h, c))
                    hc[0] += 1
            drain(pend, finq)

        def mixer_conv(l):
            barrier()
            a = Alloc()
            PADW = 32
            xg = a.get([128, 2, PADW + S], BF16)
            r_xg = [[Res() for _ in range(NB)] for _ in range(2)]
            dg = a.get([128, 2, 31, 128], BF16)
            dwT = a.get([128, 2, 31], F32)
            vb = {k: a.get([128, 2], F32) for k in ("dw_b", "ln_g", "ln_b", "pw_b")}
            pw = a.get([128, 2, 256], BF16)
            sig = [a.get([128, 512], F32) for _ in range(2)]
            yc = a.get([128, 2, 512], F32)
            ysq = a.get([128, 2, 512], F32)
            mean = a.get([128, 512], F32)
            rstd = a.get([128, 512], F32)
            msq = a.get([128, 512], F32)
            sw = a.get([128, 2, 512], BF16)
            ee = a.get([128, 512], F32)
            sg = [a.get([128, 512], F32) for _ in range(2)]
            yo = [a.get([128, 512], BF16) for _ in range(2)]
            identf = a.get([128, 128], F32)
            c0w = 352 + 1536
            load_w(wK, dr["w_in"][l][:, c0w:c0w + 512], 512)
            load_w(wQ, dr["w_in"][l][:, 3168:3424], 256)
            dma(SP, dwT.ap, dr["dw_wT"][l], ph([]), [dwT.res], dwT.res)
            for k_, b_ in vb.items():
                dma(SP, b_.ap, dr[k_][l], ph([]), [b_.res], b_.res)
            dma(SP, identf.ap, dr["ident"], ph([]), [identf.res], identf.res)
            P.op(POOL, lambda e, o=pw.ap, i=dr["pw_w"][l].rearrange("(c p) n -> p c n", p=128): e.dma_start(out=o, in_=i),
                 ph([]), [pw.res], dma_slot=pw.res)
            ts(DVE, pw.ap, pw.ap, 0.25, None, ALU.mult, None, [pw.res], [pw.res])
            ts(DVE, vb["pw_b"].ap, vb["pw_b"].ap, 0.5, None, ALU.mult, None, [vb["pw_b"].res], [vb["pw_b"].res])
            for c in range(2):
                for j in range(31):
                    ts(POOL if j % 2 else DVE, dg.ap[:, c, j, :], identf.ap, dwT.ap[:, c, j:j + 1], 0.5, ALU.mult, ALU.mult,
                       [identf.res, dwT.res], [dg.res])
                P.op(POOL, lambda e, o=xg.ap[:, c, 0:PADW]: e.memset(o, 0.0), ph([]), [r_xg[c][0]])
            for n in range(NB):
                for c in range(2):
                    ba, bg_ = bW.next(), bW.next()
                    inproj_fm(wK, 128 * c, 128, n, ba)
                    inproj_fm(wK, 256 + 128 * c, 128, n, bg_)
                    s_ = sig[c]
                    act(s_.ap, bg_.f32, AF.Tanh, [bg_.res], [s_.res], scale=0.5)
                    P.op(DVE, lambda e, o=xg.ap[:, c, PADW + n * 512:PADW + (n + 1) * 512], a_=s_.ap, b_=ba.f32:
                         e.scalar_tensor_tensor(o, a_, 1.0, b_, ALU.add, ALU.mult), ph([s_.res, ba.res]), [r_xg[c][n]])
            for n in range(NB):
                for c in range(2):
                    bc = bW.next()
                    rd = [dg.res, r_xg[c][n]] + ([r_xg[c][n - 1]] if n > 0 else [])
                    for j in range(31):
                        st0 = PADW + n * 512 - 30 + j
                        mm(bc.f32, dg.ap[:, c, j, :], xg.ap[:, c, st0:st0 + 512], j == 0, j == 30, rd, [bc.res])
                    ts(DVE, yc.ap[:, c, :], bc.f32, vb["dw_b"].ap[:, c:c + 1], None, ALU.add, None, [bc.res, vb["dw_b"].res], [yc.res])
                    act(ysq.ap[:, c, :], yc.ap[:, c, :], AF.Square, [yc.res], [ysq.res])
                bm, bq = bW.next(), bW.next()
                mm(bm.f32, ones.ap[:], yc.ap[:, 0, :], True, False, [ones.res, yc.res], [bm.res])
                mm(bm.f32, ones.ap[:], yc.ap[:, 1, :], False, True, [ones.res, yc.res], [bm.res])
                mm(bq.f32, ones.ap[:], ysq.ap[:, 0, :], True, False, [ones.res, ysq.res], [bq.res])
                mm(bq.f32, ones.ap[:], ysq.ap[:, 1, :], False, True, [ones.res, ysq.res], [bq.res])
                ts(DVE, mean.ap, bm.f32, 1.0 / 256, None, ALU.mult, None, [bm.res], [mean.res])
                tt(DVE, msq.ap, mean.ap, mean.ap, ALU.mult, [mean.res], [msq.res])
                P.op(DVE, lambda e, o=msq.ap, a_=bq.f32, b_=msq.ap: e.scalar_tensor_tensor(o, a_, 1.0 / 256, b_, ALU.mult, ALU.subtract),
                     ph([bq.res, msq.res]), [msq.res])
                act(rstd.ap, msq.ap, AF.Ln, [msq.res], [rstd.res], bias=EPS)
                act(rstd.ap, rstd.ap, AF.Exp, [rstd.res], [rstd.res], scale=-0.5)
                for c in range(2):
                    tt(DVE, yc.ap[:, c, :], yc.ap[:, c, :], mean.ap, ALU.subtract, [yc.res, mean.res], [yc.res])
                    tt(DVE, yc.ap[:, c, :], yc.ap[:, c, :], rstd.ap, ALU.mult, [yc.res, rstd.res], [yc.res])
                    ts(DVE, yc.ap[:, c, :], yc.ap[:, c, :], vb["ln_g"].ap[:, c:c + 1], vb["ln_b"].ap[:, c:c + 1], ALU.mult, ALU.add,
                       [yc.res, vb["ln_g"].res, vb["ln_b"].res], [yc.res])
                    act(ee.ap, yc.ap[:, c, :], AF.Tanh, [yc.res], [ee.res], scale=0.5)
                    P.op(DVE, lambda e, o=sw.ap[:, c, :], a_=ee.ap, b_=yc.ap[:, c, :]:
                         e.scalar_tensor_tensor(o, a_, 1.0, b_, ALU.add, ALU.mult), ph([ee.res, yc.res]), [sw.res])
                for c in range(2):
                    bgt = bW.next()
                    inproj_fm(wQ, 128 * c, 128, n, bgt)
                    g_ = sg[c]
                    act(g_.ap, bgt.f32, AF.Tanh, [bgt.res], [g_.res], scale=0.5)
                    P.op(DVE, lambda e, o=g_.ap, a_=g_.ap, b_=bgt.f32: e.scalar_tensor_tensor(o, a_, 1.0, b_, ALU.add, ALU.mult),
                         ph([g_.res, bgt.res]), [g_.res])
                    bp = bW.next()
                    mm(bp.f32, pw.ap[:, 0, 128 * c:128 * c + 128], sw.ap[:, 0, :], True, False, [pw.res, sw.res], [bp.res])
                    mm(bp.f32, pw.ap[:, 1, 128 * c:128 * c + 128], sw.ap[:, 1, :], False, True, [pw.res, sw.res], [bp.res])
                    y_ = yo[c]
                    P.op(DVE, lambda e, o=y_.ap, a_=bp.f32, s1=vb["pw_b"].ap[:, c:c + 1], b_=g_.ap:
                         e.scalar_tensor_tensor(o, a_, s1, b_, ALU.add, ALU.mult), ph([bp.res, vb["pw_b"].res, g_.res]), [y_.res])
                    row0 = 768 + 128 * c
                    dma(SP, yT_h[row0:row0 + 128, n * 512:(n + 1) * 512], y_.ap, ph([y_.res]), [r_yT[row0 // 128][n]], y_.res)

        MIX = {"mla": mixer_mla, "sb": mixer_sb, "moba": mixer_moba, "conv": mixer_conv}
        phase_A0()
        fin = []
        for l in range(n_layers):
            for m in MIX_ALL:
                if m in mixers:
                    MIX[m](l)
            fin = phase_C(l, last=(l == n_layers - 1))
        barrier()
        last_bar = P.ops[DVE][-1]
        P.emit(final_waits=[(POOL, d) for d in fin[-2:]] + [(SP, last_bar)])
    return nc


_CACHE = {}


def kernel(**inputs):
    inp = {k: np.asarray(v, dtype=np.float32) for k, v in inputs.items()}
    w = host_layout(inp)
    if "nc" not in _CACHE:
        _CACHE["nc"] = build()
    nc = _CACHE["nc"]
    x = inp["x"]
    in_maps = []
    for c in range(8):
        m = {"x": np.ascontiguousarray(x[c])}
        m.update(w)
        in_maps.append(m)
    res = run_bass_kernel_spmd(nc, in_maps, core_ids=list(range(8)))
    return np.stack([np.asarray(r["out"], dtype=np.float32) for r in res.results], axis=0)
```

```python
import contextlib
import math
import numpy as np
import concourse.bass as bass
import concourse.mybir as mybir
from concourse.bass_utils import run_bass_kernel_spmd

F32 = mybir.dt.float32
BF16 = mybir.dt.bfloat16
F32R = mybir.dt.float32r
ALU = mybir.AluOpType
AF = mybir.ActivationFunctionType
AX = mybir.AxisListType

S = 4096
D = 1024
NL = 4
NT = 32
NB = 8
EPS = 1e-6
DBG_STOP = 99
DBG_SKIP = ''
SB_WINDOW_TILES = 2

PE, ACT, DVE, POOL, SP = "pe", "act", "dve", "pool", "sp"
COMPUTE = (PE, ACT, DVE, POOL)


class Res:
    __slots__ = ("name", "writer", "readers", "slot", "excl", "persist")

    def __init__(self, name="", excl=False, persist=False):
        self.name = name
        self.excl = excl
        self.persist = persist
        self.writer = None
        self.readers = []
        self.slot = None


class SemSlot:
    __slots__ = ("sem", "count", "sw")

    def __init__(self):
        self.sem = None
        self.count = 0
        self.sw = False


class Op:
    __slots__ = ("eng", "idx", "fn", "waits", "signal", "count", "is_dma", "dma_res",
                 "dma_count", "n_dma")

    def __init__(self, eng, idx, fn):
        self.eng = eng
        self.idx = idx
        self.fn = fn
        self.waits = []
        self.signal = False
        self.count = 0
        self.is_dma = False
        self.dma_res = None
        self.dma_count = 0
        self.n_dma = 0


class Prog:
    def __init__(self, nc):
        self.nc = nc
        self.ops = {e: [] for e in (PE, ACT, DVE, POOL, SP)}
        self.seen = {e: {} for e in self.ops}
        self.slots = []
        self.free_slots = {True: [], False: []}
        self.phase_res = []

    def release_phase_slots(self):
        for r in self.phase_res:
            if r.slot is not None:
                self.free_slots[r.slot.sw].append(r.slot)
                r.slot = None
        self.phase_res = []

    def op(self, eng, fn, reads=(), writes=(), dma_slot=None, n_dma=1):
        lst = self.ops[eng]
        o = Op(eng, len(lst), fn)
        ex = [r for r in reads if r.excl and r not in writes]
        if ex:
            writes = list(writes) + ex
        if dma_slot is not None:
            sw = (eng == POOL)
            if dma_slot.slot is not None and dma_slot.slot.sw != sw:
                dma_slot.slot = None
            if dma_slot.slot is None:
                if self.free_slots[sw]:
                    dma_slot.slot = self.free_slots[sw].pop()
                else:
                    dma_slot.slot = SemSlot()
                    dma_slot.slot.sw = sw
                    self.slots.append(dma_slot.slot)
                if not dma_slot.persist:
                    self.phase_res.append(dma_slot)
            o.is_dma = True
            o.dma_res = dma_slot.slot
            o.n_dma = n_dma
            dma_slot.slot.count += 16 * n_dma
            o.dma_count = dma_slot.slot.count
        need = {}

        def same(d):
            return (not d.is_dma) and (not o.is_dma) and d.eng == eng

        def add(d):
            if d is None or d is o:
                return
            if d.is_dma:
                key, val = ("dma", id(d.dma_res)), d.dma_count
            else:
                key, val = ("eng", d.eng), d.idx
            if key not in need or val > need[key][0]:
                need[key] = (val, d)

        for r in reads:
            d = r.writer
            if d is not None and not (same(d) and eng == PE):
                add(d)
        for w in writes:
            d = w.writer
            if d is not None and not same(d):
                add(d)
            for rd in w.readers:
                if not same(rd):
                    add(rd)
        seen = self.seen[eng]
        for key, (val, d) in need.items():
            if seen.get(key, -1) >= val:
                continue
            seen[key] = val
            o.waits.append(d)
            if not d.is_dma:
                d.signal = True
        for r in reads:
            r.readers.append(o)
        for w in writes:
            w.writer = o
            w.readers = []
        lst.append(o)
        return o

    def emit(self, final_waits=()):
        nc = self.nc
        final = {}
        for (e, d) in final_waits:
            final.setdefault(e, []).append(d)
            if not d.is_dma:
                d.signal = True
        for e in COMPUTE:
            c = 0
            for o in self.ops[e]:
                if o.signal:
                    c += 1
                o.count = c
        with contextlib.ExitStack() as st:
            esem = {e: st.enter_context(nc.semaphore("prog_" + e)) for e in COMPUTE}
            for i, r in enumerate(self.slots):
                r.sem = st.enter_context(nc.semaphore("dma_%d" % i))
            block = st.enter_context(nc.Block())

            def wait(eng, d):
                if d.is_dma:
                    eng.wait_ge(d.dma_res.sem, d.dma_count)
                else:
                    eng.wait_ge(esem[d.eng], d.count)

            def run(e):
                def body(eng):
                    for o in self.ops[e]:
                        for d in o.waits:
                            wait(eng, d)
                        ins = o.fn(eng)
                        if o.is_dma:
                            if not isinstance(ins, (list, tuple)):
                                ins = [ins]
                            assert len(ins) == o.n_dma, (len(ins), o.n_dma)
                            for i_ in ins:
                                i_.then_inc(o.dma_res.sem, 16)
                        elif o.signal:
                            ins.then_inc(esem[e], 1)
                    for d in final.get(e, []):
                        wait(eng, d)
                return body

            block.tensor(run(PE))
            block.scalar(run(ACT))
            block.vector(run(DVE))
            block.gpsimd(run(POOL))
            block.sync(run(SP))


def _t5_bucket(n):
    n = np.maximum(n, 0)
    max_exact = 16
    n_large = np.maximum(n, max_exact).astype(np.float32)
    large = max_exact + (np.log(n_large / max_exact) / math.log(1024 / max_exact) * (32 - max_exact)).astype(np.int32)
    large = np.minimum(large, 31)
    return np.where(n < max_exact, n, large)


GT_W = 1664


def host_consts():
    c = {}
    c["ident"] = np.eye(128, dtype=np.float32)
    p = np.arange(128)[:, None]
    q = np.arange(128)[None, :]
    c["tri_le"] = (p <= q).astype(np.float32)
    c["tri_lt"] = (p < q).astype(np.float32)
    c["tri_gt"] = (p > q).astype(np.float32)
    c["ones"] = np.ones((128, 128), np.float32)
    half = 16
    freqs = (10000.0 ** (-np.arange(half, dtype=np.float32) / half)).astype(np.float32)
    ang = np.arange(S, dtype=np.float32)[:, None] * freqs[None, :]
    cos = np.cos(ang).astype(np.float32).T
    sin = np.sin(ang).astype(np.float32).T
    sc = np.float32(96 ** -0.5)
    rq_c = np.zeros((96, S), np.float32)
    rq_s = np.zeros((96, S), np.float32)
    rq_c[0:64] = sc
    rq_c[64:80] = cos * sc
    rq_c[80:96] = cos * sc
    rq_s[64:80] = -sin * sc
    rq_s[80:96] = sin * sc
    rk_c = np.zeros((96, S), np.float32)
    rk_s = np.zeros((96, S), np.float32)
    rk_c[64:80] = cos
    rk_c[80:96] = cos
    rk_s[64:80] = -sin
    rk_s[80:96] = sin
    c["rq_c"], c["rq_s"], c["rk_c"], c["rk_s"] = rq_c, rq_s, rk_c, rk_s
    oh = np.zeros((16, S), np.float32)
    for b in range(16):
        oh[b, b * 256:(b + 1) * 256] = 1.0
    c["onehot"] = oh
    am = np.zeros((16, 16), np.float32)
    for ob in range(16):
        am[ob, ob] = 1e30
        am[ob, ob + 1:] = -1e30
    c["addm"] = np.ascontiguousarray(np.broadcast_to(am[None], (128, 16, 16)))
    return c


def host_layout(inp):
    w = {}
    w_in = inp["w_in"]
    w["w_in"] = np.ascontiguousarray(w_in)
    wk = np.zeros((NL, D, 320), np.float32)
    wk[:, :, 0:128] = w_in[:, :, 192:320]
    wk[:, :, 192:224] = w_in[:, :, 320:352]
    wk[:, :, 288:304] = w_in[:, :, 336:352]
    wk[:, :, 304:320] = w_in[:, :, 320:336]
    w["w_mla_k"] = wk
    w["gpre"] = np.ascontiguousarray(inp["pre_norm_g"].reshape(NL, 8, 128).transpose(0, 2, 1))
    gq = np.zeros((NL, 128, 2), np.float32)
    gq[:, :, 0] = inp["mla_q_norm_g"][:, 0:128]
    gq[:, 0:64, 1] = inp["mla_q_norm_g"][:, 128:192]
    w["gq"] = gq
    uq = inp["mla_w_uq"].reshape(NL, 192, 4, 96)
    uq_p = np.zeros((NL, 128, 2, 4, 96), np.float32)
    uq_p[:, :, 0] = uq[:, 0:128]
    uq_p[:, 0:64, 1] = uq[:, 128:192]
    w["w_uq"] = uq_p
    sw = np.zeros((NL, 192, 4, 96), np.float32)
    sw[:, :, :, 64:80] = uq[:, :, :, 80:96]
    sw[:, :, :, 80:96] = uq[:, :, :, 64:80]
    sw_p = np.zeros((NL, 128, 2, 4, 96), np.float32)
    sw_p[:, :, 0] = sw[:, 0:128]
    sw_p[:, 0:64, 1] = sw[:, 128:192]
    w["w_uq_sw"] = sw_p
    w["gkv"] = np.ascontiguousarray(inp["mla_kv_norm_g"].reshape(NL, 128, 1))
    ukv = inp["mla_w_ukv"].reshape(NL, 128, 4, 128)
    w["w_uk"] = np.ascontiguousarray(ukv[:, :, :, 0:64])
    w["w_uv"] = np.ascontiguousarray(ukv[:, :, :, 64:128].reshape(NL, 128, 256))
    rb = inp["rel_bias"]
    pp = np.arange(128)[:, None]
    xx = np.arange(GT_W)[None, :]
    bucket = _t5_bucket(xx - pp)
    w["gt"] = np.ascontiguousarray(rb[bucket].transpose(0, 2, 1))
    w["c31"] = np.ascontiguousarray(np.broadcast_to(rb[31][None, :], (128, 4)))
    w["dw_wT"] = np.ascontiguousarray(inp["conv_dw_w"].transpose(0, 2, 1).reshape(NL, 2, 128, 31).transpose(0, 2, 1, 3))

    def pv(a):
        return np.ascontiguousarray(a.reshape(NL, 2, 128).transpose(0, 2, 1))
    w["dw_b"] = pv(inp["conv_dw_b"])
    w["ln_g"] = pv(inp["conv_ln_g"])
    w["ln_b"] = pv(inp["conv_ln_b"])
    w["pw_b"] = pv(inp["conv_pw_b"])
    w["pw_w"] = np.ascontiguousarray(inp["conv_pw_w"])
    w["w_out"] = np.ascontiguousarray(inp["w_out"])
    w["post_g"] = np.ascontiguousarray(inp["post_norm_g"])
    w.update(host_consts())
    return w


WSHAPES = {
    "w_in": [NL, D, 3424], "w_mla_k": [NL, D, 320], "gpre": [NL, 128, 8], "gq": [NL, 128, 2],
    "w_uq": [NL, 128, 2, 4, 96], "w_uq_sw": [NL, 128, 2, 4, 96], "gkv": [NL, 128, 1],
    "w_uk": [NL, 128, 4, 64], "w_uv": [NL, 128, 256], "gt": [128, 4, GT_W], "c31": [128, 4],
    "dw_wT": [NL, 128, 2, 31], "dw_b": [NL, 128, 2], "ln_g": [NL, 128, 2], "ln_b": [NL, 128, 2],
    "pw_b": [NL, 128, 2], "pw_w": [NL, 256, 256], "w_out": [NL, D, D], "post_g": [NL, D],
    "ident": [128, 128], "tri_le": [128, 128], "tri_lt": [128, 128], "tri_gt": [128, 128],
    "ones": [128, 128], "rq_c": [96, S], "rq_s": [96, S], "rk_c": [96, S], "rk_s": [96, S],
    "onehot": [16, S], "addm": [128, 16, 16],
}


class Bank:
    def __init__(self, t):
        self.t = t
        self.res = Res(excl=True)
        self.f32 = t[:]
        self.bf = t[:].bitcast(BF16)


class RR:
    def __init__(self, items):
        self.items = items
        self.i = 0

    def next(self):
        it = self.items[self.i % len(self.items)]
        self.i += 1
        return it


class Buf:
    def __init__(self, ap, persist=False):
        self.ap = ap if isinstance(ap, bass.AP) else ap[:]
        self.res = Res(persist=persist)

    @property
    def r(self):
        return self.ap.bitcast(F32R)


ARENA_BYTES = 111104
MIX_ALL = ("mla", "sb", "moba", "conv")


def build(n_layers=NL, mixers=MIX_ALL, dbg=False):
    nc = bass.Bass("TRN2", target_bir_lowering=False)
    P = Prog(nc)
    dr = {}
    x_in = nc.dram_tensor("x", [S, D], F32, kind="ExternalInput").ap()
    for k, shp in WSHAPES.items():
        dr[k] = nc.dram_tensor(k, shp, F32, kind="ExternalInput").ap()
    out = nc.dram_tensor("out", [S, D], F32, kind="ExternalOutput").ap()
    yT_h = nc.dram_tensor("yT", [D, S], BF16, kind="ExternalOutput" if dbg else "Internal").ap()
    r_yT = [[Res() for _ in range(NB)] for _ in range(8)]
    r_xh = [Res() for _ in range(NT)]

    st = contextlib.ExitStack()
    with st:
        def sb(name, shape, dt):
            return st.enter_context(nc.sbuf_tensor("s_" + name, shape, dt))

        hT = sb("hT", [128, 8, S], BF16)
        r_hT = [Res() for _ in range(NT)]
        wK = Buf(sb("wK", [128, 8, 512], BF16), persist=True)
        wQ = Buf(sb("wQ", [128, 8, 512], BF16), persist=True)
        ident = Buf(sb("ident", [128, 128], BF16), persist=True)
        tri_le = Buf(sb("tri_le", [128, 128], BF16), persist=True)
        tri_lt = Buf(sb("tri_lt", [128, 128], BF16), persist=True)
        tri_gt = Buf(sb("tri_gt", [128, 128], F32), persist=True)
        tri_lef = Buf(sb("tri_lef", [128, 128], F32), persist=True)
        ones = Buf(sb("ones", [128, 128], F32), persist=True)
        gpre = Buf(sb("gpre", [128, NL, 8], F32), persist=True)
        c31 = Buf(sb("c31", [128, 4], F32), persist=True)
        zeros_bf = Buf(sb("zeros_bf", [128, 512], BF16), persist=True)
        addm = Buf(sb("addm", [128, 16, 16], F32), persist=True)
        LNR = [Buf(sb("lnr%d" % i, [128, 512], F32), persist=True) for i in range(4)]
        arena_t = sb("arena", [128, ARENA_BYTES // 4], F32)
        r_phase = Res("phase")
        dummy = Buf(sb("dummy", [128, 8], F32), persist=True)

        def view(off, shape, dt):
            isz = 2 if dt == BF16 else 4
            n = 1
            for s_ in shape[1:]:
                n *= s_
            nb = n * isz
            assert off % 4 == 0 and nb % 4 == 0 and off + nb <= ARENA_BYTES, (off, nb)
            ap = arena_t[:, off // 4:(off + nb) // 4]
            if dt == BF16:
                ap = ap.bitcast(BF16)
            if len(shape) == 3:
                ap = ap.rearrange("p (a b) -> p a b", a=shape[1])
            elif len(shape) == 4:
                ap = ap.rearrange("p (a b c) -> p a b c", a=shape[1], b=shape[2])
            if shape[0] != 128:
                ap = ap[0:shape[0]]
            return ap

        class Alloc:
            def __init__(self, start=0):
                self.off = start

            def get(self, shape, dt):
                isz = 2 if dt == BF16 else 4
                n = 1
                for s_ in shape[1:]:
                    n *= s_
                nb = (n * isz + 31) // 32 * 32
                v = view(self.off, shape, dt)
                b_ = Buf(v)
                b_.off = self.off
                self.off += nb
                return b_

        banks = [Bank(st.enter_context(nc.psum_tensor("bank%d" % i, [128, 512], F32))) for i in range(8)]
        bS = RR(banks[0:3])
        bO = RR(banks[3:5])
        bW = RR(banks[5:8])

        def dma(q, out_ap, in_ap, reads, writes, slot):
            return P.op(q, lambda e, o=out_ap, i=in_ap: e.dma_start(out=o, in_=i), reads, writes, dma_slot=slot)

        def barrier():
            P.op(DVE, lambda e: e.memset(dummy.ap, 0.0), reads=[], writes=[r_phase, dummy.res])
            P.release_phase_slots()

        def ph(reads):
            return list(reads) + [r_phase]

        def mm(o_ap, lhsT, rhs, start, stop, reads, writes, **kw):
            return P.op(PE, lambda e, o=o_ap, l=lhsT, r=rhs, s0=start, s1=stop, kw=kw:
                        e.matmul(o, l, r, start=s0, stop=s1, **kw), ph(reads), writes)

        def act(o_ap, i_ap, func, reads, writes, scale=1.0, bias=None, accum=None):
            def f(e, o=o_ap, i=i_ap, fu=func, sc=scale, b=bias, a=accum):
                kw = {}
                if b is not None:
                    kw["bias"] = b
                if a is not None:
                    kw["accum_out"] = a
                return e.activation(o, i, fu, scale=sc, **kw)
            return P.op(ACT, f, ph(reads), writes)

        def tt(eng, o_ap, a, b, op, reads, writes):
            return P.op(eng, lambda e, o=o_ap, a=a, b=b, op=op: e.tensor_tensor(o, a, b, op), ph(reads), writes)

        def ts(eng, o_ap, a, s1, s2, op0, op1, reads, writes):
            if op1 is None:
                return P.op(eng, lambda e, o=o_ap, a=a, s1=s1, op0=op0: e.tensor_scalar(o, a, s1, None, op0), ph(reads), writes)
            return P.op(eng, lambda e, o=o_ap, a=a, s1=s1, s2=s2, op0=op0, op1=op1:
                        e.tensor_scalar(o, a, s1, s2, op0, op1), ph(reads), writes)

        def cp(eng, o_ap, i_ap, reads, writes):
            return P.op(eng, lambda e, o=o_ap, i=i_ap: e.tensor_copy(o, i), ph(reads), writes)

        def rsqrt_act(o_ap, i_ap, n_mean, reads, writes):
            act(o_ap, i_ap, AF.Ln, reads, writes, scale=1.0 / n_mean, bias=EPS)
            act(o_ap, o_ap, AF.Exp, writes, writes, scale=-0.5)

        for (b_, nm) in [(ident, "ident"), (tri_le, "tri_le"), (tri_lt, "tri_lt")]:
            dma(POOL, b_.ap[:], dr[nm], [], [b_.res], b_.res)
        dma(SP, tri_gt.ap[:], dr["tri_gt"], [], [tri_gt.res], tri_gt.res)
        dma(SP, tri_lef.ap[:], dr["tri_le"], [], [tri_lef.res], tri_lef.res)
        dma(SP, ones.ap[:], dr["ones"], [], [ones.res], ones.res)
        dma(SP, gpre.ap[:], dr["gpre"].rearrange("l p c -> p l c"), [], [gpre.res], gpre.res)
        dma(SP, c31.ap[:], dr["c31"], [], [c31.res], c31.res)
        ones_r = Buf(sb("ones_r", [128, 128], F32), persist=True)
        tri_gt_r = Buf(sb("tri_gt_r", [128, 128], F32), persist=True)
        tri_le_r = Buf(sb("tri_le_r", [128, 128], F32), persist=True)
        for (dst_, src_) in [(ones_r, ones), (tri_gt_r, tri_gt), (tri_le_r, tri_lef)]:
            P.op(DVE, lambda e, o=dst_.r, i=src_.ap: e.tensor_copy(o, i), [src_.res], [dst_.res])
        dma(SP, addm.ap[:], dr["addm"], [], [addm.res], addm.res)
        P.op(POOL, lambda e: e.memset(zeros_bf.ap[:], 0.0), [], [zeros_bf.res])

        def load_w(buf, src_ap, ncols, col0=0):
            P.op(POOL, lambda e, o=buf.ap[:, :, col0:col0 + ncols], i=src_ap.rearrange("(c p) n -> p c n", p=128):
                 e.dma_start(out=o, in_=i), [], [buf.res], dma_slot=buf.res)

        def inproj_fm(wbuf, c0, ncols, n, bank, extra_reads=()):
            for kc in range(8):
                mm(bank.f32[0:ncols, :], wbuf.ap[:, kc, c0:c0 + ncols], hT[:, kc, n * 512:(n + 1) * 512],
                   kc == 0, kc == 7, [wbuf.res] + r_hT[4 * n:4 * n + 4] + list(extra_reads), [bank.res])

        def inproj_tm(wbuf, c0, ncols, t, bank):
            for kc in range(8):
                mm(bank.f32[:, 0:ncols], hT[:, kc, t * 128:(t + 1) * 128], wbuf.ap[:, kc, c0:c0 + ncols],
                   kc == 0, kc == 7, [wbuf.res, r_hT[t]], [bank.res])

        def layout_C():
            a = Alloc()
            L = {}
            L["w_out"] = a.get([128, 8, 1024], BF16)
            L["yt"] = [a.get([128, 8, 512], BF16) for _ in range(2)]
            L["x"] = [a.get([128, 1024], F32) for _ in range(2)]
            L["xn"] = [a.get([128, 1024], F32) for _ in range(2)]
            L["xs"] = [a.get([128, 1024], BF16) for _ in range(2)]
            L["junk"] = a.get([128, 1024], BF16)
            L["postg"] = a.get([128, 1024], F32)
            L["st"] = [a.get([128, 8], F32) for _ in range(4)]
            return L

        def norm_transpose(L, l, t, xbuf, i):
            stt = L["st"][i % 4]
            xs = L["xs"][i % 2]
            act(L["junk"].ap, xbuf.ap, AF.Square, [xbuf.res], [L["junk"].res, stt.res], accum=stt.ap[:, 0:1])
            rsqrt_act(stt.ap[:, 1:2], stt.ap[:, 0:1], float(D), [stt.res], [stt.res])
            ts(DVE, xs.ap, xbuf.ap, stt.ap[:, 1:2], None, ALU.mult, None, [xbuf.res, stt.res], [xs.res])
            bk = bW.next()
            for c in range(8):
                P.op(PE, lambda e, o=bk.bf[:, c * 128:(c + 1) * 128], i_=xs.ap[:, c * 128:(c + 1) * 128]:
                     e.transpose(o, i_, ident.ap[:]), ph([xs.res, ident.res]), [bk.res])
            tt(DVE, hT[:, :, t * 128:(t + 1) * 128], bk.bf[:, 0:1024].rearrange("p (c t) -> p c t", c=8),
               gpre.ap[:, l, :].unsqueeze(2).to_broadcast([128, 8, 128]), ALU.mult,
               [bk.res, gpre.res], [r_hT[t]])

        def phase_A0():
            barrier()
            L = layout_C()
            for t in range(NT):
                xb = L["x"][t % 2]
                dma(SP, xb.ap, x_in[t * 128:(t + 1) * 128, :], ph([]), [xb.res], xb.res)
                norm_transpose(L, 0, t, xb, t)

        def phase_C(l, last):
            barrier()
            L = layout_C()
            src = x_in if l == 0 else out
            for half in range(2):
                P.op(POOL, lambda e, o=L["w_out"].ap[:, :, half * 512:(half + 1) * 512],
                     i=dr["w_out"][l][:, half * 512:(half + 1) * 512].rearrange("(c p) n -> p c n", p=128):
                     e.dma_start(out=o, in_=i), ph([]), [L["w_out"].res], dma_slot=L["w_out"].res)
            dma(SP, L["postg"].ap, dr["post_g"][l:l + 1, :].to_broadcast([128, D]), ph([]), [L["postg"].res], L["postg"].res)
            fin = []
            pend_store = []
            for n in range(NB):
                yt = L["yt"][n % 2]
                P.op(SP, lambda e, o=yt.ap, i=yT_h[:, n * 512:(n + 1) * 512].rearrange("(c p) t -> p c t", p=128):
                     e.dma_start(out=o, in_=i), ph([r_yT[c][n] for c in range(8)]), [yt.res], dma_slot=yt.res)
                for tl in range(4):
                    t = 4 * n + tl
                    i = t
                    xb = L["x"][i % 2]
                    xn = L["xn"][i % 2]
                    stt = L["st"][(i + 2) % 4]
                    dma(SP, xb.ap, src[t * 128:(t + 1) * 128, :], ph([r_xh[t]]), [xb.res], xb.res)
                    bk2 = [bW.next(), bW.next()]
                    for hf in range(2):
                        for kc in range(8):
                            mm(bk2[hf].f32, yt.ap[:, kc, tl * 128:(tl + 1) * 128], L["w_out"].ap[:, kc, hf * 512:(hf + 1) * 512],
                               kc == 0, kc == 7, [yt.res, L["w_out"].res], [bk2[hf].res])
                    for hf in range(2):
                        act(L["junk"].ap[:, hf * 512:(hf + 1) * 512], bk2[hf].f32, AF.Square, [bk2[hf].res],
                            [L["junk"].res, stt.res], accum=stt.ap[:, 2 + hf:3 + hf])
                    tt(DVE, stt.ap[:, 4:5], stt.ap[:, 2:3], stt.ap[:, 3:4], ALU.add, [stt.res], [stt.res])
                    rsqrt_act(stt.ap[:, 5:6], stt.ap[:, 4:5], float(D), [stt.res], [stt.res])
                    for hf in range(2):
                        sl = slice(hf * 512, (hf + 1) * 512)
                        P.op(DVE, lambda e, o=xn.ap[:, sl], a=bk2[hf].f32, s_=stt.ap[:, 5:6], b=L["postg"].ap[:, sl]:
                             e.scalar_tensor_tensor(o, a, s_, b, ALU.mult, ALU.mult),
                             ph([bk2[hf].res, stt.res, L["postg"].res]), [xn.res])
                    tt(POOL, xn.ap, xn.ap, xb.ap, ALU.add, [xn.res, xb.res], [xn.res])
                    if pend_store:
                        fin.append(pend_store.pop()())
                    pend_store.append(lambda t=t, xn=xn: dma(SP, out[t * 128:(t + 1) * 128, :], xn.ap, ph([xn.res]), [r_xh[t]], xn.res))
                    if not last:
                        norm_transpose(L, l + 1, t, xn, i)
            fin.append(pend_store.pop()())
            return fin

        def layout_attn():
            a = Alloc()
            L = {}
            L["KT"] = a.get([128, 4, S], BF16)
            L["KT_res"] = [[Res() for _ in range(NB)] for _ in range(4)]
            L["V"] = a.get([128, NT, 4, 65], BF16)
            L["V_res"] = [Res() for _ in range(NT)]
            L["QT"] = [a.get([128, 4, 512], BF16) for _ in range(2)]
            L["QT_res"] = [[Res() for _ in range(4)] for _ in range(2)]
            L["PT"] = [a.get([128, 512], BF16) for _ in range(4)]
            L["YST"] = [a.get([64, 512], BF16) for _ in range(2)]
            sg_ = a.get([64, 4, 512], F32)
            L["SG"] = [sg_, sg_]
            sgr_ = [Res() for _ in range(4)]
            L["SG_res"] = [sgr_, sgr_]
            L["RD"] = [a.get([64, 512], F32) for _ in range(2)]
            L["DEN"] = [LNR[0], LNR[1]]
            L["GE"] = [a.get([64, 512], F32) for _ in range(2)]
            L["alloc"] = a
            return L

        def v_lhsT(L, t, h):
            return L["V"].ap[:, t, h, :]

        def init_V_ones(L):
            P.op(POOL, lambda e, o=L["V"].ap[:, :, :, 64:65]: e.memset(o, 2.0), ph([]), list(L["V_res"]))

        def gates_for_pair(L, wbuf, c0, n, c):
            bk = bW.next()
            inproj_fm(wbuf, c0 + 128 * c, 128, n, bk)
            for hh in range(2):
                h = 2 * c + hh
                ge = L["GE"][h % 2]
                sg = L["SG"][0].ap[:, h, :]
                r_sg = L["SG_res"][0][h]
                src = bk.f32[64 * hh:64 * hh + 64, :]
                act(ge.ap, src, AF.Tanh, [bk.res], [ge.res], scale=0.5)
                P.op(DVE, lambda e, o=sg, a_=ge.ap, b_=src: e.scalar_tensor_tensor(o, a_, 1.0, b_, ALU.add, ALU.mult),
                     ph([ge.res, bk.res]), [r_sg])

        def gates_for_block(L, wbuf, c0, n, qi):
            for c in range(2):
                gates_for_pair(L, wbuf, c0, n, c)

        def finish_head(L, ob, n, h, qi, row0, cnt):
            rd = L["RD"][cnt % 2]
            ys = L["YST"][cnt % 2]
            den = L["DEN"][cnt % 2]
            act(rd.ap[0:1, :], ob.f32[64:65, :], AF.Ln, [ob.res], [rd.res])
            act(den.r[64:65, :], rd.ap[0:1, :], AF.Exp, [rd.res], [den.res], scale=-1.0)
            bb = bW.next()
            mm(bb.f32[0:64, :], ones_r.r[64:65, 0:64], den.r[64:65, :], True, True, [ones_r.res, den.res], [bb.res])
            tt(DVE, rd.ap, bb.f32[0:64, :], L["SG"][qi].ap[:, h, :], ALU.mult, [bb.res, L["SG_res"][qi][h]], [rd.res])
            tt(DVE, ys.ap, ob.f32[0:64, :], rd.ap, ALU.mult, [ob.res, rd.res], [ys.res])
            c, r0 = row0 // 128, row0 % 128
            dma(SP, yT_h[row0:row0 + 64, n * 512:(n + 1) * 512], ys.ap, ph([ys.res]), [r_yT[c][n]], ys.res)

        def attn_block(L, n, h, qi, dk, kind, cnt, pend, finq):
            ob = bO.next()
            qt = L["QT"][qi].ap[0:dk, h, :]
            r_q = L["QT_res"][qi][h]
            tiles = [(4 * n + r, r) for r in range(4)] + [(j, -1) for j in range(4 * n)]
            ntl = len(tiles)
            for idx, (j, r) in enumerate(tiles):
                c0 = 128 * r if r > 0 else 0
                sbk = bS.next()
                pt = L["PT"][(cnt[0]) % 4]
                cnt[0] += 1
                mm(sbk.f32[:, c0:512], L["KT"].ap[0:dk, h, j * 128:(j + 1) * 128], qt[:, c0:512], True, True,
                   [L["KT_res"][h][j // 4], r_q], [sbk.res])
                if kind == "moba" and (4 * n - j) <= 9:
                    stg = L["STG"][cnt[0] % 2]
                    dq = 512 * n - 128 * j
                    tt(DVE, stg.ap[:, c0:512], sbk.f32[:, c0:512], L["GT"].ap[:, h, dq + c0:dq + 512], ALU.add,
                       [sbk.res, L["GT"].res], [stg.res])
                    act(pt.ap[:, c0:512], stg.ap[:, c0:512], AF.Exp, [stg.res], [pt.res])
                elif kind == "moba":
                    act(pt.ap[:, c0:512], sbk.f32[:, c0:512], AF.Exp, [sbk.res, c31.res], [pt.res], bias=c31.ap[:, h:h + 1])
                else:
                    act(pt.ap[:, c0:512], sbk.f32[:, c0:512], AF.Exp, [sbk.res], [pt.res])
                if r >= 0:
                    tt(POOL, pt.ap[:, c0:c0 + 128], pt.ap[:, c0:c0 + 128], tri_le.ap[:], ALU.mult, [pt.res, tri_le.res], [pt.res])
                pend.append((ob, v_lhsT(L, j, h), pt, c0, idx == 0, idx == ntl - 1, L["V_res"][j]))
                if len(pend) > 2:
                    flush_pv(pend)
                    while finq and all(finq[0][0] is not p_[0] for p_ in pend):
                        finq.pop(0)[1]()
            return ob

        def flush_pv(pend):
            (ob, lhsT, pt, c0, s0, s1, r_v) = pend.pop(0)
            mm(ob.f32[0:65, c0:512], lhsT, pt.ap[:, c0:512], s0, s1, [pt.res, r_v], [ob.res], skip_group_check=True)

        def drain(pend, finq):
            while pend:
                flush_pv(pend)
            while finq:
                finq.pop(0)[1]()

        def mixer_mla(l):
            barrier()
            L = layout_attn()
            a = L["alloc"]
            ckv = a.get([128, 512], BF16)
            sq = a.get([128, 2, 512], F32)
            rq = a.get([128, 512], F32)
            rkv = rq
            cq = a.get([128, 2, 512], BF16)
            tq = [a.get([96, 512], F32) for _ in range(2)]
            tk = tq
            csq = [a.get([96, 512], F32) for _ in range(2)]
            t12 = [a.get([96, 512], F32) for _ in range(2)]
            rtm = a.get([128, 8], F32)
            wuq = a.get([128, 2, 4, 96], BF16)
            wsw = a.get([128, 2, 4, 96], BF16)
            wuk = a.get([128, 4, 64], BF16)
            wuv = a.get([128, 256], BF16)
            wtmp = Buf(view(sq.off, [128, 2, 4, 96], F32)); wtmp.res = sq.res
            wtmp2 = Buf(view(tq[0].off, [128, 2, 4, 96], F32)); wtmp2.res = tq[0].res; wtmp2.res2 = tq[1].res
            wtmp3 = Buf(view(csq[0].off, [128, 4, 64], F32)); wtmp3.res = csq[0].res
            wtmp4 = Buf(view(csq[1].off, [128, 256], F32)); wtmp4.res = csq[1].res
            gq = a.get([128, 2], F32)
            gkv = a.get([128, 1], F32)
            init_V_ones(L)
            load_w(wK, dr["w_mla_k"][l], 320)
            load_w(wQ, dr["w_in"][l][:, 0:192], 192)
            load_w(wQ, dr["w_in"][l][:, 2400:2656], 256, col0=192)
            dma(SP, gq.ap, dr["gq"][l], ph([]), [gq.res], gq.res)
            dma(SP, gkv.ap, dr["gkv"][l], ph([]), [gkv.res], gkv.res)
            dma(SP, wtmp.ap, dr["w_uq"][l], ph([]), [wtmp.res], wtmp.res)
            dma(SP, wtmp2.ap, dr["w_uq_sw"][l], ph([]), [wtmp2.res, wtmp2.res2], wtmp2.res)
            dma(SP, wtmp3.ap, dr["w_uk"][l], ph([]), [wtmp3.res], wtmp3.res)
            dma(SP, wtmp4.ap, dr["w_uv"][l], ph([]), [wtmp4.res], wtmp4.res)
            for c in range(2):
                ts(DVE, wuq.ap[:, c], wtmp.ap[:, c], gq.ap[:, c:c + 1], None, ALU.mult, None, [wtmp.res, gq.res], [wuq.res])
                ts(DVE, wsw.ap[:, c], wtmp2.ap[:, c], gq.ap[:, c:c + 1], None, ALU.mult, None, [wtmp2.res, wtmp2.res2, gq.res], [wsw.res])
            ts(DVE, wuk.ap, wtmp3.ap, gkv.ap[:, 0:1], None, ALU.mult, None, [wtmp3.res, gkv.res], [wuk.res])
            ts(DVE, wuv.ap, wtmp4.ap, gkv.ap[:, 0:1], None, ALU.mult, None, [wtmp4.res, gkv.res], [wuv.res])
            if DBG_STOP <= 1:
                return
            for n in range(NB):
                if DBG_STOP <= 2 and n >= 1:
                    return
                tkc, tks = tk[0], tk[1]
                blk = slice(n * 512, (n + 1) * 512)
                dma(SP, tkc.ap[64:96, :], dr["rk_c"][64:96, blk], ph([]), [tkc.res], tkc.res)
                dma(SP, tks.ap[64:96, :], dr["rk_s"][64:96, blk], ph([]), [tks.res], tks.res)
                b_ckv, b_kr, b_ks = bW.next(), bW.next(), bW.next()
                inproj_fm(wK, 0, 128, n, b_ckv)
                inproj_fm(wK, 128, 96, n, b_kr)
                inproj_fm(wK, 224, 96, n, b_ks)
                if DBG_STOP <= 2.1:
                    return
                if 'A' not in DBG_SKIP:
                    cp(DVE, ckv.ap, b_ckv.f32, [b_ckv.res], [ckv.res])
                if 'B' not in DBG_SKIP:
                    act(sq.ap[:, 0, :], b_ckv.f32, AF.Square, [b_ckv.res], [sq.res])
                t1, t2 = t12[0], t12[1]
                if 'C' not in DBG_SKIP:
                    tt(DVE, t1.ap[64:96, :], b_kr.f32[64:96, :], tkc.ap[64:96, :], ALU.mult, [b_kr.res, tkc.res], [t1.res])
                if DBG_STOP <= 2.11:
                    return
                tt(DVE, t2.ap[64:96, :], b_ks.f32[64:96, :], tks.ap[64:96, :], ALU.mult, [b_ks.res, tks.res], [t2.res])
                if DBG_STOP <= 2.12:
                    return
                for h in range(4):
                    if DBG_STOP <= 2.13 and h % 2:
                        continue
                    tt(POOL if h % 2 else DVE, L["KT"].ap[64:96, h, blk], t1.ap[64:96, :], t2.ap[64:96, :], ALU.add,
                       [t1.res, t2.res], [L["KT_res"][h][n]])
                if DBG_STOP <= 2.2:
                    return
                b_ss = bW.next()
                mm(b_ss.f32, ones.ap[:], sq.ap[:, 0, :], True, True, [ones.res, sq.res], [b_ss.res])
                rsqrt_act(rkv.ap, b_ss.f32, 128.0, [b_ss.res], [rkv.res])
                if DBG_STOP <= 2.3:
                    return
                for h in range(4):
                    bk = bW.next()
                    mm(bk.f32[0:64, :], wuk.ap[:, h, :], ckv.ap, True, True, [wuk.res, ckv.res], [bk.res])
                    tt(DVE, L["KT"].ap[0:64, h, blk], bk.f32[0:64, :], rkv.ap[0:64, :], ALU.mult, [bk.res, rkv.res],
                       [L["KT_res"][h][n]])
                if DBG_STOP <= 2.4:
                    return
                b_st = bW.next()
                for tl in range(4):
                    mm(b_st.f32[:, tl:tl + 1], sq.ap[:, 0, tl * 128:(tl + 1) * 128], ones.ap[:, 0:1], True, True,
                       [sq.res, ones.res], [b_st.res])
                if DBG_STOP <= 2.5:
                    return
                rsqrt_act(rtm.ap[:, 0:4], b_st.f32[:, 0:4], 128.0, [b_st.res], [rtm.res])
                for tl in range(4):
                    t = 4 * n + tl
                    bk = bW.next()
                    mm(bk.f32[:, 0:256], ckv.ap[:, tl * 128:(tl + 1) * 128], wuv.ap, True, True, [ckv.res, wuv.res], [bk.res])
                    ts(DVE, L["V"].ap[:, t, :, 0:64], bk.f32[:, 0:256].rearrange("p (h d) -> p h d", h=4), rtm.ap[:, tl:tl + 1], None, ALU.mult, None,
                       [bk.res, rtm.res], [L["V_res"][t]])
            if DBG_STOP <= 3:
                return
            pend, finq, cnt, hc = [], [], [0], [0]

            def mla_qpass(n):
                qi = n % 2
                blk = slice(n * 512, (n + 1) * 512)
                tqc, tqs = tq[0], tq[1]
                dma(SP, tqc.ap, dr["rq_c"][:, blk], ph([]), [tqc.res], tqc.res)
                dma(SP, tqs.ap, dr["rq_s"][:, blk], ph([]), [tqs.res], tqs.res)
                b0, b1 = bW.next(), bW.next()
                inproj_fm(wQ, 0, 128, n, b0)
                inproj_fm(wQ, 128, 64, n, b1)
                cp(DVE, cq.ap[:, 0, :], b0.f32, [b0.res], [cq.res])
                cp(DVE, cq.ap[0:64, 1, :], b1.f32[0:64, :], [b1.res], [cq.res])
                act(sq.ap[:, 0, :], b0.f32, AF.Square, [b0.res], [sq.res])
                act(sq.ap[0:64, 1, :], b1.f32[0:64, :], AF.Square, [b1.res], [sq.res])
                b_ss = bW.next()
                mm(b_ss.f32, ones.ap[:], sq.ap[:, 0, :], True, False, [ones.res, sq.res], [b_ss.res])
                mm(b_ss.f32, ones.ap[0:64, :], sq.ap[0:64, 1, :], False, True, [ones.res, sq.res], [b_ss.res])
                rsqrt_act(rq.ap, b_ss.f32, 192.0, [b_ss.res], [rq.res])
                tt(DVE, csq[0].ap, tqc.ap, rq.ap[0:96, :], ALU.mult, [tqc.res, rq.res], [csq[0].res])
                tt(DVE, csq[1].ap, tqs.ap, rq.ap[0:96, :], ALU.mult, [tqs.res, rq.res], [csq[1].res])
                for h in range(4):
                    bA, bB = bW.next(), bW.next()
                    mm(bA.f32[0:96, :], wuq.ap[:, 0, h, :], cq.ap[:, 0, :], True, False, [wuq.res, cq.res], [bA.res])
                    mm(bA.f32[0:96, :], wuq.ap[0:64, 1, h, :], cq.ap[0:64, 1, :], False, True, [wuq.res, cq.res], [bA.res])
                    mm(bB.f32[0:96, :], wsw.ap[:, 0, h, :], cq.ap[:, 0, :], True, False, [wsw.res, cq.res], [bB.res])
                    mm(bB.f32[0:96, :], wsw.ap[0:64, 1, h, :], cq.ap[0:64, 1, :], False, True, [wsw.res, cq.res], [bB.res])
                    t1, t2 = t12[0], t12[1]
                    tt(DVE, t1.ap, bA.f32[0:96, :], csq[0].ap, ALU.mult, [bA.res, csq[0].res], [t1.res])
                    tt(DVE, t2.ap, bB.f32[0:96, :], csq[1].ap, ALU.mult, [bB.res, csq[1].res], [t2.res])
                    tt(POOL, L["QT"][qi].ap[0:96, h, :], t1.ap, t2.ap, ALU.add, [t1.res, t2.res], [L["QT_res"][qi][h]])

            if DBG_STOP <= 4:
                return
            mla_qpass(0)
            for n in range(NB):
                qi = n % 2
                for c in range(2):
                    gates_for_pair(L, wQ, 192, n, c)
                    for h in (2 * c, 2 * c + 1):
                        ob = attn_block(L, n, h, qi, 96, "mla", cnt, pend, finq)
                        finq.append((ob, lambda ob=ob, n=n, h=h, qi=qi, c=hc[0]: finish_head(L, ob, n, h, qi, 0 + 64 * h, c)))
                        hc[0] += 1
                    if c == 0 and n + 1 < NB:
                        mla_qpass(n + 1)
            drain(pend, finq)

        def kv_pass(L, l, colK, colV, kmean=None):
            load_w(wK, dr["w_in"][l][:, colK:colK + 256], 256)
            load_w(wK, dr["w_in"][l][:, colV:colV + 256], 256, col0=256)
            for n in range(NB):
                blk = slice(n * 512, (n + 1) * 512)
                for c in range(2):
                    bk = bW.next()
                    inproj_fm(wK, 128 * c, 128, n, bk)
                    cp(DVE, L["KT"].ap[0:64, 2 * c, blk], bk.f32[0:64, :], [bk.res], [L["KT_res"][2 * c][n]])
                    cp(POOL if False else DVE, L["KT"].ap[0:64, 2 * c + 1, blk], bk.f32[64:128, :], [bk.res], [L["KT_res"][2 * c + 1][n]])
                    if kmean is not None:
                        P.op(DVE, lambda e, o=kmean.ap[:, c, 2 * n:2 * n + 2], i=bk.f32.rearrange("p (b t) -> p b t", b=2):
                             e.tensor_reduce(o, i, AX.X, ALU.add), ph([bk.res]), [kmean.res])
                for tl in range(4):
                    t = 4 * n + tl
                    bk = bW.next()
                    inproj_tm(wK, 256, 256, t, bk)
                    cp(DVE, L["V"].ap[:, t, :, 0:64], bk.f32[:, 0:256].rearrange("p (h d) -> p h d", h=4), [bk.res], [L["V_res"][t]])

        def q_pass(L, n, qi, scale, qf=None):
            for c in range(2):
                bk = bW.next()
                inproj_fm(wQ, 128 * c, 128, n, bk)
                for hh in range(2):
                    h = 2 * c + hh
                    ts(DVE, L["QT"][qi].ap[0:64, h, :], bk.f32[64 * hh:64 * hh + 64, :], scale, None, ALU.mult, None,
                       [bk.res], [L["QT_res"][qi][h]])
                    if qf is not None:
                        cp(DVE, qf.ap[:, h, :], bk.f32[64 * hh:64 * hh + 64, :], [bk.res], [qf.res])

        def mixer_sb(l):
            barrier()
            L = layout_attn()
            a = L["alloc"]
            E = [[a.get([128, 512], F32) for _ in range(2)] for _ in range(2)]
            LN = [[LNR[0], LNR[1]], [LNR[2], LNR[3]]]
            TS = [a.get([128, 512], F32) for _ in range(2)]
            c0w = 352
            kv_pass(L, l, c0w + 256, c0w + 512)
            load_w(wQ, dr["w_in"][l][:, c0w:c0w + 256], 256)
            load_w(wQ, dr["w_in"][l][:, 2656:2912], 256, col0=256)
            cnt, hc = [0], [0]
            for n in range(NB):
                qi = n % 2
                q_pass(L, n, qi, 0.125)
                gates_for_block(L, wQ, 256, n, qi)
                tiles = [(4 * n + r, r) for r in (3, 2, 1, 0)] + \
                        [(j, -1) for j in range(4 * n - 1, max(4 * n - 1 - SB_WINDOW_TILES, -1), -1)]
                ntl = len(tiles)
                for hp in range(2):
                    heads = (2 * hp, 2 * hp + 1)
                    obs = [bO.next(), bO.next()]
                    Bs = [bW.next(), bW.next()]
                    for hh, h in enumerate(heads):
                        mm(obs[hh].f32[0:64, :], L["V"].ap[:, 4 * n + 3, h, 0:64], zeros_bf.ap[:], True, False,
                           [zeros_bf.res, L["V_res"][4 * n + 3]], [obs[hh].res], skip_group_check=True)
                        mm(Bs[hh].f32, ident.ap[:], zeros_bf.ap[:], True, False, [ident.res, zeros_bf.res], [Bs[hh].res], skip_group_check=True)

                    def stage_a(idx):
                        j, r = tiles[idx]
                        c0 = 128 * r if r > 0 else 0
                        cs = slice(c0, 512)
                        for hh, h in enumerate(heads):
                            e_, ln_ = E[hh][idx % 2], LN[hh][idx % 2]
                            zb = bS.next()
                            mm(zb.f32[:, cs], L["KT"].ap[0:64, h, j * 128:(j + 1) * 128], L["QT"][qi].ap[0:64, h, cs], True, True,
                               [L["KT_res"][h][j // 4], L["QT_res"][qi][h]], [zb.res])
                            act(e_.ap[:, cs], zb.f32[:, cs], AF.Exp, [zb.res], [e_.res], scale=-1.0)
                            act(e_.ap[:, cs], e_.ap[:, cs], AF.Ln, [e_.res], [e_.res], bias=1.0)
                            tt(DVE, ln_.r[:, cs], e_.ap[:, cs], zb.f32[:, cs], ALU.add, [e_.res, zb.res], [ln_.res])
                            if r >= 0:
                                tt(POOL, ln_.r[:, c0:c0 + 128], ln_.r[:, c0:c0 + 128], tri_lt.ap[:], ALU.mult,
                                   [ln_.res, tri_lt.res], [ln_.res])

                    def stage_b(idx):
                        j, r = tiles[idx]
                        c0 = 128 * r if r > 0 else 0
                        cs = slice(c0, 512)
                        for hh, h in enumerate(heads):
                            e_, ln_, ts_ = E[hh][idx % 2], LN[hh][idx % 2], TS[hh]
                            B, ob = Bs[hh], obs[hh]
                            pt = L["PT"][cnt[0] % 4]
                            cnt[0] += 1
                            mm(B.f32[:, cs], tri_gt_r.r, ln_.r[:, cs], False, False, [tri_gt_r.res, ln_.res], [B.res], skip_group_check=True)
                            tt(DVE, ts_.ap[:, cs], e_.ap[:, cs], B.f32[:, cs], ALU.add, [e_.res, B.res], [ts_.res])
                            act(pt.ap[:, cs], ts_.ap[:, cs], AF.Exp, [ts_.res], [pt.res], scale=-1.0)
                            if r >= 0:
                                tt(POOL, pt.ap[:, c0:c0 + 128], pt.ap[:, c0:c0 + 128], tri_lt.ap[:], ALU.mult,
                                   [pt.res, tri_lt.res], [pt.res])
                            if idx < ntl - 1:
                                mm(B.f32[:, cs], tri_le_r.r, ln_.r[:, cs], False, False, [tri_le_r.res, ln_.res], [B.res], skip_group_check=True)
                            mm(ob.f32[0:64, cs], L["V"].ap[:, j, h, 0:64], pt.ap[:, cs], False, idx == ntl - 1,
                               [pt.res, L["V_res"][j]], [ob.res], skip_group_check=True)

                    stage_a(0)
                    for idx in range(ntl):
                        if idx + 1 < ntl:
                            stage_a(idx + 1)
                        stage_b(idx)
                    for hh, h in enumerate(heads):
                        ys = L["YST"][hc[0] % 2]
                        P.op(DVE, lambda e, o=ys.ap, a_=obs[hh].f32[0:64, :], b_=L["SG"][qi].ap[:, h, :]:
                             e.scalar_tensor_tensor(o, a_, 0.5, b_, ALU.mult, ALU.mult), ph([obs[hh].res, L["SG_res"][qi][h]]), [ys.res])
                        row0 = 256 + 64 * h
                        dma(SP, yT_h[row0:row0 + 64, n * 512:(n + 1) * 512], ys.ap, ph([ys.res]), [r_yT[row0 // 128][n]], ys.res)
                        hc[0] += 1

        def mixer_moba(l):
            barrier()
            L = layout_attn()
            a = L["alloc"]
            L["GT"] = a.get([128, 4, GT_W], BF16)
            L["STG"] = [a.get([128, 512], F32) for _ in range(2)]
            kmean = a.get([128, 2, 16], F32)
            kmh = a.get([64, 4, 16], F32)
            qf = a.get([64, 4, 512], F32)
            g16 = a.get([128, 4, 16], F32)
            top8 = a.get([128, 4, 8], F32)
            mb = a.get([128, 4, 32], BF16)
            init_V_ones(L)
            dma(POOL, L["GT"].ap, dr["gt"], ph([]), [L["GT"].res], L["GT"].res)
            P.op(POOL, lambda e, o=mb.ap: e.memset(o, 0.0), ph([]), [mb.res])
            for h in range(4):
                P.op(POOL, lambda e, o=L["KT"].ap[64:80, h, :], i=dr["onehot"]: e.dma_start(out=o, in_=i),
                     ph([]), [L["KT_res"][h][nn] for nn in range(NB)], dma_slot=L["KT_res"][h][0])
            c0w = 352 + 768
            kv_pass(L, l, c0w + 256, c0w + 512, kmean=kmean)
            for h in range(4):
                cp(DVE, kmh.ap[:, h, :], kmean.ap[64 * (h % 2):64 * (h % 2) + 64, h // 2, :], [kmean.res], [kmh.res])
            load_w(wQ, dr["w_in"][l][:, c0w:c0w + 256], 256)
            load_w(wQ, dr["w_in"][l][:, 2912:3168], 256, col0=256)
            pend, finq, cnt, hc = [], [], [0], [0]

            def moba_qpass(n):
                qi = n % 2
                q_pass(L, n, qi, 0.125, qf=qf)
                for tl in range(4):
                    T = 4 * n + tl
                    ob_ = T // 2
                    bg = bW.next()
                    for h in range(4):
                        mm(bg.f32[:, 16 * h:16 * h + 16], qf.ap[:, h, tl * 128:(tl + 1) * 128], kmh.ap[:, h, :], True, True,
                           [qf.res, kmh.res], [bg.res])
                    tt(DVE, g16.ap, bg.f32[:, 0:64].rearrange("p (h b) -> p h b", h=4),
                       addm.ap[:, ob_, :].unsqueeze(1).to_broadcast([128, 4, 16]), ALU.add, [bg.res, addm.res], [g16.res])
                    for h in range(4):
                        P.op(DVE, lambda e, o=top8.ap[:, h, :], i=g16.ap[:, h, :]: e.max(o, i), ph([g16.res]), [top8.res])
                    for h in range(4):
                        ts(DVE, mb.ap[:, h, 0:16], g16.ap[:, h, :], top8.ap[:, h, 3:4], -30000.0, ALU.is_lt, ALU.mult,
                           [g16.res, top8.res], [mb.res])
                    bt = bW.next()
                    P.op(PE, lambda e, o=bt.bf[:, 0:128], i_=mb.ap.rearrange("p h b -> p (h b)"): e.transpose(o, i_, ident.ap[:]),
                         ph([mb.res, ident.res]), [bt.res])
                    for h in range(4):
                        cp(DVE, L["QT"][qi].ap[64:80, h, tl * 128:(tl + 1) * 128], bt.bf[32 * h:32 * h + 16, 0:128],
                           [bt.res], [L["QT_res"][qi][h]])

            moba_qpass(0)
            for n in range(NB):
                qi = n % 2
                for c in range(2):
                    gates_for_pair(L, wQ, 256, n, c)
                    for h in (2 * c, 2 * c + 1):
                        ob = attn_block(L, n, h, qi, 80, "moba", cnt, pend, finq)
                        finq.append((ob, lambda ob=ob, n=n, h=h, qi=qi, c=hc[0]: finish_head(L, ob, n, h, qi, 512 + 64 * h, c)))
                        hc[0] += 1
                    if c == 0 and n + 1 < NB:
                        moba_qpass(n + 1)
            drain(pend, finq)

        def mixer_conv(l):
            barrier()
            a = Alloc()
            PADW = 32
            xg = a.get([128, 2, PADW + S], BF16)
            r_xg = [[Res() for _ in range(NB)] for _ in range(2)]
            dg = a.get([128, 2, 31, 128], BF16)
            dwT = a.get([128, 2, 31], F32)
            vb = {k: a.get([128, 2], F32) for k in ("dw_b", "ln_g", "ln_b", "pw_b")}
            pw = a.get([128, 2, 256], BF16)
            sig = [a.get([128, 512], F32) for _ in range(2)]
            yc = a.get([128, 2, 512], F32)
            ysq = a.get([128, 2, 512], F32)
            mean = a.get([128, 512], F32)
            rstd = a.get([128, 512], F32)
            msq = a.get([128, 512], F32)
            sw = a.get([128, 2, 512], BF16)
            ee = a.get([128, 512], F32)
            sg = [a.get([128, 512], F32) for _ in range(2)]
            yo = [a.get([128, 512], BF16) for _ in range(2)]
            identf = a.get([128, 128], F32)
            c0w = 352 + 1536
            load_w(wK, dr["w_in"][l][:, c0w:c0w + 512], 512)
            load_w(wQ, dr["w_in"][l][:, 3168:3424], 256)
            dma(SP, dwT.ap, dr["dw_wT"][l], ph([]), [dwT.res], dwT.res)
            for k_, b_ in vb.items():
                dma(SP, b_.ap, dr[k_][l], ph([]), [b_.res], b_.res)
            dma(SP, identf.ap, dr["ident"], ph([]), [identf.res], identf.res)
            P.op(POOL, lambda e, o=pw.ap, i=dr["pw_w"][l].rearrange("(c p) n -> p c n", p=128): e.dma_start(out=o, in_=i),
                 ph([]), [pw.res], dma_slot=pw.res)
            ts(DVE, pw.ap, pw.ap, 0.25, None, ALU.mult, None, [pw.res], [pw.res])
            ts(DVE, vb["pw_b"].ap, vb["pw_b"].ap, 0.5, None, ALU.mult, None, [vb["pw_b"].res], [vb["pw_b"].res])
            for c in range(2):
                for j in range(31):
                    ts(POOL if j % 2 else DVE, dg.ap[:, c, j, :], identf.ap, dwT.ap[:, c, j:j + 1], 0.5, ALU.mult, ALU.mult,
                       [identf.res, dwT.res], [dg.res])
                P.op(POOL, lambda e, o=xg.ap[:, c, 0:PADW]: e.memset(o, 0.0), ph([]), [r_xg[c][0]])
            for n in range(NB):
                for c in range(2):
                    ba, bg_ = bW.next(), bW.next()
                    inproj_fm(wK, 128 * c, 128, n, ba)
                    inproj_fm(wK, 256 + 128 * c, 128, n, bg_)
                    s_ = sig[c]
                    act(s_.ap, bg_.f32, AF.Tanh, [bg_.res], [s_.res], scale=0.5)
                    P.op(DVE, lambda e, o=xg.ap[:, c, PADW + n * 512:PADW + (n + 1) * 512], a_=s_.ap, b_=ba.f32:
                         e.scalar_tensor_tensor(o, a_, 1.0, b_, ALU.add, ALU.mult), ph([s_.res, ba.res]), [r_xg[c][n]])
            for n in range(NB):
                for c in range(2):
                    bc = bW.next()
                    rd = [dg.res, r_xg[c][n]] + ([r_xg[c][n - 1]] if n > 0 else [])
                    for j in range(31):
                        st0 = PADW + n * 512 - 30 + j
                        mm(bc.f32, dg.ap[:, c, j, :], xg.ap[:, c, st0:st0 + 512], j == 0, j == 30, rd, [bc.res])
                    ts(DVE, yc.ap[:, c, :], bc.f32, vb["dw_b"].ap[:, c:c + 1], None, ALU.add, None, [bc.res, vb["dw_b"].res], [yc.res])
                    act(ysq.ap[:, c, :], yc.ap[:, c, :], AF.Square, [yc.res], [ysq.res])
                bm, bq = bW.next(), bW.next()
                mm(bm.f32, ones.ap[:], yc.ap[:, 0, :], True, False, [ones.res, yc.res], [bm.res])
                mm(bm.f32, ones.ap[:], yc.ap[:, 1, :], False, True, [ones.res, yc.res], [bm.res])
                mm(bq.f32, ones.ap[:], ysq.ap[:, 0, :], True, False, [ones.res, ysq.res], [bq.res])
                mm(bq.f32, ones.ap[:], ysq.ap[:, 1, :], False, True, [ones.res, ysq.res], [bq.res])
                ts(DVE, mean.ap, bm.f32, 1.0 / 256, None, ALU.mult, None, [bm.res], [mean.res])
                tt(DVE, msq.ap, mean.ap, mean.ap, ALU.mult, [mean.res], [msq.res])
                P.op(DVE, lambda e, o=msq.ap, a_=bq.f32, b_=msq.ap: e.scalar_tensor_tensor(o, a_, 1.0 / 256, b_, ALU.mult, ALU.subtract),
                     ph([bq.res, msq.res]), [msq.res])
                act(rstd.ap, msq.ap, AF.Ln, [msq.res], [rstd.res], bias=EPS)
                act(rstd.ap, rstd.ap, AF.Exp, [rstd.res], [rstd.res], scale=-0.5)
                for c in range(2):
                    tt(DVE, yc.ap[:, c, :], yc.ap[:, c, :], mean.ap, ALU.subtract, [yc.res, mean.res], [yc.res])
                    tt(DVE, yc.ap[:, c, :], yc.ap[:, c, :], rstd.ap, ALU.mult, [yc.res, rstd.res], [yc.res])
                    ts(DVE, yc.ap[:, c, :], yc.ap[:, c, :], vb["ln_g"].ap[:, c:c + 1], vb["ln_b"].ap[:, c:c + 1], ALU.mult, ALU.add,
                       [yc.res, vb["ln_g"].res, vb["ln_b"].res], [yc.res])
                    act(ee.ap, yc.ap[:, c, :], AF.Tanh, [yc.res], [ee.res], scale=0.5)
                    P.op(DVE, lambda e, o=sw.ap[:, c, :], a_=ee.ap, b_=yc.ap[:, c, :]:
                         e.scalar_tensor_tensor(o, a_, 1.0, b_, ALU.add, ALU.mult), ph([ee.res, yc.res]), [sw.res])
                for c in range(2):
                    bgt = bW.next()
                    inproj_fm(wQ, 128 * c, 128, n, bgt)
                    g_ = sg[c]
                    act(g_.ap, bgt.f32, AF.Tanh, [bgt.res], [g_.res], scale=0.5)
                    P.op(DVE, lambda e, o=g_.ap, a_=g_.ap, b_=bgt.f32: e.scalar_tensor_tensor(o, a_, 1.0, b_, ALU.add, ALU.mult),
                         ph([g_.res, bgt.res]), [g_.res])
                    bp = bW.next()
                    mm(bp.f32, pw.ap[:, 0, 128 * c:128 * c + 128], sw.ap[:, 0, :], True, False, [pw.res, sw.res], [bp.res])
                    mm(bp.f32, pw.ap[:, 1, 128 * c:128 * c + 128], sw.ap[:, 1, :], False, True, [pw.res, sw.res], [bp.res])
                    y_ = yo[c]
                    P.op(DVE, lambda e, o=y_.ap, a_=bp.f32, s1=vb["pw_b"].ap[:, c:c + 1], b_=g_.ap:
                         e.scalar_tensor_tensor(o, a_, s1, b_, ALU.add, ALU.mult), ph([bp.res, vb["pw_b"].res, g_.res]), [y_.res])
                    row0 = 768 + 128 * c
                    dma(SP, yT_h[row0:row0 + 128, n * 512:(n + 1) * 512], y_.ap, ph([y_.res]), [r_yT[row0 // 128][n]], y_.res)

        MIX = {"mla": mixer_mla, "sb": mixer_sb, "moba": mixer_moba, "conv": mixer_conv}
        phase_A0()
        fin = []
        for l in range(n_layers):
            for m in MIX_ALL:
                if m in mixers:
                    MIX[m](l)
            fin = phase_C(l, last=(l == n_layers - 1))
        barrier()
        last_bar = P.ops[DVE][-1]
        P.emit(final_waits=[(SP, d) for d in fin[-2:]] + [(SP, last_bar)])
    return nc


_CACHE = {}


def kernel(**inputs):
    inp = {k: np.asarray(v, dtype=np.float32) for k, v in inputs.items()}
    w = host_layout(inp)
    if "nc" not in _CACHE:
        _CACHE["nc"] = build()
    nc = _CACHE["nc"]
    x = inp["x"]
    in_maps = []
    for c in range(8):
        m = {"x": np.ascontiguousarray(x[c])}
        m.update(w)
        in_maps.append(m)
    res = run_bass_kernel_spmd(nc, in_maps, core_ids=list(range(8)))
    return np.stack([np.asarray(r["out"], dtype=np.float32) for r in res.results], axis=0)
```

```python
import contextlib
import math
import numpy as np
import concourse.bass as bass
import concourse.mybir as mybir
from concourse.bass_utils import run_bass_kernel_spmd

F32 = mybir.dt.float32
BF16 = mybir.dt.bfloat16
F32R = mybir.dt.float32r
ALU = mybir.AluOpType
AF = mybir.ActivationFunctionType
AX = mybir.AxisListType

S = 4096
D = 1024
NL = 4
NT = 32
NB = 8
EPS = 1e-6
DBG_STOP = 99
DBG_SKIP = ''
SB_WINDOW_TILES = 2

PE, ACT, DVE, POOL, SP = "pe", "act", "dve", "pool", "sp"
COMPUTE = (PE, ACT, DVE, POOL)


class Res:
    __slots__ = ("name", "writer", "readers", "slot", "excl", "persist")

    def __init__(self, name="", excl=False, persist=False):
        self.name = name
        self.excl = excl
        self.persist = persist
        self.writer = None
        self.readers = []
        self.slot = None


class SemSlot:
    __slots__ = ("sem", "count", "sw")

    def __init__(self):
        self.sem = None
        self.count = 0
        self.sw = False


class Op:
    __slots__ = ("eng", "idx", "fn", "waits", "signal", "count", "is_dma", "dma_res",
                 "dma_count", "n_dma")

    def __init__(self, eng, idx, fn):
        self.eng = eng
        self.idx = idx
        self.fn = fn
        self.waits = []
        self.signal = False
        self.count = 0
        self.is_dma = False
        self.dma_res = None
        self.dma_count = 0
        self.n_dma = 0


class Prog:
    def __init__(self, nc):
        self.nc = nc
        self.ops = {e: [] for e in (PE, ACT, DVE, POOL, SP)}
        self.seen = {e: {} for e in self.ops}
        self.slots = []
        self.free_slots = {True: [], False: []}
        self.phase_res = []

    def release_phase_slots(self):
        for r in self.phase_res:
            if r.slot is not None:
                self.free_slots[r.slot.sw].append(r.slot)
                r.slot = None
        self.phase_res = []

    def op(self, eng, fn, reads=(), writes=(), dma_slot=None, n_dma=1):
        lst = self.ops[eng]
        o = Op(eng, len(lst), fn)
        ex = [r for r in reads if r.excl and r not in writes]
        if ex:
            writes = list(writes) + ex
        if dma_slot is not None:
            sw = (eng == POOL)
            if dma_slot.slot is not None and dma_slot.slot.sw != sw:
                dma_slot.slot = None
            if dma_slot.slot is None:
                if self.free_slots[sw]:
                    dma_slot.slot = self.free_slots[sw].pop()
                else:
                    dma_slot.slot = SemSlot()
                    dma_slot.slot.sw = sw
                    self.slots.append(dma_slot.slot)
                if not dma_slot.persist:
                    self.phase_res.append(dma_slot)
            o.is_dma = True
            o.dma_res = dma_slot.slot
            o.n_dma = n_dma
            dma_slot.slot.count += 16 * n_dma
            o.dma_count = dma_slot.slot.count
        need = {}

        def same(d):
            return (not d.is_dma) and (not o.is_dma) and d.eng == eng

        def add(d):
            if d is None or d is o:
                return
            if d.is_dma:
                key, val = ("dma", id(d.dma_res)), d.dma_count
            else:
                key, val = ("eng", d.eng), d.idx
            if key not in need or val > need[key][0]:
                need[key] = (val, d)

        for r in reads:
            d = r.writer
            if d is not None and not (same(d) and eng == PE):
                add(d)
        for w in writes:
            d = w.writer
            if d is not None and not same(d):
                add(d)
            for rd in w.readers:
                if not same(rd):
                    add(rd)
        seen = self.seen[eng]
        for key, (val, d) in need.items():
            if seen.get(key, -1) >= val:
                continue
            seen[key] = val
            o.waits.append(d)
            if not d.is_dma:
                d.signal = True
        for r in reads:
            r.readers.append(o)
        for w in writes:
            w.writer = o
            w.readers = []
        lst.append(o)
        return o

    def emit(self, final_waits=()):
        nc = self.nc
        final = {}
        for (e, d) in final_waits:
            final.setdefault(e, []).append(d)
            if not d.is_dma:
                d.signal = True
        for e in COMPUTE:
            c = 0
            for o in self.ops[e]:
                if o.signal:
                    c += 1
                o.count = c
        with contextlib.ExitStack() as st:
            esem = {e: st.enter_context(nc.semaphore("prog_" + e)) for e in COMPUTE}
            for i, r in enumerate(self.slots):
                r.sem = st.enter_context(nc.semaphore("dma_%d" % i))
            block = st.enter_context(nc.Block())

            def wait(eng, d):
                if d.is_dma:
                    eng.wait_ge(d.dma_res.sem, d.dma_count)
                else:
                    eng.wait_ge(esem[d.eng], d.count)

            def run(e):
                def body(eng):
                    for o in self.ops[e]:
                        for d in o.waits:
                            wait(eng, d)
                        ins = o.fn(eng)
                        if o.is_dma:
                            if not isinstance(ins, (list, tuple)):
                                ins = [ins]
                            assert len(ins) == o.n_dma, (len(ins), o.n_dma)
                            for i_ in ins:
                                i_.then_inc(o.dma_res.sem, 16)
                        elif o.signal:
                            ins.then_inc(esem[e], 1)
                    for d in final.get(e, []):
                        wait(eng, d)
                return body

            block.tensor(run(PE))
            block.scalar(run(ACT))
            block.vector(run(DVE))
            block.gpsimd(run(POOL))
            block.sync(run(SP))


def _t5_bucket(n):
    n = np.maximum(n, 0)
    max_exact = 16
    n_large = np.maximum(n, max_exact).astype(np.float32)
    large = max_exact + (np.log(n_large / max_exact) / math.log(1024 / max_exact) * (32 - max_exact)).astype(np.int32)
    large = np.minimum(large, 31)
    return np.where(n < max_exact, n, large)


GT_W = 1664


def host_consts():
    c = {}
    c["ident"] = np.eye(128, dtype=np.float32)
    p = np.arange(128)[:, None]
    q = np.arange(128)[None, :]
    c["tri_le"] = (p <= q).astype(np.float32)
    c["tri_lt"] = (p < q).astype(np.float32)
    c["tri_gt"] = (p > q).astype(np.float32)
    c["ones"] = np.ones((128, 128), np.float32)
    half = 16
    freqs = (10000.0 ** (-np.arange(half, dtype=np.float32) / half)).astype(np.float32)
    ang = np.arange(S, dtype=np.float32)[:, None] * freqs[None, :]
    cos = np.cos(ang).astype(np.float32).T
    sin = np.sin(ang).astype(np.float32).T
    sc = np.float32(96 ** -0.5)
    rq_c = np.zeros((96, S), np.float32)
    rq_s = np.zeros((96, S), np.float32)
    rq_c[0:64] = sc
    rq_c[64:80] = cos * sc
    rq_c[80:96] = cos * sc
    rq_s[64:80] = -sin * sc
    rq_s[80:96] = sin * sc
    rk_c = np.zeros((96, S), np.float32)
    rk_s = np.zeros((96, S), np.float32)
    rk_c[64:80] = cos
    rk_c[80:96] = cos
    rk_s[64:80] = -sin
    rk_s[80:96] = sin
    c["rq_c"], c["rq_s"], c["rk_c"], c["rk_s"] = rq_c, rq_s, rk_c, rk_s
    oh = np.zeros((16, S), np.float32)
    for b in range(16):
        oh[b, b * 256:(b + 1) * 256] = 1.0
    c["onehot"] = oh
    am = np.zeros((16, 16), np.float32)
    for ob in range(16):
        am[ob, ob] = 1e30
        am[ob, ob + 1:] = -1e30
    c["addm"] = np.ascontiguousarray(np.broadcast_to(am[None], (128, 16, 16)))
    return c


def host_layout(inp):
    w = {}
    w_in = inp["w_in"]
    w["w_in"] = np.ascontiguousarray(w_in)
    wk = np.zeros((NL, D, 320), np.float32)
    wk[:, :, 0:128] = w_in[:, :, 192:320]
    wk[:, :, 192:224] = w_in[:, :, 320:352]
    wk[:, :, 288:304] = w_in[:, :, 336:352]
    wk[:, :, 304:320] = w_in[:, :, 320:336]
    w["w_mla_k"] = wk
    w["gpre"] = np.ascontiguousarray(inp["pre_norm_g"].reshape(NL, 8, 128).transpose(0, 2, 1))
    gq = np.zeros((NL, 128, 2), np.float32)
    gq[:, :, 0] = inp["mla_q_norm_g"][:, 0:128]
    gq[:, 0:64, 1] = inp["mla_q_norm_g"][:, 128:192]
    w["gq"] = gq
    uq = inp["mla_w_uq"].reshape(NL, 192, 4, 96)
    uq_p = np.zeros((NL, 128, 2, 4, 96), np.float32)
    uq_p[:, :, 0] = uq[:, 0:128]
    uq_p[:, 0:64, 1] = uq[:, 128:192]
    w["w_uq"] = uq_p
    sw = np.zeros((NL, 192, 4, 96), np.float32)
    sw[:, :, :, 64:80] = uq[:, :, :, 80:96]
    sw[:, :, :, 80:96] = uq[:, :, :, 64:80]
    sw_p = np.zeros((NL, 128, 2, 4, 96), np.float32)
    sw_p[:, :, 0] = sw[:, 0:128]
    sw_p[:, 0:64, 1] = sw[:, 128:192]
    w["w_uq_sw"] = sw_p
    w["gkv"] = np.ascontiguousarray(inp["mla_kv_norm_g"].reshape(NL, 128, 1))
    ukv = inp["mla_w_ukv"].reshape(NL, 128, 4, 128)
    w["w_uk"] = np.ascontiguousarray(ukv[:, :, :, 0:64])
    w["w_uv"] = np.ascontiguousarray(ukv[:, :, :, 64:128].reshape(NL, 128, 256))
    rb = inp["rel_bias"]
    pp = np.arange(128)[:, None]
    xx = np.arange(GT_W)[None, :]
    bucket = _t5_bucket(xx - pp)
    w["gt"] = np.ascontiguousarray(rb[bucket].transpose(0, 2, 1))
    w["c31"] = np.ascontiguousarray(np.broadcast_to(rb[31][None, :], (128, 4)))
    w["dw_wT"] = np.ascontiguousarray(inp["conv_dw_w"].transpose(0, 2, 1).reshape(NL, 2, 128, 31).transpose(0, 2, 1, 3))

    def pv(a):
        return np.ascontiguousarray(a.reshape(NL, 2, 128).transpose(0, 2, 1))
    w["dw_b"] = pv(inp["conv_dw_b"])
    w["ln_g"] = pv(inp["conv_ln_g"])
    w["ln_b"] = pv(inp["conv_ln_b"])
    w["pw_b"] = pv(inp["conv_pw_b"])
    w["pw_w"] = np.ascontiguousarray(inp["conv_pw_w"])
    w["w_out"] = np.ascontiguousarray(inp["w_out"])
    w["post_g"] = np.ascontiguousarray(inp["post_norm_g"])
    w.update(host_consts())
    return w


WSHAPES = {
    "w_in": [NL, D, 3424], "w_mla_k": [NL, D, 320], "gpre": [NL, 128, 8], "gq": [NL, 128, 2],
    "w_uq": [NL, 128, 2, 4, 96], "w_uq_sw": [NL, 128, 2, 4, 96], "gkv": [NL, 128, 1],
    "w_uk": [NL, 128, 4, 64], "w_uv": [NL, 128, 256], "gt": [128, 4, GT_W], "c31": [128, 4],
    "dw_wT": [NL, 128, 2, 31], "dw_b": [NL, 128, 2], "ln_g": [NL, 128, 2], "ln_b": [NL, 128, 2],
    "pw_b": [NL, 128, 2], "pw_w": [NL, 256, 256], "w_out": [NL, D, D], "post_g": [NL, D],
    "ident": [128, 128], "tri_le": [128, 128], "tri_lt": [128, 128], "tri_gt": [128, 128],
    "ones": [128, 128], "rq_c": [96, S], "rq_s": [96, S], "rk_c": [96, S], "rk_s": [96, S],
    "onehot": [16, S], "addm": [128, 16, 16],
}


class Bank:
    def __init__(self, t):
        self.t = t
        self.res = Res(excl=True)
        self.f32 = t[:]
        self.bf = t[:].bitcast(BF16)


class RR:
    def __init__(self, items):
        self.items = items
        self.i = 0

    def next(self):
        it = self.items[self.i % len(self.items)]
        self.i += 1
        return it


class Buf:
    def __init__(self, ap, persist=False):
        self.ap = ap if isinstance(ap, bass.AP) else ap[:]
        self.res = Res(persist=persist)

    @property
    def r(self):
        return self.ap.bitcast(F32R)


ARENA_BYTES = 111104
MIX_ALL = ("mla", "sb", "moba", "conv")


def build(n_layers=NL, mixers=MIX_ALL, dbg=False):
    nc = bass.Bass("TRN2", target_bir_lowering=False)
    P = Prog(nc)
    dr = {}
    x_in = nc.dram_tensor("x", [S, D], F32, kind="ExternalInput").ap()
    for k, shp in WSHAPES.items():
        dr[k] = nc.dram_tensor(k, shp, F32, kind="ExternalInput").ap()
    out = nc.dram_tensor("out", [S, D], F32, kind="ExternalOutput").ap()
    yT_h = nc.dram_tensor("yT", [D, S], BF16, kind="ExternalOutput" if dbg else "Internal").ap()
    r_yT = [[Res() for _ in range(NB)] for _ in range(8)]
    r_xh = [Res() for _ in range(NT)]

    st = contextlib.ExitStack()
    with st:
        def sb(name, shape, dt):
            return st.enter_context(nc.sbuf_tensor("s_" + name, shape, dt))

        hT = sb("hT", [128, 8, S], BF16)
        r_hT = [Res() for _ in range(NT)]
        wK = Buf(sb("wK", [128, 8, 512], BF16), persist=True)
        wQ = Buf(sb("wQ", [128, 8, 512], BF16), persist=True)
        ident = Buf(sb("ident", [128, 128], BF16), persist=True)
        tri_le = Buf(sb("tri_le", [128, 128], BF16), persist=True)
        tri_lt = Buf(sb("tri_lt", [128, 128], BF16), persist=True)
        tri_gt = Buf(sb("tri_gt", [128, 128], F32), persist=True)
        tri_lef = Buf(sb("tri_lef", [128, 128], F32), persist=True)
        ones = Buf(sb("ones", [128, 128], F32), persist=True)
        gpre = Buf(sb("gpre", [128, NL, 8], F32), persist=True)
        c31 = Buf(sb("c31", [128, 4], F32), persist=True)
        zeros_bf = Buf(sb("zeros_bf", [128, 512], BF16), persist=True)
        addm = Buf(sb("addm", [128, 16, 16], F32), persist=True)
        LNR = [Buf(sb("lnr%d" % i, [128, 512], F32), persist=True) for i in range(4)]
        arena_t = sb("arena", [128, ARENA_BYTES // 4], F32)
        r_phase = Res("phase")
        dummy = Buf(sb("dummy", [128, 8], F32), persist=True)

        def view(off, shape, dt):
            isz = 2 if dt == BF16 else 4
            n = 1
            for s_ in shape[1:]:
                n *= s_
            nb = n * isz
            assert off % 4 == 0 and nb % 4 == 0 and off + nb <= ARENA_BYTES, (off, nb)
            ap = arena_t[:, off // 4:(off + nb) // 4]
            if dt == BF16:
                ap = ap.bitcast(BF16)
            if len(shape) == 3:
                ap = ap.rearrange("p (a b) -> p a b", a=shape[1])
            elif len(shape) == 4:
                ap = ap.rearrange("p (a b c) -> p a b c", a=shape[1], b=shape[2])
            if shape[0] != 128:
                ap = ap[0:shape[0]]
            return ap

        class Alloc:
            def __init__(self, start=0):
                self.off = start

            def get(self, shape, dt):
                isz = 2 if dt == BF16 else 4
                n = 1
                for s_ in shape[1:]:
                    n *= s_
                nb = (n * isz + 31) // 32 * 32
                v = view(self.off, shape, dt)
                b_ = Buf(v)
                b_.off = self.off
                self.off += nb
                return b_

        banks = [Bank(st.enter_context(nc.psum_tensor("bank%d" % i, [128, 512], F32))) for i in range(8)]
        bS = RR(banks[0:3])
        bO = RR(banks[3:5])
        bW = RR(banks[5:8])
        PO = {}

        def set_pools(S_, O_, F_, G_):
            PO["S"], PO["O"], PO["F"], PO["G"] = RR(S_), RR(O_), RR(F_), RR(G_)

        set_pools(banks[0:3], banks[3:5], banks[5:8], banks[5:8])

        def dma(q, out_ap, in_ap, reads, writes, slot):
            return P.op(q, lambda e, o=out_ap, i=in_ap: e.dma_start(out=o, in_=i), reads, writes, dma_slot=slot)

        def barrier():
            P.op(DVE, lambda e: e.memset(dummy.ap, 0.0), reads=[], writes=[r_phase, dummy.res])
            P.release_phase_slots()

        def ph(reads):
            return list(reads) + [r_phase]

        def mm(o_ap, lhsT, rhs, start, stop, reads, writes, **kw):
            return P.op(PE, lambda e, o=o_ap, l=lhsT, r=rhs, s0=start, s1=stop, kw=kw:
                        e.matmul(o, l, r, start=s0, stop=s1, **kw), ph(reads), writes)

        def act(o_ap, i_ap, func, reads, writes, scale=1.0, bias=None, accum=None):
            def f(e, o=o_ap, i=i_ap, fu=func, sc=scale, b=bias, a=accum):
                kw = {}
                if b is not None:
                    kw["bias"] = b
                if a is not None:
                    kw["accum_out"] = a
                return e.activation(o, i, fu, scale=sc, **kw)
            return P.op(ACT, f, ph(reads), writes)

        def tt(eng, o_ap, a, b, op, reads, writes):
            return P.op(eng, lambda e, o=o_ap, a=a, b=b, op=op: e.tensor_tensor(o, a, b, op), ph(reads), writes)

        def ts(eng, o_ap, a, s1, s2, op0, op1, reads, writes):
            if op1 is None:
                return P.op(eng, lambda e, o=o_ap, a=a, s1=s1, op0=op0: e.tensor_scalar(o, a, s1, None, op0), ph(reads), writes)
            return P.op(eng, lambda e, o=o_ap, a=a, s1=s1, s2=s2, op0=op0, op1=op1:
                        e.tensor_scalar(o, a, s1, s2, op0, op1), ph(reads), writes)

        def cp(eng, o_ap, i_ap, reads, writes):
            return P.op(eng, lambda e, o=o_ap, i=i_ap: e.tensor_copy(o, i), ph(reads), writes)

        def rsqrt_act(o_ap, i_ap, n_mean, reads, writes):
            act(o_ap, i_ap, AF.Ln, reads, writes, scale=1.0 / n_mean, bias=EPS)
            act(o_ap, o_ap, AF.Exp, writes, writes, scale=-0.5)

        for (b_, nm) in [(ident, "ident"), (tri_le, "tri_le"), (tri_lt, "tri_lt")]:
            dma(POOL, b_.ap[:], dr[nm], [], [b_.res], b_.res)
        dma(SP, tri_gt.ap[:], dr["tri_gt"], [], [tri_gt.res], tri_gt.res)
        dma(SP, tri_lef.ap[:], dr["tri_le"], [], [tri_lef.res], tri_lef.res)
        dma(SP, ones.ap[:], dr["ones"], [], [ones.res], ones.res)
        dma(SP, gpre.ap[:], dr["gpre"].rearrange("l p c -> p l c"), [], [gpre.res], gpre.res)
        dma(SP, c31.ap[:], dr["c31"], [], [c31.res], c31.res)
        ones_r = Buf(sb("ones_r", [128, 128], F32), persist=True)
        tri_gt_r = Buf(sb("tri_gt_r", [128, 128], F32), persist=True)
        tri_le_r = Buf(sb("tri_le_r", [128, 128], F32), persist=True)
        for (dst_, src_) in [(ones_r, ones), (tri_gt_r, tri_gt), (tri_le_r, tri_lef)]:
            P.op(DVE, lambda e, o=dst_.r, i=src_.ap: e.tensor_copy(o, i), [src_.res], [dst_.res])
        dma(SP, addm.ap[:], dr["addm"], [], [addm.res], addm.res)
        P.op(POOL, lambda e: e.memset(zeros_bf.ap[:], 0.0), [], [zeros_bf.res])

        def load_w(buf, src_ap, ncols, col0=0):
            P.op(POOL, lambda e, o=buf.ap[:, :, col0:col0 + ncols], i=src_ap.rearrange("(c p) n -> p c n", p=128):
                 e.dma_start(out=o, in_=i), [], [buf.res], dma_slot=buf.res)

        def run(g):
            for _ in g:
                pass

        BGH = [None]

        def bg_step(k=1):
            g = BGH[0]
            if g is None:
                return
            for _ in range(k):
                try:
                    next(g)
                except StopIteration:
                    BGH[0] = None
                    return

        def bg_flush():
            if BGH[0] is not None:
                run(BGH[0])
                BGH[0] = None

        def inproj_fm_g(wbuf, c0, ncols, n, bank, extra_reads=()):
            for kc in range(8):
                mm(bank.f32[0:ncols, :], wbuf.ap[:, kc, c0:c0 + ncols], hT[:, kc, n * 512:(n + 1) * 512],
                   kc == 0, kc == 7, [wbuf.res] + r_hT[4 * n:4 * n + 4] + list(extra_reads), [bank.res])
                if kc == 3:
                    yield

        def inproj_fm(wbuf, c0, ncols, n, bank, extra_reads=()):
            run(inproj_fm_g(wbuf, c0, ncols, n, bank, extra_reads))

        def inproj_tm_g(wbuf, c0, ncols, t, bank):
            for kc in range(8):
                mm(bank.f32[:, 0:ncols], hT[:, kc, t * 128:(t + 1) * 128], wbuf.ap[:, kc, c0:c0 + ncols],
                   kc == 0, kc == 7, [wbuf.res, r_hT[t]], [bank.res])
                if kc == 3:
                    yield

        def inproj_tm(wbuf, c0, ncols, t, bank):
            run(inproj_tm_g(wbuf, c0, ncols, t, bank))

        def layout_C():
            a = Alloc()
            L = {}
            L["w_out"] = a.get([128, 8, 1024], BF16)
            L["yt"] = [a.get([128, 8, 512], BF16) for _ in range(2)]
            L["x"] = [a.get([128, 1024], F32) for _ in range(2)]
            L["xn"] = [a.get([128, 1024], F32) for _ in range(2)]
            L["xs"] = [a.get([128, 1024], BF16) for _ in range(2)]
            L["junk"] = a.get([128, 1024], BF16)
            L["postg"] = a.get([128, 1024], F32)
            L["st"] = [a.get([128, 8], F32) for _ in range(4)]
            return L

        def norm_transpose(L, l, t, xbuf, i, bpool=None):
            stt = L["st"][i % 4]
            xs = L["xs"][i % 2]
            act(L["junk"].ap, xbuf.ap, AF.Square, [xbuf.res], [L["junk"].res, stt.res], accum=stt.ap[:, 0:1])
            rsqrt_act(stt.ap[:, 1:2], stt.ap[:, 0:1], float(D), [stt.res], [stt.res])
            ts(DVE, xs.ap, xbuf.ap, stt.ap[:, 1:2], None, ALU.mult, None, [xbuf.res, stt.res], [xs.res])
            bk = (bpool or bW).next()
            for c in range(8):
                P.op(PE, lambda e, o=bk.bf[:, c * 128:(c + 1) * 128], i_=xs.ap[:, c * 128:(c + 1) * 128]:
                     e.transpose(o, i_, ident.ap[:]), ph([xs.res, ident.res]), [bk.res])
            tt(DVE, hT[:, :, t * 128:(t + 1) * 128], bk.bf[:, 0:1024].rearrange("p (c t) -> p c t", c=8),
               gpre.ap[:, l, :].unsqueeze(2).to_broadcast([128, 8, 128]), ALU.mult,
               [bk.res, gpre.res], [r_hT[t]])

        def phase_A0():
            barrier()
            L = layout_C()
            for t in range(NT):
                xb = L["x"][t % 2]
                dma(SP, xb.ap, x_in[t * 128:(t + 1) * 128, :], ph([]), [xb.res], xb.res)
                norm_transpose(L, 0, t, xb, t)

        def phase_C(l, last):
            barrier()
            L = layout_C()
            src = x_in if l == 0 else out
            for half in range(2):
                P.op(POOL, lambda e, o=L["w_out"].ap[:, :, half * 512:(half + 1) * 512],
                     i=dr["w_out"][l][:, half * 512:(half + 1) * 512].rearrange("(c p) n -> p c n", p=128):
                     e.dma_start(out=o, in_=i), ph([]), [L["w_out"].res], dma_slot=L["w_out"].res)
            dma(SP, L["postg"].ap, dr["post_g"][l:l + 1, :].to_broadcast([128, D]), ph([]), [L["postg"].res], L["postg"].res)
            fin = []
            pend_store = []
            bC = RR(banks)

            def stage1(t):
                n, tl = t // 4, t % 4
                yt = L["yt"][n % 2]
                if tl == 0:
                    P.op(SP, lambda e, o=yt.ap, i=yT_h[:, n * 512:(n + 1) * 512].rearrange("(c p) t -> p c t", p=128):
                         e.dma_start(out=o, in_=i), ph([r_yT[c][n] for c in range(8)]), [yt.res], dma_slot=yt.res)
                xb = L["x"][t % 2]
                dma(SP, xb.ap, src[t * 128:(t + 1) * 128, :], ph([r_xh[t]]), [xb.res], xb.res)
                bk2 = [bC.next(), bC.next()]
                for hf in range(2):
                    for kc in range(8):
                        mm(bk2[hf].f32, yt.ap[:, kc, tl * 128:(tl + 1) * 128], L["w_out"].ap[:, kc, hf * 512:(hf + 1) * 512],
                           kc == 0, kc == 7, [yt.res, L["w_out"].res], [bk2[hf].res])
                return bk2

            def stage2(t, bk2):
                i = t
                xb = L["x"][i % 2]
                xn = L["xn"][i % 2]
                stt = L["st"][(i + 2) % 4]
                for hf in range(2):
                    act(L["junk"].ap[:, hf * 512:(hf + 1) * 512], bk2[hf].f32, AF.Square, [bk2[hf].res],
                        [L["junk"].res, stt.res], accum=stt.ap[:, 2 + hf:3 + hf])
                tt(DVE, stt.ap[:, 4:5], stt.ap[:, 2:3], stt.ap[:, 3:4], ALU.add, [stt.res], [stt.res])
                rsqrt_act(stt.ap[:, 5:6], stt.ap[:, 4:5], float(D), [stt.res], [stt.res])
                for hf in range(2):
                    sl = slice(hf * 512, (hf + 1) * 512)
                    P.op(DVE, lambda e, o=xn.ap[:, sl], a=bk2[hf].f32, s_=stt.ap[:, 5:6], b=L["postg"].ap[:, sl]:
                         e.scalar_tensor_tensor(o, a, s_, b, ALU.mult, ALU.mult),
                         ph([bk2[hf].res, stt.res, L["postg"].res]), [xn.res])
                tt(POOL, xn.ap, xn.ap, xb.ap, ALU.add, [xn.res, xb.res], [xn.res])
                if pend_store:
                    fin.append(pend_store.pop()())
                pend_store.append(lambda t=t, xn=xn: dma(SP, out[t * 128:(t + 1) * 128, :], xn.ap, ph([xn.res]), [r_xh[t]], xn.res))
                if not last:
                    norm_transpose(L, l + 1, t, xn, i, bC)

            cur = stage1(0)
            for t in range(NT):
                nxt = stage1(t + 1) if t + 1 < NT else None
                stage2(t, cur)
                cur = nxt
            fin.append(pend_store.pop()())
            return fin

        def layout_attn():
            a = Alloc()
            L = {}
            L["KT"] = a.get([128, 4, S], BF16)
            L["KT_res"] = [[Res() for _ in range(NB)] for _ in range(4)]
            L["V"] = a.get([128, NT, 4, 65], BF16)
            L["V_res"] = [Res() for _ in range(NT)]
            L["QT"] = [a.get([128, 4, 512], BF16) for _ in range(2)]
            L["QT_res"] = [[Res() for _ in range(4)] for _ in range(2)]
            L["PT"] = [a.get([128, 512], BF16) for _ in range(4)]
            L["YST"] = [a.get([64, 512], BF16) for _ in range(2)]
            sg_ = a.get([64, 4, 512], F32)
            L["SG"] = [sg_, sg_]
            sgr_ = [Res() for _ in range(4)]
            L["SG_res"] = [sgr_, sgr_]
            L["RD"] = [a.get([64, 512], F32) for _ in range(2)]
            L["DEN"] = [LNR[0], LNR[1]]
            L["GE"] = [a.get([64, 512], F32) for _ in range(2)]
            L["alloc"] = a
            return L

        def v_lhsT(L, t, h):
            return L["V"].ap[:, t, h, :]

        def zero_pad_rows(L):
            P.op(POOL, lambda e, o=L["KT"].ap[64:96, :, :]: e.memset(o, 0.0), ph([]),
                 [L["KT_res"][h][n] for h in range(4) for n in range(NB)])
            for qi in range(2):
                P.op(POOL, lambda e, o=L["QT"][qi].ap[64:96, :, :]: e.memset(o, 0.0), ph([]), list(L["QT_res"][qi]))

        def init_V_ones(L):
            P.op(POOL, lambda e, o=L["V"].ap[:, :, :, 64:65]: e.memset(o, 2.0), ph([]), list(L["V_res"]))

        def gates_for_pair(L, wbuf, c0, n, c):
            bk = PO["F"].next()
            inproj_fm(wbuf, c0 + 128 * c, 128, n, bk)
            for hh in range(2):
                h = 2 * c + hh
                ge = L["GE"][h % 2]
                sg = L["SG"][0].ap[:, h, :]
                r_sg = L["SG_res"][0][h]
                src = bk.f32[64 * hh:64 * hh + 64, :]
                act(ge.ap, src, AF.Tanh, [bk.res], [ge.res], scale=0.5)
                P.op(DVE, lambda e, o=sg, a_=ge.ap, b_=src: e.scalar_tensor_tensor(o, a_, 1.0, b_, ALU.add, ALU.mult),
                     ph([ge.res, bk.res]), [r_sg])

        def gates_for_block(L, wbuf, c0, n, qi):
            for c in range(2):
                gates_for_pair(L, wbuf, c0, n, c)

        def finish_head(L, ob, n, h, qi, row0, cnt):
            rd = L["RD"][cnt % 2]
            ys = L["YST"][cnt % 2]
            den = L["DEN"][cnt % 2]
            act(rd.ap[0:1, :], ob.f32[64:65, :], AF.Ln, [ob.res], [rd.res])
            act(den.r[64:65, :], rd.ap[0:1, :], AF.Exp, [rd.res], [den.res], scale=-1.0)
            bb = PO["F"].next()
            mm(bb.f32[0:64, :], ones_r.r[64:65, 0:64], den.r[64:65, :], True, True, [ones_r.res, den.res], [bb.res])
            tt(DVE, rd.ap, bb.f32[0:64, :], L["SG"][qi].ap[:, h, :], ALU.mult, [bb.res, L["SG_res"][qi][h]], [rd.res])
            tt(DVE, ys.ap, ob.f32[0:64, :], rd.ap, ALU.mult, [ob.res, rd.res], [ys.res])
            c, r0 = row0 // 128, row0 % 128
            dma(SP, yT_h[row0:row0 + 64, n * 512:(n + 1) * 512], ys.ap, ph([ys.res]), [r_yT[c][n]], ys.res)

        def attn_block(L, n, h, qi, dk, kind, cnt, pend, finq):
            ob = PO["O"].next()
            qt = L["QT"][qi].ap[0:dk, h, :]
            r_q = L["QT_res"][qi][h]
            tiles = [(4 * n + r, r) for r in range(4)] + [(j, -1) for j in range(4 * n)]
            ntl = len(tiles)
            for idx, (j, r) in enumerate(tiles):
                c0 = 128 * r if r > 0 else 0
                sbk = PO["S"].next()
                pt = L["PT"][(cnt[0]) % 4]
                cnt[0] += 1
                near = (kind == "moba" and (4 * n - j) <= 9)
                mm(sbk.f32[:, c0:512], L["KT"].ap[0:dk, h, j * 128:(j + 1) * 128], qt[:, c0:512], True, not near,
                   [L["KT_res"][h][j // 4], r_q], [sbk.res])
                if near:
                    dq = 512 * n - 128 * j
                    mm(sbk.f32[:, c0:512], ident.ap[:], L["GT"].ap[:, h, dq + c0:dq + 512], False, True,
                       [ident.res, L["GT"].res], [sbk.res])
                    act(pt.ap[:, c0:512], sbk.f32[:, c0:512], AF.Exp, [sbk.res], [pt.res])
                elif kind == "moba":
                    act(pt.ap[:, c0:512], sbk.f32[:, c0:512], AF.Exp, [sbk.res, c31.res], [pt.res], bias=c31.ap[:, h:h + 1])
                else:
                    act(pt.ap[:, c0:512], sbk.f32[:, c0:512], AF.Exp, [sbk.res], [pt.res])
                if r >= 0:
                    tt(POOL, pt.ap[:, c0:c0 + 128], pt.ap[:, c0:c0 + 128], tri_le.ap[:], ALU.mult, [pt.res, tri_le.res], [pt.res])
                pend.append((ob, v_lhsT(L, j, h), pt, c0, idx == 0, idx == ntl - 1, L["V_res"][j]))
                if len(pend) > 2:
                    flush_pv(pend)
                    while finq and all(finq[0][0] is not p_[0] for p_ in pend):
                        finq.pop(0)[1]()
                bg_step()
            return ob

        def flush_pv(pend):
            (ob, lhsT, pt, c0, s0, s1, r_v) = pend.pop(0)
            mm(ob.f32[0:65, c0:512], lhsT, pt.ap[:, c0:512], s0, s1, [pt.res, r_v], [ob.res], skip_group_check=True)

        def drain(pend, finq):
            while pend:
                flush_pv(pend)
            while finq:
                finq.pop(0)[1]()

        def mixer_mla(l):
            barrier()
            set_pools(banks[0:3], banks[3:5], banks[5:6], banks[6:8])
            L = layout_attn()
            a = L["alloc"]
            ckv = a.get([128, 512], BF16)
            sq = a.get([128, 2, 512], F32)
            rq = a.get([128, 512], F32)
            rkv = rq
            cq = a.get([128, 2, 512], BF16)
            tq = [a.get([96, 512], F32) for _ in range(2)]
            tk = tq
            csq = [a.get([96, 512], F32) for _ in range(2)]
            t12 = [a.get([96, 512], F32) for _ in range(2)]
            rtm = a.get([128, 8], F32)
            wuq = a.get([128, 2, 4, 96], BF16)
            wsw = a.get([128, 2, 4, 96], BF16)
            wuk = a.get([128, 4, 64], BF16)
            wuv = a.get([128, 256], BF16)
            wtmp = Buf(view(sq.off, [128, 2, 4, 96], F32)); wtmp.res = sq.res
            wtmp2 = Buf(view(tq[0].off, [128, 2, 4, 96], F32)); wtmp2.res = tq[0].res; wtmp2.res2 = tq[1].res
            wtmp3 = Buf(view(csq[0].off, [128, 4, 64], F32)); wtmp3.res = csq[0].res
            wtmp4 = Buf(view(csq[1].off, [128, 256], F32)); wtmp4.res = csq[1].res
            gq = a.get([128, 2], F32)
            gkv = a.get([128, 1], F32)
            init_V_ones(L)
            load_w(wK, dr["w_mla_k"][l], 320)
            load_w(wQ, dr["w_in"][l][:, 0:192], 192)
            load_w(wQ, dr["w_in"][l][:, 2400:2656], 256, col0=192)
            dma(SP, gq.ap, dr["gq"][l], ph([]), [gq.res], gq.res)
            dma(SP, gkv.ap, dr["gkv"][l], ph([]), [gkv.res], gkv.res)
            dma(SP, wtmp.ap, dr["w_uq"][l], ph([]), [wtmp.res], wtmp.res)
            dma(SP, wtmp2.ap, dr["w_uq_sw"][l], ph([]), [wtmp2.res, wtmp2.res2], wtmp2.res)
            dma(SP, wtmp3.ap, dr["w_uk"][l], ph([]), [wtmp3.res], wtmp3.res)
            dma(SP, wtmp4.ap, dr["w_uv"][l], ph([]), [wtmp4.res], wtmp4.res)
            for c in range(2):
                ts(DVE, wuq.ap[:, c], wtmp.ap[:, c], gq.ap[:, c:c + 1], None, ALU.mult, None, [wtmp.res, gq.res], [wuq.res])
                ts(DVE, wsw.ap[:, c], wtmp2.ap[:, c], gq.ap[:, c:c + 1], None, ALU.mult, None, [wtmp2.res, wtmp2.res2, gq.res], [wsw.res])
            ts(DVE, wuk.ap, wtmp3.ap, gkv.ap[:, 0:1], None, ALU.mult, None, [wtmp3.res, gkv.res], [wuk.res])
            ts(DVE, wuv.ap, wtmp4.ap, gkv.ap[:, 0:1], None, ALU.mult, None, [wtmp4.res, gkv.res], [wuv.res])
            def mla_kv(n):
                tkc, tks = tk[0], tk[1]
                blk = slice(n * 512, (n + 1) * 512)
                dma(SP, tkc.ap[64:96, :], dr["rk_c"][64:96, blk], ph([]), [tkc.res], tkc.res)
                dma(SP, tks.ap[64:96, :], dr["rk_s"][64:96, blk], ph([]), [tks.res], tks.res)
                b_ckv = PO["G"].next()
                yield from inproj_fm_g(wK, 0, 128, n, b_ckv)
                cp(DVE, ckv.ap, b_ckv.f32, [b_ckv.res], [ckv.res])
                act(sq.ap[:, 0, :], b_ckv.f32, AF.Square, [b_ckv.res], [sq.res])
                yield
                t1, t2 = t12[0], t12[1]
                b_kr = PO["G"].next()
                yield from inproj_fm_g(wK, 128, 96, n, b_kr)
                tt(DVE, t1.ap[64:96, :], b_kr.f32[64:96, :], tkc.ap[64:96, :], ALU.mult, [b_kr.res, tkc.res], [t1.res])
                yield
                b_ks = PO["G"].next()
                yield from inproj_fm_g(wK, 224, 96, n, b_ks)
                tt(DVE, t2.ap[64:96, :], b_ks.f32[64:96, :], tks.ap[64:96, :], ALU.mult, [b_ks.res, tks.res], [t2.res])
                yield
                for h in range(4):
                    tt(POOL if h % 2 else DVE, L["KT"].ap[64:96, h, blk], t1.ap[64:96, :], t2.ap[64:96, :], ALU.add,
                       [t1.res, t2.res], [L["KT_res"][h][n]])
                b_ss = PO["G"].next()
                mm(b_ss.f32, ones.ap[:], sq.ap[:, 0, :], True, True, [ones.res, sq.res], [b_ss.res])
                rsqrt_act(rkv.ap, b_ss.f32, 128.0, [b_ss.res], [rkv.res])
                yield
                for h in range(4):
                    bk = PO["G"].next()
                    mm(bk.f32[0:64, :], wuk.ap[:, h, :], ckv.ap, True, True, [wuk.res, ckv.res], [bk.res])
                    tt(DVE, L["KT"].ap[0:64, h, blk], bk.f32[0:64, :], rkv.ap[0:64, :], ALU.mult, [bk.res, rkv.res],
                       [L["KT_res"][h][n]])
                    yield
                b_st = PO["G"].next()
                for tl in range(4):
                    mm(b_st.f32[:, tl:tl + 1], sq.ap[:, 0, tl * 128:(tl + 1) * 128], ones.ap[:, 0:1], True, True,
                       [sq.res, ones.res], [b_st.res])
                rsqrt_act(rtm.ap[:, 0:4], b_st.f32[:, 0:4], 128.0, [b_st.res], [rtm.res])
                yield
                for tl in range(4):
                    t = 4 * n + tl
                    bk = PO["G"].next()
                    mm(bk.f32[:, 0:256], ckv.ap[:, tl * 128:(tl + 1) * 128], wuv.ap, True, True, [ckv.res, wuv.res], [bk.res])
                    ts(DVE, L["V"].ap[:, t, :, 0:64], bk.f32[:, 0:256].rearrange("p (h d) -> p h d", h=4), rtm.ap[:, tl:tl + 1], None, ALU.mult, None,
                       [bk.res, rtm.res], [L["V_res"][t]])
                    yield

            def mla_q(n):
                qi = n % 2
                blk = slice(n * 512, (n + 1) * 512)
                tqc, tqs = tq[0], tq[1]
                dma(SP, tqc.ap, dr["rq_c"][:, blk], ph([]), [tqc.res], tqc.res)
                dma(SP, tqs.ap, dr["rq_s"][:, blk], ph([]), [tqs.res], tqs.res)
                b0, b1 = PO["G"].next(), PO["G"].next()
                yield from inproj_fm_g(wQ, 0, 128, n, b0)
                cp(DVE, cq.ap[:, 0, :], b0.f32, [b0.res], [cq.res])
                act(sq.ap[:, 0, :], b0.f32, AF.Square, [b0.res], [sq.res])
                yield
                yield from inproj_fm_g(wQ, 128, 64, n, b1)
                cp(DVE, cq.ap[0:64, 1, :], b1.f32[0:64, :], [b1.res], [cq.res])
                act(sq.ap[0:64, 1, :], b1.f32[0:64, :], AF.Square, [b1.res], [sq.res])
                yield
                b_ss = PO["G"].next()
                mm(b_ss.f32, ones.ap[:], sq.ap[:, 0, :], True, False, [ones.res, sq.res], [b_ss.res])
                mm(b_ss.f32, ones.ap[0:64, :], sq.ap[0:64, 1, :], False, True, [ones.res, sq.res], [b_ss.res])
                rsqrt_act(rq.ap, b_ss.f32, 192.0, [b_ss.res], [rq.res])
                tt(DVE, csq[0].ap, tqc.ap, rq.ap[0:96, :], ALU.mult, [tqc.res, rq.res], [csq[0].res])
                tt(DVE, csq[1].ap, tqs.ap, rq.ap[0:96, :], ALU.mult, [tqs.res, rq.res], [csq[1].res])
                yield
                for h in range(4):
                    bA, bB = PO["G"].next(), PO["G"].next()
                    mm(bA.f32[0:96, :], wuq.ap[:, 0, h, :], cq.ap[:, 0, :], True, False, [wuq.res, cq.res], [bA.res])
                    mm(bA.f32[0:96, :], wuq.ap[0:64, 1, h, :], cq.ap[0:64, 1, :], False, True, [wuq.res, cq.res], [bA.res])
                    mm(bB.f32[0:96, :], wsw.ap[:, 0, h, :], cq.ap[:, 0, :], True, False, [wsw.res, cq.res], [bB.res])
                    mm(bB.f32[0:96, :], wsw.ap[0:64, 1, h, :], cq.ap[0:64, 1, :], False, True, [wsw.res, cq.res], [bB.res])
                    t1, t2 = t12[0], t12[1]
                    tt(DVE, t1.ap, bA.f32[0:96, :], csq[0].ap, ALU.mult, [bA.res, csq[0].res], [t1.res])
                    tt(DVE, t2.ap, bB.f32[0:96, :], csq[1].ap, ALU.mult, [bB.res, csq[1].res], [t2.res])
                    tt(POOL, L["QT"][qi].ap[0:96, h, :], t1.ap, t2.ap, ALU.add, [t1.res, t2.res], [L["QT_res"][qi][h]])
                    yield

            def mla_block(n):
                yield from mla_kv(n)
                yield from mla_q(n)

            pend, finq, cnt, hc = [], [], [0], [0]
            run(mla_block(0))
            for n in range(NB):
                qi = n % 2
                BGH[0] = mla_block(n + 1) if n + 1 < NB else None
                for c in range(2):
                    gates_for_pair(L, wQ, 192, n, c)
                    for h in (2 * c, 2 * c + 1):
                        ob = attn_block(L, n, h, qi, 96, "mla", cnt, pend, finq)
                        finq.append((ob, lambda ob=ob, n=n, h=h, qi=qi, c=hc[0]: finish_head(L, ob, n, h, qi, 0 + 64 * h, c)))
                        hc[0] += 1
                bg_flush()
            drain(pend, finq)

        def kv_load(l, colK, colV):
            load_w(wK, dr["w_in"][l][:, colK:colK + 256], 256)
            load_w(wK, dr["w_in"][l][:, colV:colV + 256], 256, col0=256)

        def kv_block(L, n, kmean=None, kmh=None, atomic=False):
            blk = slice(n * 512, (n + 1) * 512)
            for c in range(2):
                bk = PO["G"].next()
                if atomic:
                    inproj_fm(wK, 128 * c, 128, n, bk)
                else:
                    yield from inproj_fm_g(wK, 128 * c, 128, n, bk)
                cp(DVE, L["KT"].ap[0:64, 2 * c, blk], bk.f32[0:64, :], [bk.res], [L["KT_res"][2 * c][n]])
                cp(DVE, L["KT"].ap[0:64, 2 * c + 1, blk], bk.f32[64:128, :], [bk.res], [L["KT_res"][2 * c + 1][n]])
                if kmean is not None:
                    P.op(DVE, lambda e, o=kmean.ap[:, c, 2 * n:2 * n + 2], i=bk.f32.rearrange("p (b t) -> p b t", b=2):
                         e.tensor_reduce(o, i, AX.X, ALU.add), ph([bk.res]), [kmean.res])
                    for hh in range(2):
                        cp(DVE, kmh.ap[:, 2 * c + hh, 2 * n:2 * n + 2], kmean.ap[64 * hh:64 * hh + 64, c, 2 * n:2 * n + 2],
                           [kmean.res], [kmh.res])
                yield
            for tl in range(4):
                t = 4 * n + tl
                bk = PO["G"].next()
                if atomic:
                    inproj_tm(wK, 256, 256, t, bk)
                else:
                    yield from inproj_tm_g(wK, 256, 256, t, bk)
                cp(DVE, L["V"].ap[:, t, :, 0:64], bk.f32[:, 0:256].rearrange("p (h d) -> p h d", h=4), [bk.res], [L["V_res"][t]])
                yield

        def q_block(L, n, qi, scale, qf=None, atomic=False):
            for c in range(2):
                bk = PO["G"].next()
                if atomic:
                    inproj_fm(wQ, 128 * c, 128, n, bk)
                else:
                    yield from inproj_fm_g(wQ, 128 * c, 128, n, bk)
                for hh in range(2):
                    h = 2 * c + hh
                    ts(DVE, L["QT"][qi].ap[0:64, h, :], bk.f32[64 * hh:64 * hh + 64, :], scale, None, ALU.mult, None,
                       [bk.res], [L["QT_res"][qi][h]])
                    if qf is not None:
                        cp(DVE, qf.ap[:, h, :], bk.f32[64 * hh:64 * hh + 64, :], [bk.res], [qf.res])
                yield

        def mixer_sb(l):
            barrier()
            set_pools(banks[6:8], banks[0:2], banks[6:8], banks[6:8])
            Bs = banks[2:6]
            L = layout_attn()
            a = L["alloc"]
            E = [a.get([128, 512], F32) for _ in range(4)]
            LN = LNR
            TS = [a.get([128, 512], F32) for _ in range(4)]
            zero_pad_rows(L)
            c0w = 352
            kv_load(l, c0w + 256, c0w + 512)
            load_w(wQ, dr["w_in"][l][:, c0w:c0w + 256], 256)
            load_w(wQ, dr["w_in"][l][:, 2656:2912], 256, col0=256)
            cnt, hc = [0], [0]

            def sb_block(n):
                yield from kv_block(L, n, atomic=True)
                yield from q_block(L, n, n % 2, 0.125, atomic=True)

            run(sb_block(0))
            for n in range(NB):
                qi = n % 2
                BGH[0] = sb_block(n + 1) if n + 1 < NB else None
                gates_for_block(L, wQ, 256, n, qi)
                tiles = [(4 * n + r, r) for r in (3, 2, 1, 0)] + \
                        [(j, -1) for j in range(4 * n - 1, max(4 * n - 1 - SB_WINDOW_TILES, -1), -1)]
                ntl = len(tiles)
                obank = [PO["O"].next(), PO["O"].next()]

                def o_ap(h, cs):
                    return obank[h // 2].f32[64 * (h % 2):64 * (h % 2) + 64, cs]

                def tp(h):
                    return {"tile_position": (0, 64)} if h % 2 else {}

                for h in range(4):
                    mm(o_ap(h, slice(0, 512)), L["V"].ap[:, 4 * n + 3, h, 0:64], zeros_bf.ap[:], True, False,
                       [zeros_bf.res, L["V_res"][4 * n + 3]], [obank[h // 2].res], skip_group_check=True, **tp(h))
                    mm(Bs[h].f32, ident.ap[:], zeros_bf.ap[:], True, False, [ident.res, zeros_bf.res], [Bs[h].res], skip_group_check=True)
                for idx, (j, r) in enumerate(tiles):
                    c0 = 128 * r if r > 0 else 0
                    cs = slice(c0, 512)
                    for h in range(4):
                        e_, ln_ = E[h], LN[h]
                        zb = PO["S"].next()
                        mm(zb.f32[:, cs], L["KT"].ap[0:96, h, j * 128:(j + 1) * 128], L["QT"][qi].ap[0:96, h, cs], True, True,
                           [L["KT_res"][h][j // 4], L["QT_res"][qi][h]], [zb.res])
                        act(e_.ap[:, cs], zb.f32[:, cs], AF.Exp, [zb.res], [e_.res], scale=-1.0)
                        act(e_.ap[:, cs], e_.ap[:, cs], AF.Ln, [e_.res], [e_.res], bias=1.0)
                        tt(DVE, ln_.r[:, cs], e_.ap[:, cs], zb.f32[:, cs], ALU.add, [e_.res, zb.res], [ln_.res])
                        if r >= 0:
                            tt(POOL, ln_.r[:, c0:c0 + 128], ln_.r[:, c0:c0 + 128], tri_lt.ap[:], ALU.mult,
                               [ln_.res, tri_lt.res], [ln_.res])
                    for h in range(4):
                        e_, ln_, ts_ = E[h], LN[h], TS[h]
                        B = Bs[h]
                        pt = L["PT"][cnt[0] % 4]
                        cnt[0] += 1
                        mm(B.f32[:, cs], tri_gt_r.r, ln_.r[:, cs], False, False, [tri_gt_r.res, ln_.res], [B.res], skip_group_check=True)
                        tt(DVE, ts_.ap[:, cs], e_.ap[:, cs], B.f32[:, cs], ALU.add, [e_.res, B.res], [ts_.res])
                        act(pt.ap[:, cs], ts_.ap[:, cs], AF.Exp, [ts_.res], [pt.res], scale=-1.0)
                        if r >= 0:
                            tt(POOL, pt.ap[:, c0:c0 + 128], pt.ap[:, c0:c0 + 128], tri_lt.ap[:], ALU.mult,
                               [pt.res, tri_lt.res], [pt.res])
                        if idx < ntl - 1:
                            mm(B.f32[:, cs], tri_le_r.r, ln_.r[:, cs], False, False, [tri_le_r.res, ln_.res], [B.res], skip_group_check=True)
                        mm(o_ap(h, cs), L["V"].ap[:, j, h, 0:64], pt.ap[:, cs], False, idx == ntl - 1,
                           [pt.res, L["V_res"][j]], [obank[h // 2].res], skip_group_check=True, **tp(h))
                    bg_step(2)
                for h in range(4):
                    ys = L["YST"][hc[0] % 2]
                    P.op(DVE, lambda e, o=ys.ap, a_=o_ap(h, slice(0, 512)), b_=L["SG"][qi].ap[:, h, :]:
                         e.scalar_tensor_tensor(o, a_, 0.5, b_, ALU.mult, ALU.mult), ph([obank[h // 2].res, L["SG_res"][qi][h]]), [ys.res])
                    row0 = 256 + 64 * h
                    dma(SP, yT_h[row0:row0 + 64, n * 512:(n + 1) * 512], ys.ap, ph([ys.res]), [r_yT[row0 // 128][n]], ys.res)
                    hc[0] += 1
                bg_flush()

        def mixer_moba(l):
            barrier()
            set_pools(banks[0:3], banks[3:5], banks[5:6], banks[6:8])
            L = layout_attn()
            a = L["alloc"]
            L["GT"] = a.get([128, 4, GT_W], BF16)
            kmean = a.get([128, 2, 16], F32)
            kmh = a.get([64, 4, 16], F32)
            qf = a.get([64, 4, 512], F32)
            g16 = a.get([128, 4, 16], F32)
            top8 = a.get([128, 4, 8], F32)
            mb = a.get([128, 4, 32], BF16)
            init_V_ones(L)
            zero_pad_rows(L)
            dma(POOL, L["GT"].ap, dr["gt"], ph([]), [L["GT"].res], L["GT"].res)
            P.op(POOL, lambda e, o=mb.ap: e.memset(o, 0.0), ph([]), [mb.res])
            for h in range(4):
                P.op(POOL, lambda e, o=L["KT"].ap[64:80, h, :], i=dr["onehot"]: e.dma_start(out=o, in_=i),
                     ph([]), [L["KT_res"][h][nn] for nn in range(NB)], dma_slot=L["KT_res"][h][0])
            c0w = 352 + 768
            P.op(POOL, lambda e, o=kmean.ap: e.memset(o, 0.0), ph([]), [kmean.res])
            P.op(POOL, lambda e, o=kmh.ap: e.memset(o, 0.0), ph([]), [kmh.res])
            kv_load(l, c0w + 256, c0w + 512)
            load_w(wQ, dr["w_in"][l][:, c0w:c0w + 256], 256)
            load_w(wQ, dr["w_in"][l][:, 2912:3168], 256, col0=256)
            pend, finq, cnt, hc = [], [], [0], [0]

            def moba_q(n):
                qi = n % 2
                yield from q_block(L, n, qi, 0.125, qf=qf)
                for tl in range(4):
                    T = 4 * n + tl
                    ob_ = T // 2
                    bg = PO["G"].next()
                    for h in range(4):
                        mm(bg.f32[:, 16 * h:16 * h + 16], qf.ap[:, h, tl * 128:(tl + 1) * 128], kmh.ap[:, h, :], True, True,
                           [qf.res, kmh.res], [bg.res])
                    tt(DVE, g16.ap, bg.f32[:, 0:64].rearrange("p (h b) -> p h b", h=4),
                       addm.ap[:, ob_, :].unsqueeze(1).to_broadcast([128, 4, 16]), ALU.add, [bg.res, addm.res], [g16.res])
                    yield
                    for h in range(4):
                        P.op(DVE, lambda e, o=top8.ap[:, h, :], i=g16.ap[:, h, :]: e.max(o, i), ph([g16.res]), [top8.res])
                    for h in range(4):
                        ts(DVE, mb.ap[:, h, 0:16], g16.ap[:, h, :], top8.ap[:, h, 3:4], -30000.0, ALU.is_lt, ALU.mult,
                           [g16.res, top8.res], [mb.res])
                    yield
                    bt = PO["G"].next()
                    P.op(PE, lambda e, o=bt.bf[:, 0:128], i_=mb.ap.rearrange("p h b -> p (h b)"): e.transpose(o, i_, ident.ap[:]),
                         ph([mb.res, ident.res]), [bt.res])
                    for h in range(4):
                        cp(DVE, L["QT"][qi].ap[64:80, h, tl * 128:(tl + 1) * 128], bt.bf[32 * h:32 * h + 16, 0:128],
                           [bt.res], [L["QT_res"][qi][h]])
                    yield

            def moba_block(n):
                yield from kv_block(L, n, kmean=kmean, kmh=kmh)
                yield from moba_q(n)

            run(moba_block(0))
            for n in range(NB):
                qi = n % 2
                BGH[0] = moba_block(n + 1) if n + 1 < NB else None
                for c in range(2):
                    gates_for_pair(L, wQ, 256, n, c)
                    for h in (2 * c, 2 * c + 1):
                        ob = attn_block(L, n, h, qi, 96, "moba", cnt, pend, finq)
                        finq.append((ob, lambda ob=ob, n=n, h=h, qi=qi, c=hc[0]: finish_head(L, ob, n, h, qi, 512 + 64 * h, c)))
                        hc[0] += 1
                bg_flush()
            drain(pend, finq)

        def mixer_conv(l):
            barrier()
            a = Alloc()
            PADW = 32
            xg = a.get([128, 2, PADW + S], BF16)
            r_xg = [[Res() for _ in range(NB)] for _ in range(2)]
            dg = a.get([128, 2, 31, 128], BF16)
            dwT = a.get([128, 2, 31], F32)
            vb = {k: a.get([128, 2], F32) for k in ("dw_b", "ln_g", "ln_b", "pw_b")}
            pw = a.get([128, 2, 256], BF16)
            sig = [a.get([128, 512], F32) for _ in range(2)]
            yc = a.get([128, 2, 512], F32)
            ysq = a.get([128, 2, 512], F32)
            mean = a.get([128, 512], F32)
            rstd = a.get([128, 512], F32)
            msq = a.get([128, 512], F32)
            sw = a.get([128, 2, 512], BF16)
            ee = a.get([128, 512], F32)
            sg = [a.get([128, 512], F32) for _ in range(2)]
            yo = [a.get([128, 512], BF16) for _ in range(2)]
            identf = a.get([128, 128], F32)
            c0w = 352 + 1536
            load_w(wK, dr["w_in"][l][:, c0w:c0w + 512], 512)
            load_w(wQ, dr["w_in"][l][:, 3168:3424], 256)
            dma(SP, dwT.ap, dr["dw_wT"][l], ph([]), [dwT.res], dwT.res)
            for k_, b_ in vb.items():
                dma(SP, b_.ap, dr[k_][l], ph([]), [b_.res], b_.res)
            dma(SP, identf.ap, dr["ident"], ph([]), [identf.res], identf.res)
            P.op(POOL, lambda e, o=pw.ap, i=dr["pw_w"][l].rearrange("(c p) n -> p c n", p=128): e.dma_start(out=o, in_=i),
                 ph([]), [pw.res], dma_slot=pw.res)
            ts(DVE, pw.ap, pw.ap, 0.25, None, ALU.mult, None, [pw.res], [pw.res])
            ts(DVE, vb["pw_b"].ap, vb["pw_b"].ap, 0.5, None, ALU.mult, None, [vb["pw_b"].res], [vb["pw_b"].res])
            for c in range(2):
                for j in range(31):
                    ts(POOL if j % 2 else DVE, dg.ap[:, c, j, :], identf.ap, dwT.ap[:, c, j:j + 1], 0.5, ALU.mult, ALU.mult,
                       [identf.res, dwT.res], [dg.res])
                P.op(POOL, lambda e, o=xg.ap[:, c, 0:PADW]: e.memset(o, 0.0), ph([]), [r_xg[c][0]])
            for n in range(NB):
                for c in range(2):
                    ba, bg_ = bW.next(), bW.next()
                    inproj_fm(wK, 128 * c, 128, n, ba)
                    inproj_fm(wK, 256 + 128 * c, 128, n, bg_)
                    s_ = sig[c]
                    act(s_.ap, bg_.f32, AF.Tanh, [bg_.res], [s_.res], scale=0.5)
                    P.op(DVE, lambda e, o=xg.ap[:, c, PADW + n * 512:PADW + (n + 1) * 512], a_=s_.ap, b_=ba.f32:
                         e.scalar_tensor_tensor(o, a_, 1.0, b_, ALU.add, ALU.mult), ph([s_.res, ba.res]), [r_xg[c][n]])
            for n in range(NB):
                for c in range(2):
                    bc = bW.next()
                    rd = [dg.res, r_xg[c][n]] + ([r_xg[c][n - 1]] if n > 0 else [])
                    for j in range(31):
                        st0 = PADW + n * 512 - 30 + j
                        mm(bc.f32, dg.ap[:, c, j, :], xg.ap[:, c, st0:st0 + 512], j == 0, j == 30, rd, [bc.res])
                    ts(DVE, yc.ap[:, c, :], bc.f32, vb["dw_b"].ap[:, c:c + 1], None, ALU.add, None, [bc.res, vb["dw_b"].res], [yc.res])
                    act(ysq.ap[:, c, :], yc.ap[:, c, :], AF.Square, [yc.res], [ysq.res])
                bm, bq = bW.next(), bW.next()
                mm(bm.f32, ones.ap[:], yc.ap[:, 0, :], True, False, [ones.res, yc.res], [bm.res])
                mm(bm.f32, ones.ap[:], yc.ap[:, 1, :], False, True, [ones.res, yc.res], [bm.res])
                mm(bq.f32, ones.ap[:], ysq.ap[:, 0, :], True, False, [ones.res, ysq.res], [bq.res])
                mm(bq.f32, ones.ap[:], ysq.ap[:, 1, :], False, True, [ones.res, ysq.res], [bq.res])
                ts(DVE, mean.ap, bm.f32, 1.0 / 256, None, ALU.mult, None, [bm.res], [mean.res])
                tt(DVE, msq.ap, mean.ap, mean.ap, ALU.mult, [mean.res], [msq.res])
                P.op(DVE, lambda e, o=msq.ap, a_=bq.f32, b_=msq.ap: e.scalar_tensor_tensor(o, a_, 1.0 / 256, b_, ALU.mult, ALU.subtract),
                     ph([bq.res, msq.res]), [msq.res])
                act(rstd.ap, msq.ap, AF.Ln, [msq.res], [rstd.res], bias=EPS)
                act(rstd.ap, rstd.ap, AF.Exp, [rstd.res], [rstd.res], scale=-0.5)
                for c in range(2):
                    tt(DVE, yc.ap[:, c, :], yc.ap[:, c, :], mean.ap, ALU.subtract, [yc.res, mean.res], [yc.res])
                    tt(DVE, yc.ap[:, c, :], yc.ap[:, c, :], rstd.ap, ALU.mult, [yc.res, rstd.res], [yc.res])
                    ts(DVE, yc.ap[:, c, :], yc.ap[:, c, :], vb["ln_g"].ap[:, c:c + 1], vb["ln_b"].ap[:, c:c + 1], ALU.mult, ALU.add,
                       [yc.res, vb["ln_g"].res, vb["ln_b"].res], [yc.res])
                    act(ee.ap, yc.ap[:, c, :], AF.Tanh, [yc.res], [ee.res], scale=0.5)
                    P.op(DVE, lambda e, o=sw.ap[:, c, :], a_=ee.ap, b_=yc.ap[:, c, :]:
                         e.scalar_tensor_tensor(o, a_, 1.0, b_, ALU.add, ALU.mult), ph([ee.res, yc.res]), [sw.res])
                for c in range(2):
                    bgt = bW.next()
                    inproj_fm(wQ, 128 * c, 128, n, bgt)
                    g_ = sg[c]
                    act(g_.ap, bgt.f32, AF.Tanh, [bgt.res], [g_.res], scale=0.5)
                    P.op(DVE, lambda e, o=g_.ap, a_=g_.ap, b_=bgt.f32: e.scalar_tensor_tensor(o, a_, 1.0, b_, ALU.add, ALU.mult),
                         ph([g_.res, bgt.res]), [g_.res])
                    bp = bW.next()
                    mm(bp.f32, pw.ap[:, 0, 128 * c:128 * c + 128], sw.ap[:, 0, :], True, False, [pw.res, sw.res], [bp.res])
                    mm(bp.f32, pw.ap[:, 1, 128 * c:128 * c + 128], sw.ap[:, 1, :], False, True, [pw.res, sw.res], [bp.res])
                    y_ = yo[c]
                    P.op(DVE, lambda e, o=y_.ap, a_=bp.f32, s1=vb["pw_b"].ap[:, c:c + 1], b_=g_.ap:
                         e.scalar_tensor_tensor(o, a_, s1, b_, ALU.add, ALU.mult), ph([bp.res, vb["pw_b"].res, g_.res]), [y_.res])
                    row0 = 768 + 128 * c
                    dma(SP, yT_h[row0:row0 + 128, n * 512:(n + 1) * 512], y_.ap, ph([y_.res]), [r_yT[row0 // 128][n]], y_.res)

        MIX = {"mla": mixer_mla, "sb": mixer_sb, "moba": mixer_moba, "conv": mixer_conv}
        phase_A0()
        fin = []
        for l in range(n_layers):
            for m in MIX_ALL:
                if m in mixers:
                    MIX[m](l)
            fin = phase_C(l, last=(l == n_layers - 1))
        barrier()
        last_bar = P.ops[DVE][-1]
        P.emit(final_waits=[(SP, d) for d in fin[-2:]] + [(SP, last_bar)])
    return nc


_CACHE = {}


def kernel(**inputs):
    inp = {k: np.asarray(v, dtype=np.float32) for k, v in inputs.items()}
    w = host_layout(inp)
    if "nc" not in _CACHE:
        _CACHE["nc"] = build()
    nc = _CACHE["nc"]
    x = inp["x"]
    in_maps = []
    for c in range(8):
        m = {"x": np.ascontiguousarray(x[c])}
        m.update(w)
        in_maps.append(m)
    res = run_bass_kernel_spmd(nc, in_maps, core_ids=list(range(8)))
    return np.stack([np.asarray(r["out"], dtype=np.float32) for r in res.results], axis=0)
```
